# Optimizing a Trainium2 kernel written in Bass

```python
import math
import jax, jax.numpy as jnp
from jax import lax
import numpy as np

D_MODEL = 1024
BATCH = 8
SEQ = 2048
DEPTH = 4

DN_HEADS = 8
DN_DK = 128
DN_DV = 128
DN_CONV = 4
DN_CHUNK = 64
SB_HEADS = 8
SB_DH = 64
SB_BLOCK = 128
D_FF = 3584
N_EXPERTS = 8
TOP_K = 2
MOE_BLOCK = 128
EPS = 1e-6
N_DENSE = (DEPTH + 1) // 2
N_MOE = DEPTH // 2
N_BRANCH = 2

DN_QK = DN_HEADS * DN_DK
DN_V = DN_HEADS * DN_DV
SB_W = SB_HEADS * SB_DH
_WIDTHS = (2 * DN_QK + DN_V, DN_V, DN_HEADS, DN_HEADS, SB_W, SB_W, SB_W, N_BRANCH * D_MODEL)
IN_COLS = sum(_WIDTHS)
SPLIT_AT = tuple(int(s) for s in np.cumsum(_WIDTHS)[:-1])

kernel_name = "hybrid_deltanet_stickbreaking_moe_adaln"


def rms_norm(x, w):
    xf = x.astype(jnp.float32)
    y = xf * lax.rsqrt(jnp.mean(xf * xf, axis=-1, keepdims=True) + EPS)
    return (y * w.astype(jnp.float32)).astype(x.dtype)


def l2_norm(x):
    xf = x.astype(jnp.float32)
    return xf * lax.rsqrt(jnp.sum(xf * xf, axis=-1, keepdims=True) + EPS)


def causal_depthwise_conv(x, w):
    k = w.shape[0]
    y = lax.conv_general_dilated(x, w[:, None, :].astype(x.dtype), window_strides=(1,),
                                 padding=[(k - 1, 0)], dimension_numbers=('NWC', 'WIO', 'NWC'),
                                 feature_group_count=x.shape[-1])
    return jax.nn.silu(y)


def gated_delta_rule(q, k, v, g, beta):
    B, S, H, DK = q.shape
    DV = v.shape[-1]
    N = S // DN_CHUNK

    def chunk(t):
        return jnp.moveaxis(t.reshape((B, N, DN_CHUNK) + t.shape[2:]), 2, 3)

    q, k, v, g, beta = (chunk(t) for t in (q, k, v, g, beta))
    g = jnp.cumsum(g, axis=-1)
    idx = jnp.arange(DN_CHUNK)
    lower = idx[:, None] >= idx[None, :]
    strict = idx[:, None] > idx[None, :]
    decay = jnp.exp(jnp.where(lower, g[..., :, None] - g[..., None, :], -jnp.inf))
    kk = jnp.einsum('bnhcd,bnhed->bnhce', k, k)
    lmat = jnp.where(strict, beta[..., :, None] * kk * decay, 0.0)
    amat = lmat + jnp.eye(DN_CHUNK, dtype=lmat.dtype)
    rhs = jnp.concatenate([v * beta[..., None], k * (beta * jnp.exp(g))[..., None]], axis=-1)
    sol = lax.linalg.triangular_solve(amat, rhs, left_side=True, lower=True, unit_diagonal=True)
    u, w = sol[..., :DV], sol[..., DV:]
    qk = jnp.einsum('bnhcd,bnhed->bnhce', q, k) * decay
    q_dec = q * jnp.exp(g)[..., None]
    k_dec = k * jnp.exp(g[..., -1:] - g)[..., None]
    g_last = jnp.exp(g[..., -1])

    def step(state, inp):
        qd, kd, u_c, w_c, qk_c, gl = inp
        v_new = u_c - jnp.einsum('bhcd,bhde->bhce', w_c, state)
        o = jnp.einsum('bhcd,bhde->bhce', qd, state) + jnp.einsum('bhcs,bhse->bhce', qk_c, v_new)
        state = state * gl[..., None, None] + jnp.einsum('bhcd,bhce->bhde', kd, v_new)
        return state, o

    xs = tuple(jnp.moveaxis(t, 1, 0) for t in (q_dec, k_dec, u, w, qk, g_last))
    _, o = lax.scan(step, jnp.zeros((B, H, DK, DV), jnp.float32), xs)
    return jnp.transpose(o, (1, 0, 3, 2, 4)).reshape(B, S, H, DV)


def stick_breaking_attention(q, k, v):
    B, S, H, Dh = q.shape
    nq = S // SB_BLOCK
    scale = Dh ** -0.5
    kf = k.astype(jnp.float32)
    vf = v.astype(jnp.float32)
    qb = jnp.moveaxis(q.astype(jnp.float32).reshape(B, nq, SB_BLOCK, H, Dh), 1, 0)
    key_pos = jnp.arange(S)

    def block(args):
        q_blk, i = args
        z = jnp.einsum('bqhd,bkhd->bhqk', q_blk, kf) * scale
        q_pos = i * SB_BLOCK + jnp.arange(SB_BLOCK)
        causal = key_pos[None, :] < q_pos[:, None]
        log_rem = jnp.where(causal, jax.nn.log_sigmoid(-z), 0.0)
        after = lax.cumsum(log_rem, axis=3, reverse=True) - log_rem
        attn = jnp.where(causal, jnp.exp(jax.nn.log_sigmoid(z) + after), 0.0)
        return jnp.einsum('bhqk,bkhd->bqhd', attn, vf)

    o = lax.map(block, (qb, jnp.arange(nq)))
    return jnp.moveaxis(o, 0, 1).reshape(B, S, H, Dh)


def hybrid_mixer(h, w_in, conv_w, a_log, dt_bias, out_norm, w_up_dn, w_up_sb, w_out):
    B, S, _ = h.shape
    proj = h @ w_in
    dn_qkv, dn_z, dn_b, dn_a, sb_q, sb_k, sb_v, gates = jnp.split(proj, SPLIT_AT, axis=-1)
    dn_qkv = causal_depthwise_conv(dn_qkv, conv_w)
    dq, dk, dv = jnp.split(dn_qkv, [DN_QK, 2 * DN_QK], axis=-1)
    q = l2_norm(dq.reshape(B, S, DN_HEADS, DN_DK)) * (DN_DK ** -0.5)
    k = l2_norm(dk.reshape(B, S, DN_HEADS, DN_DK))
    v = dv.reshape(B, S, DN_HEADS, DN_DV).astype(jnp.float32)
    beta = jax.nn.sigmoid(dn_b.astype(jnp.float32))
    g = -jnp.exp(a_log.astype(jnp.float32)) * jax.nn.softplus(dn_a.astype(jnp.float32) + dt_bias.astype(jnp.float32))
    o = gated_delta_rule(q, k, v, g, beta)
    o = rms_norm(o, out_norm) * jax.nn.silu(dn_z.reshape(B, S, DN_HEADS, DN_DV).astype(jnp.float32))
    y_dn = o.reshape(B, S, DN_V).astype(h.dtype) @ w_up_dn
    o_sb = stick_breaking_attention(sb_q.reshape(B, S, SB_HEADS, SB_DH),
                                    sb_k.reshape(B, S, SB_HEADS, SB_DH),
                                    sb_v.reshape(B, S, SB_HEADS, SB_DH))
    y_sb = o_sb.reshape(B, S, SB_W).astype(h.dtype) @ w_up_sb
    g_dn, g_sb = jnp.split(jax.nn.sigmoid(gates), N_BRANCH, axis=-1)
    return (g_dn * y_dn + g_sb * y_sb) @ w_out


def swiglu(h, w1, w3, w2):
    return (jax.nn.silu(h @ w1) * (h @ w3)) @ w2


def moe_swiglu(h, router_w, router_b, w1, w3, w2):
    B, S, D = h.shape
    T = B * S
    hf = h.reshape(T, D)
    logits = (hf @ router_w).astype(jnp.float32) + router_b.astype(jnp.float32)
    top_logit, top_idx = lax.top_k(logits, TOP_K)
    top_w = jax.nn.softmax(top_logit, axis=-1)
    n_assign = T * TOP_K
    flat_e = top_idx.reshape(-1).astype(jnp.int32)
    flat_tok = jnp.arange(n_assign, dtype=jnp.int32) // TOP_K
    order = jnp.argsort(flat_e)
    e_sorted = flat_e[order]
    counts = jnp.bincount(flat_e, length=N_EXPERTS).astype(jnp.int32)
    padded = (counts + MOE_BLOCK - 1) // MOE_BLOCK * MOE_BLOCK
    pad_end = jnp.cumsum(padded)
    pad_start = pad_end - padded
    start = jnp.cumsum(counts) - counts
    dest = pad_start[e_sorted] + jnp.arange(n_assign, dtype=jnp.int32) - start[e_sorted]
    n_rows = (n_assign + N_EXPERTS * (MOE_BLOCK - 1) + MOE_BLOCK - 1) // MOE_BLOCK * MOE_BLOCK
    n_blocks = n_rows // MOE_BLOCK
    row_tok = jnp.full((n_rows,), T, jnp.int32).at[dest].set(flat_tok[order])
    row_w = jnp.zeros((n_rows,), jnp.float32).at[dest].set(top_w.reshape(-1)[order])
    block_start = jnp.arange(n_blocks, dtype=jnp.int32) * MOE_BLOCK
    block_e = jnp.minimum(jnp.sum(pad_end[None, :] <= block_start[:, None], axis=1), N_EXPERTS - 1)
    h_pad = jnp.concatenate([hf, jnp.zeros((1, D), hf.dtype)], axis=0)

    def expert_block(args):
        toks, e = args
        xb = h_pad[toks]
        return (jax.nn.silu(xb @ w1[e]) * (xb @ w3[e])) @ w2[e]

    y = lax.map(expert_block, (row_tok.reshape(n_blocks, MOE_BLOCK), block_e))
    y = y.reshape(n_rows, D) * row_w[:, None].astype(h.dtype)
    out = jax.ops.segment_sum(y, row_tok, num_segments=T + 1)[:T]
    return out.reshape(B, S, D)


def setup_inputs(seed: int = 0) -> dict:
    key = jax.random.key(seed)
    ks = jax.random.split(key, 26)
    f32 = jnp.float32

    def nrm(k, shape, scale):
        return jax.random.normal(k, shape, f32) * scale

    L = DEPTH
    dt = jnp.exp(jax.random.uniform(ks[9], (L, DN_HEADS), f32, math.log(1e-3), math.log(1e-1)))
    return {
        "x": nrm(ks[0], (BATCH, SEQ, D_MODEL), 1.0),
        "c": nrm(ks[1], (BATCH, D_MODEL), 1.0),
        "ada_w": nrm(ks[2], (L, D_MODEL, 6 * D_MODEL), 0.5 * D_MODEL ** -0.5),
        "ada_b": nrm(ks[3], (L, 6 * D_MODEL), 0.02),
        "norm_mix": 1.0 + nrm(ks[4], (L, D_MODEL), 0.05),
        "norm_ffn": 1.0 + nrm(ks[5], (L, D_MODEL), 0.05),
        "w_in": nrm(ks[6], (L, D_MODEL, IN_COLS), D_MODEL ** -0.5),
        "conv_w": nrm(ks[7], (L, DN_CONV, 2 * DN_QK + DN_V), DN_CONV ** -0.5),
        "dn_a_log": jnp.log(jax.random.uniform(ks[8], (L, DN_HEADS), f32, 1.0, 16.0)),
        "dn_dt_bias": dt + jnp.log(-jnp.expm1(-dt)),
        "dn_out_norm": 1.0 + nrm(ks[10], (L, DN_DV), 0.05),
        "w_up_dn": nrm(ks[11], (L, DN_V, D_MODEL), DN_V ** -0.5),
        "w_up_sb": nrm(ks[12], (L, SB_W, D_MODEL), SB_W ** -0.5),
        "w_out": nrm(ks[13], (L, D_MODEL, D_MODEL), D_MODEL ** -0.5),
        "ffn_w1": nrm(ks[14], (N_DENSE, D_MODEL, D_FF), D_MODEL ** -0.5),
        "ffn_w3": nrm(ks[15], (N_DENSE, D_MODEL, D_FF), D_MODEL ** -0.5),
        "ffn_w2": nrm(ks[16], (N_DENSE, D_FF, D_MODEL), D_FF ** -0.5),
        "router_w": nrm(ks[17], (N_MOE, D_MODEL, N_EXPERTS), D_MODEL ** -0.5),
        "router_b": nrm(ks[18], (N_MOE, N_EXPERTS), 0.01),
        "moe_w1": nrm(ks[19], (N_MOE, N_EXPERTS, D_MODEL, D_FF), D_MODEL ** -0.5),
        "moe_w3": nrm(ks[20], (N_MOE, N_EXPERTS, D_MODEL, D_FF), D_MODEL ** -0.5),
        "moe_w2": nrm(ks[21], (N_MOE, N_EXPERTS, D_FF, D_MODEL), D_FF ** -0.5),
        "final_norm": 1.0 + nrm(ks[22], (D_MODEL,), 0.05),
    }


def reference(x, c, ada_w, ada_b, norm_mix, norm_ffn, w_in, conv_w, dn_a_log, dn_dt_bias,
              dn_out_norm, w_up_dn, w_up_sb, w_out, ffn_w1, ffn_w3, ffn_w2, router_w, router_b,
              moe_w1, moe_w3, moe_w2, final_norm):
    c_act = jax.nn.silu(c)
    for l in range(DEPTH):
        mod = (c_act @ ada_w[l] + ada_b[l])[:, None, :]
        sh1, sc1, gt1, sh2, sc2, gt2 = jnp.split(mod, 6, axis=-1)
        h = rms_norm(x, norm_mix[l]) * (1.0 + sc1) + sh1
        x = x + gt1 * hybrid_mixer(h, w_in[l], conv_w[l], dn_a_log[l], dn_dt_bias[l], dn_out_norm[l],
                                   w_up_dn[l], w_up_sb[l], w_out[l])
        h = rms_norm(x, norm_ffn[l]) * (1.0 + sc2) + sh2
        if l % 2 == 0:
            f = swiglu(h, ffn_w1[l // 2], ffn_w3[l // 2], ffn_w2[l // 2])
        else:
            j = l // 2
            f = moe_swiglu(h, router_w[j], router_b[j], moe_w1[j], moe_w3[j], moe_w2[j])
        x = x + gt2 * f
    return rms_norm(x, final_norm)
```

```python
import bisect
import contextlib
import numpy as np
import concourse.bass as bass
import concourse.mybir as mybir
from concourse.bass_utils import run_bass_kernel_spmd

F32 = mybir.dt.float32
BF16 = mybir.dt.bfloat16
AF = mybir.ActivationFunctionType
ALU = mybir.AluOpType

D = 1024
S = 2048
DEPTH = 4
NH = 8
DFF = 3584
NE = 8
INC = 7696
EPS = 1e-6
BIG = 30000.0
NCORES = 8
C_Q, C_K, C_V, C_Z, C_B, C_A = 0, 1024, 2048, 3072, 4096, 4104
C_SQ, C_SK, C_SV, C_G = 4112, 4624, 5136, 5648


class Sched:
    ROT = 20000

    def __init__(self, nc, es):
        self.nc, self.es = nc, es
        self.eng = {"pe": nc.tensor, "act": nc.scalar, "dve": nc.vector, "pool": nc.gpsimd, "sp": nc.sync}
        self.sem, self.cnt, self.nsem = {}, {}, 0
        self.seen = {e: {} for e in self.eng}
        self.last_w = {}
        self.readers = {}
        self.pending = {e: [] for e in self.eng}
        self.semobj = {}
        for e in ("pe", "act", "dve", "pool"):
            self._rot(e)
        self.ninst = 0
        self.dcnt = {}
        self.regions = {}
        self.dsem_by_key = {}
        self.carry = {}

    def _newsem(self, tag):
        self.nsem += 1
        s = self.es.enter_context(self.nc.semaphore(f"{tag}{self.nsem}"))
        self.semobj[id(s)] = s
        return s

    def _rot(self, e):
        self.sem[e] = self._newsem("s" + e)
        self.cnt[e] = 0

    def key(self, a):
        if isinstance(a, str):
            return a
        nm = a.tensor.name
        st = self.regions.get(nm)
        if st is None:
            return nm
        off = int(a.offset)
        idx = bisect.bisect_right(st, off) - 1
        return f"{nm}#{idx}"

    def set_regions(self, nm, starts):
        old = [k for k in list(self.last_w.keys()) + list(self.readers.keys()) if k == nm or k.startswith(nm + "#")]
        tags = []
        for k in set(old):
            lw = self.last_w.pop(k, None)
            if lw is not None:
                tags.append(lw)
            for (sid, (val, eng_r)) in self.readers.pop(k, {}).items():
                tags.append((self.semobj[sid], val, eng_r))
        self.regions[nm] = list(starts)
        self.carry[nm] = tags

    def _wait(self, e, tag):
        sem, val, _ = tag
        d = self.seen[e]
        if d.get(id(sem), -1) >= val:
            return
        self.eng[e].wait_ge(sem, val)
        d[id(sem)] = val
        self.ninst += 1

    def op(self, e, fn, W=(), R=(), inc=True, dma_sem=None):
        wk = [self.key(a) for a in W]
        rk = [self.key(a) for a in R]
        for k in wk + rk:
            if "#" in k:
                for tag in self.carry.get(k.split("#")[0], ()):
                    self._wait(e, tag)
        for k in rk:
            lw = self.last_w.get(k)
            if lw is not None:
                self._wait(e, lw)
        for k in wk:
            lw = self.last_w.get(k)
            if lw is not None and not (lw[2] == e == "pe"):
                self._wait(e, lw)
            for (sid, (val, eng_r)) in list(self.readers.get(k, {}).items()):
                self._wait(e, (self.semobj[sid], val, eng_r))
        ins = fn()
        self.ninst += 1
        if dma_sem is not None:
            sem, incv = dma_sem if len(dma_sem) == 2 else (dma_sem[0], 16)
            self.dcnt[id(sem)] = self.dcnt.get(id(sem), 0) + incv
            ins.then_inc(sem, incv)
            tag = (sem, self.dcnt[id(sem)], "dma")
        elif inc:
            self.cnt[e] += 1
            ins.then_inc(self.sem[e], 1)
            tag = (self.sem[e], self.cnt[e], e)
        else:
            self.pending[e].append((wk, rk))
            return ins
        items = self.pending[e] + [(wk, rk)]
        self.pending[e] = []
        for (wk_, rk_) in items:
            for k in rk_:
                self.readers.setdefault(k, {})[id(tag[0])] = (tag[1], tag[2])
            for k in wk_:
                self.last_w[k] = tag
                self.readers[k] = {}
        if dma_sem is None and self.cnt[e] >= self.ROT:
            self._rot(e)
        return ins

    def dma(self, q, out, in_, sem=None):
        eng = self.eng[q]
        k = self.key(out)
        if k not in self.dsem_by_key:
            self.dsem_by_key[k] = self._newsem("d")
        sem = self.dsem_by_key[k]
        return self.op(q, lambda: eng.dma_start(out=out, in_=in_), W=[out], R=[in_], dma_sem=(sem,))

    def collective(self, fn, out, in_):
        k = self.key(out)
        if k not in self.dsem_by_key:
            self.dsem_by_key[k] = self._newsem("g")
        return self.op("pool", fn, W=[out], R=[in_], dma_sem=(self.dsem_by_key[k], 1))

    def wait_all(self, e, keys):
        for k in keys:
            lw = self.last_w.get(self.key(k))
            if lw is not None:
                self._wait(e, lw)


def _consts():
    i = np.arange(128)[:, None]
    j = np.arange(128)[None, :]
    f, b = {}, {}
    f["ident"] = (i == j).astype(np.float32)
    f["ones"] = np.ones((128, 128), np.float32)
    f["tri"] = (i <= j).astype(np.float32)
    f["neg1"] = np.where(i > j, 0.0, BIG).astype(np.float32)
    f["mask3"] = np.where(j > i, 0.0, -BIG).astype(np.float32)
    f["mask2"] = np.where(j >= i, 0.0, -BIG).astype(np.float32)
    sel = np.zeros((128, 1024), np.float32)
    for h in range(8):
        sel[h, h * 128:(h + 1) * 128] = 1.0
    f["sel"] = sel
    f["hm0"] = np.where(i < 64, 0.125, 0.0).astype(np.float32)[:, :1]
    f["hm1"] = np.where(i >= 64, 0.125, 0.0).astype(np.float32)[:, :1]
    b["ident"] = f["ident"]
    b["ones"] = f["ones"]
    for l in range(7):
        s = 1 << l
        m = ((i // s) % 2 == 1) & ((j // s) == (i // s) - 1)
        b[f"lm{l}"] = m.astype(np.float32)
    b["lm0T"] = b["lm0"].T.copy()
    b["triincl"] = (i >= j).astype(np.float32)
    b["utri"] = (i < j).astype(np.float32)

    def pack(c):
        off, cols, o = {}, [], 0
        for k, v in c.items():
            off[k] = (o, v.shape[1])
            o += v.shape[1]
            cols.append(v)
        return np.concatenate(cols, axis=1), off
    return pack(f), pack(b)


(CSTF_NP, CSTF_OFF), (CSTB_NP, CSTB_OFF) = _consts()


def _small_params(inp, b):
    p = {}
    L = DEPTH
    p["c"] = inp["c"][b].reshape(8, 128).T
    p["ada_b"] = inp["ada_b"].reshape(L, 48, 128).transpose(2, 0, 1).reshape(128, L * 48)
    p["nmix"] = inp["norm_mix"].reshape(L, 8, 128).transpose(2, 0, 1).reshape(128, L * 8)
    p["nffn"] = inp["norm_ffn"].reshape(L, 8, 128).transpose(2, 0, 1).reshape(128, L * 8)
    p["conv"] = inp["conv_w"].reshape(L, 4, 24, 128).transpose(3, 0, 1, 2).reshape(128, L * 96)
    p["alog"] = np.broadcast_to(inp["dn_a_log"].reshape(1, L * 8), (128, L * 8))
    p["dtb"] = np.broadcast_to(inp["dn_dt_bias"].reshape(1, L * 8), (128, L * 8))
    p["onw"] = inp["dn_out_norm"].T
    p["rb"] = np.broadcast_to(inp["router_b"].reshape(1, 16), (128, 16))
    p["rw"] = inp["router_w"].reshape(2, 8, 128, 8).transpose(2, 0, 1, 3).reshape(128, 2 * 8 * 8)
    off, cols, o = {}, [], 0
    for k, v in p.items():
        v = np.ascontiguousarray(v, dtype=np.float32)
        off[k] = (o, v.shape[1])
        o += v.shape[1]
        cols.append(v)
    return np.concatenate(cols, axis=1), off


_SP_OFF = None


def _sp_layout():
    global _SP_OFF
    if _SP_OFF is None:
        fake = {
            "c": np.zeros((8, 1024), np.float32), "ada_b": np.zeros((4, 6144), np.float32),
            "norm_mix": np.zeros((4, 1024), np.float32), "norm_ffn": np.zeros((4, 1024), np.float32),
            "conv_w": np.zeros((4, 4, 3072), np.float32), "dn_a_log": np.zeros((4, 8), np.float32),
            "dn_dt_bias": np.zeros((4, 8), np.float32), "dn_out_norm": np.zeros((4, 128), np.float32),
            "router_b": np.zeros((2, 8), np.float32), "router_w": np.zeros((2, 1024, 8), np.float32),
        }
        a, off = _small_params(fake, 0)
        _SP_OFF = (off, a.shape[1])
    return _SP_OFF


class Prog:
    def __init__(self, nlayers=DEPTH, direct=False, dumps=(), stop=None, layers=None):
        self.nl = nlayers
        self.direct = direct
        self.dumps = set(dumps)
        self.stop = stop
        self.nc = nc = bass.Bass("TRN2", target_bir_lowering=False)
        self.es = contextlib.ExitStack()
        self.S = Sched(nc, self.es)
        self.dump_names = []
        self.nsb = 0
        self.wd = {}
        self.dsems = {}
        self.heads = list(range(NH))
        self.cut = -1
        self.cutn = 0
        self.cb = {}

    def sb(self, name, shape, dt=F32):
        return self.es.enter_context(self.nc.sbuf_tensor(name, list(shape), dt))

    def ps(self, name, shape, dt=F32):
        return self.es.enter_context(self.nc.psum_tensor(name, list(shape), dt))

    def dsem(self, name):
        if name not in self.dsems:
            self.dsems[name] = self.S._newsem("d" + name)
        return self.dsems[name]

    def dram_in(self, name, shape, dt=F32):
        return self.nc.dram_tensor(name, list(shape), dt, kind="ExternalInput").ap()

    def dump(self, name, ap, shape):
        if name not in self.dumps:
            return
        t = self.nc.dram_tensor("dbg_" + name, list(shape), ap.dtype, kind="ExternalOutput").ap()
        self.S.dma("sp", t, ap, self.dsem("dump"))
        self.dump_names.append("dbg_" + name)

    def mm(self, out, lhsT, rhs, start=True, stop=True, inc=None):
        nc = self.nc
        return self.S.op("pe", lambda: nc.tensor.matmul(out, lhsT, rhs, start=start, stop=stop),
                         W=[out], R=[lhsT, rhs], inc=(stop if inc is None else inc))

    def tr(self, out, in_, ident):
        nc = self.nc
        return self.S.op("pe", lambda: nc.tensor.transpose(out, in_, ident), W=[out], R=[in_, ident])

    def act(self, out, in_, func, bias=None, scale=None, accum=None, extraR=()):
        nc = self.nc
        kw = {}
        R = [in_] + list(extraR)
        W = [out]
        if bias is not None:
            kw["bias"] = bias
            if not isinstance(bias, (int, float)):
                R.append(bias)
        if scale is not None:
            kw["scale"] = scale
            if not isinstance(scale, (int, float)):
                R.append(scale)
        if accum is not None:
            kw["accum_out"] = accum
            W.append(accum)
        return self.S.op("act", lambda: nc.scalar.activation(out=out, in_=in_, func=func, **kw), W=W, R=R)

    def tt(self, out, a, b, op, eng="dve"):
        e = self.S.eng[eng]
        return self.S.op(eng, lambda: e.tensor_tensor(out=out, in0=a, in1=b, op=op), W=[out], R=[a, b])

    def ts(self, out, a, s1, op0, s2=None, op1=None, eng="dve"):
        e = self.S.eng[eng]
        R = [a] + [s for s in (s1, s2) if s is not None and not isinstance(s, (int, float))]
        kw = {}
        if op1 is not None:
            kw["op1"] = op1
        return self.S.op(eng, lambda: e.tensor_scalar(out=out, in0=a, scalar1=s1, scalar2=s2, op0=op0, **kw),
                         W=[out], R=R)

    def stt(self, out, a, s, b, op0, op1, eng="dve"):
        e = self.S.eng[eng]
        R = [a, b] + ([s] if not isinstance(s, (int, float)) else [])
        return self.S.op(eng, lambda: e.scalar_tensor_tensor(out=out, in0=a, scalar=s, in1=b, op0=op0, op1=op1),
                         W=[out], R=R)

    def cp(self, out, in_, eng="dve"):
        e = self.S.eng[eng]
        if eng == "act":
            return self.S.op("act", lambda: e.copy(out=out, in_=in_), W=[out], R=[in_])
        return self.S.op(eng, lambda: e.tensor_copy(out=out, in_=in_), W=[out], R=[in_])

    def rsqrt(self, out, in_, mulc, addc):
        self.act(out, in_, AF.Ln, bias=self.cbias(addc), scale=mulc)
        self.act(out, out, AF.Exp, scale=-0.5)

    def cbias(self, v):
        if v not in self.cb:
            i = len(self.cb)
            self.memset(self.CB[:, i:i + 1], float(v))
            self.cb[v] = i
        i = self.cb[v]
        return self.CB[:, i:i + 1]

    def memset(self, ap, v, eng="dve"):
        e = self.S.eng[eng]
        return self.S.op(eng, lambda: e.memset(ap, v), W=[ap])

    def wload(self, dst, src, semname):
        q = "pool" if dst.dtype != src.dtype else "sp"
        return self.S.dma(q, dst, src, self.dsem(semname))

    def wv(self, nm, idx, c0, ncol, nk=8, p=128, blk=None, k0=0, nkb=None):
        if self.direct:
            w = self.wd[(nm, idx)]
            r0 = (0 if blk is None else blk * (nkb or nk) * p) + k0 * p
            return w[r0:r0 + nk * p, c0:c0 + ncol].rearrange("(c p) n -> p c n", p=p)
        assert k0 == 0
        off, kind, cols = self.wpack_off[(nm, idx)]
        NR = self.wpack_nr
        t = self.wpack.tensor
        if kind == "row":
            assert nk == NCORES and blk is None
            return bass.AP(t, off + c0, [[cols, p], [NR, nk], [1, ncol]])
        if kind == "col":
            assert ncol == 128 and c0 % 128 == 0
            return bass.AP(t, (c0 // 128) * NR + off, [[128, p], [p * 128, nk], [1, ncol]])
        return bass.AP(t, blk * NR + off + c0, [[cols, p], [p * cols, nk], [1, ncol]])

    def wrows(self, name, idx, r0, nr, c0, ncol):
        w = self.wd[(name, idx)]
        return w[r0:r0 + nr, c0:c0 + ncol].rearrange("(c p) n -> p c n", p=128)

    def C(self, name, bf=False):
        o, w = (CSTB_OFF if bf else CSTF_OFF)[name]
        return (self.CSTB if bf else self.CST)[:, o:o + w]

    def P(self, name, i0=0, n=None):
        off, _ = _sp_layout()
        o, w = off[name]
        n = w - i0 if n is None else n
        return self.SP[:, o + i0:o + i0 + n]

    def setup(self):
        nc = self.nc
        _, spw = _sp_layout()
        wf, wb = CSTF_NP.shape[1], CSTB_NP.shape[1]
        self.x_in = self.dram_in("x", [S, D])
        self.cstf_in = self.dram_in("cstf", [128, wf])
        self.cstb_in = self.dram_in("cstb", [128, wb])
        self.sp_in = self.dram_in("sp", [128, spw])
        self.fn_in = self.dram_in("final_norm", [1, D])
        self.out = nc.dram_tensor("out", [S, D], F32, kind="ExternalOutput").ap()
        sb = self.sb
        self.CST = sb("CST", [128, wf])
        self.CSTB = sb("CSTB", [128, wb], BF16)
        self.SP = sb("SP", [128, spw])
        self.S.dma("sp", self.CST[:], self.cstf_in[:, :], self.dsem("cst"))
        self.S.dma("sp", self.SP[:], self.sp_in[:, :], self.dsem("sp"))
        self.S.dma("pool", self.CSTB[:], self.cstb_in[:, :], self.dsem("cstb"))
        self.X = [sb(f"X{t}", [128, D]) for t in range(16)]
        for t in range(16):
            self.S.dma("sp", self.X[t][:], self.x_in[t * 128:(t + 1) * 128, :], self.dsem("xin"))
        self.PB = [self.ps(f"PB{i}", [128, 512]) for i in range(6)]
        self.PT = [self.ps(f"PT{i}", [128, 1024], BF16) for i in range(2)]
        self.CACT = sb("CACT", [128, 8])
        self.act(self.CACT[:], self.P("c"), AF.Silu)
        self.CACT16 = sb("CACT16", [128, 8], BF16)
        self.cp(self.CACT16[:], self.CACT[:])
        self.MODT = sb("MODT", [128, 48])
        self.WSC1 = sb("WSC1", [128, 8])
        self.WSC2 = sb("WSC2", [128, 8])
        self.NEGA = sb("NEGA", [128, 8])
        self.F = [sb(f"F{i}", [128, 512]) for i in range(6)]
        self.H = [sb(f"H{i}", [128, 512], BF16) for i in range(15)]
        self.W = [sb("W0", [128, 8, 512], BF16), sb("W1", [128, 8, 256], BF16)]
        self.XC = sb("XC", [128, 515])
        self.XN = self.W[1][:, 0:4, :].rearrange("p c t -> p (c t)")
        self.CB = sb("CB", [128, 8])
        self.SS = sb("SS", [128, 8])
        self.RS = sb("RS", [128, 8])
        self.BIG = sb("BIG", [128, 28672], BF16)
        self.BIG2 = sb("BIG2", [128, 8192], BF16)
        self.SF = sb("SF", [128, 8, 128])
        self.SB16 = sb("SB16", [128, 8, 128], BF16)
        self.HIST = sb("HIST", [128, 24, 3])
        self.WBA = sb("WBA", [128, 8, 16], BF16)
        self.WUS = sb("WUS", [64, 8, 128], BF16)
        self.BA = sb("BA", [128, 64])
        for nm in ("BETA", "LNB", "TMPA", "GS", "G", "GL", "GP", "EGP", "KD", "EGL"):
            setattr(self, nm, sb(nm, [128, 32]))
        self.GTS = sb("GTS", [8, 512])
        self.GPTS = sb("GPTS", [8, 512])
        self.phase = None
        print("sbuf bytes remaining after alloc:", nc.sbuf_bytes_remaining)

    def set_phase(self, ph):
        if self.phase == ph:
            return
        self.phase = ph
        B, B2 = self.BIG, self.BIG2
        if ph == "mixer":
            self.S.set_regions("BIG", [0, 8192, 16384, 20480, 24576])
            self.S.set_regions("BIG2", [0, 4096])
            self.SBK = B[:, 0:8192].rearrange("p (c t) -> p c t", c=4)
            self.SBV = B[:, 8192:16384].rearrange("p (c t) -> p c t", c=16)
            self.OGT = B[:, 16384:20480].rearrange("p (c t) -> p c t", c=8)
            self.MTm = B[:, 20480:24576].rearrange("p (c t) -> p c t", c=8)
            self.OSB = B[0:64, 24576:28672].rearrange("p (c t) -> p c t", c=8)
            self.HT = B2[:, 0:4096].rearrange("p (c t) -> p c t", c=8)
            self.SBQP = B2[:, 4096:8192].rearrange("p (c t) -> p c t", c=8)
        else:
            self.S.set_regions("BIG", [0])
            self.S.set_regions("BIG2", [0])
            self.AT = B[:, :].rearrange("p (c t) -> p c t", c=28)
            self.H2T = B2[:, :].rearrange("p (c t) -> p c t", c=8)

    def layer_mod(self, l):
        AWB = self.W[0]
        for blk in range(12):
            self.wload(AWB[:], self.wv("ada_w", l, blk * 512, 512), "w0")
            for jj in range(4):
                j = blk * 4 + jj
                for dc in range(8):
                    self.mm(self.PB[0][:, j:j + 1], AWB[:, dc, jj * 128:(jj + 1) * 128],
                            self.CACT16[:, dc:dc + 1], start=(dc == 0), stop=(dc == 7))
        self.tt(self.MODT[:], self.PB[0][:, 0:48], self.P("ada_b", l * 48, 48), ALU.add)
        for (dst, nm, j0) in ((self.WSC1, "nmix", 8), (self.WSC2, "nffn", 32)):
            self.stt(dst[:], self.MODT[:, j0:j0 + 8], 1.0, self.P(nm, l * 8, 8), ALU.add, ALU.mult)
        self.act(self.NEGA[:], self.P("alog", l * 8, 8), AF.Exp)
        self.ts(self.NEGA[:], self.NEGA[:], -1.0, ALU.mult)
        self.dump(f"modT{l}", self.MODT[:], [128, 48])

    def norm_to_hT(self, tiles, HT, wsc, shc):
        n = len(tiles)
        for i, t in enumerate(tiles):
            self.act(self.XN[:], self.X[t][:], AF.Square, accum=self.SS[:, i:i + 1])
        self.rsqrt(self.RS[:, 0:n], self.SS[:, 0:n], 1.0 / D, EPS)
        for i, t in enumerate(tiles):
            self.ts(self.XN[:], self.X[t][:], self.RS[:, i:i + 1], ALU.mult)
            pt = self.PT[i % 2]
            for dc in range(8):
                self.tr(pt[:, dc * 128:(dc + 1) * 128], self.XN[:, dc * 128:(dc + 1) * 128], self.C("ident", True))
            for dc in range(8):
                self.act(HT[:, dc, i * 128:(i + 1) * 128], pt[:, dc * 128:(dc + 1) * 128], AF.Identity,
                         bias=shc[:, dc:dc + 1], scale=wsc[:, dc:dc + 1])

    def direct_weights(self, names_layers):
        shapes = {"ada_w": (D, 6 * D), "w_in": (D, INC), "w_up_dn": (D, D), "w_up_sb": (512, D), "w_out": (D, D),
                  "ffn_w1": (D, DFF), "ffn_w3": (D, DFF), "ffn_w2": (DFF, D),
                  "moe_w1": (NE * D, DFF), "moe_w3": (NE * D, DFF), "moe_w2": (NE * DFF, D)}
        for (nm, i) in names_layers:
            self.wd[(nm, i)] = self.dram_in(f"{nm}_{i}", list(shapes[nm]))

    def finish(self):
        S_ = self.S
        for k, sem in S_.dsem_by_key.items():
            v = S_.dcnt.get(id(sem), 0)
            if v:
                S_._wait("sp", (sem, v, "dma"))
        self.es.close()

    def proj_fm(self, l, col0, ncols, Wt, wofs, out_ps, semname):
        self.wload(Wt[:, :, wofs:wofs + ncols], self.wrows("w_in", l, 0, D, col0, ncols), semname)
        for dc in range(8):
            self.mm(out_ps, Wt[:, dc, wofs:wofs + ncols], self.HT[:, dc, :], start=(dc == 0), stop=(dc == 7))

    def sb_project(self, l, qt):
        W0, W1 = self.W
        t0 = qt * 512
        self.wload(W0[:], self.wv("w_in", l, C_SQ, 512), "w0")
        for cc in range(4):
            pb = self.PB[cc % 2]
            for dc in range(8):
                self.mm(pb[:, :], W0[:, dc, cc * 128:(cc + 1) * 128], self.HT[:, dc, :], start=(dc == 0), stop=(dc == 7))
            self.ts(self.SBQP[:, 2 * cc, :], pb[:, :], self.C("hm0"), ALU.mult)
            self.ts(self.SBQP[:, 2 * cc + 1, :], pb[:, :], self.C("hm1"), ALU.mult)
        self.wload(W0[:], self.wv("w_in", l, C_SK, 512), "w0")
        for cc in range(4):
            pb = self.PB[cc % 2]
            for dc in range(8):
                self.mm(pb[:, :], W0[:, dc, cc * 128:(cc + 1) * 128], self.HT[:, dc, :], start=(dc == 0), stop=(dc == 7))
            self.cp(self.SBK[:, cc, t0:t0 + 512], pb[:, :], eng="act")
        self.wload(W0[:], self.wv("w_in", l, C_SV, 512), "w0")
        for tt_ in range(4):
            pb = self.PB[tt_ % 2]
            for dc in range(8):
                self.mm(pb[:, :], self.HT[:, dc, tt_ * 128:(tt_ + 1) * 128], W0[:, dc, :], start=(dc == 0), stop=(dc == 7))
            self.cp(self.SBV[:, qt * 4 + tt_, :], pb[:, :])

    def sb_attend(self, qt):
        F, H, PB = self.F, self.H, self.PB
        R, EX, SP32, T1 = F[0], F[1], F[2], F[3]
        SPB, AT = H[0], H[1]
        utri = self.C("utri", True)
        for h in self.heads:
            cc, p0 = h // 2, 64 * (h % 2)
            self.memset(R[:], 0.0)
            nkt = 4 * qt + 4
            for idx, jb in enumerate(range(nkt - 1, -1, -1)):
                r = jb - 4 * qt
                self.mm(PB[0][:, :], self.SBK[:, cc, jb * 128:(jb + 1) * 128], self.SBQP[:, h, :])
                self.cutn += 1
                if self.cutn == self.cut: return
                self.act(EX[:], PB[0][:, :], AF.Exp)
                self.cutn += 1
                if self.cutn == self.cut: return
                if r >= 0:
                    self.act(SP32[:], EX[:], AF.Ln, bias=self.cbias(1.0))
                    self.cutn += 1
                    if self.cutn == self.cut: return
                    if r > 0:
                        self.memset(SPB[:, 0:r * 128], 0.0)
                    self.tt(SPB[:, r * 128:(r + 1) * 128], SP32[:, r * 128:(r + 1) * 128], utri, ALU.mult)
                    if r < 3:
                        self.cp(SPB[:, (r + 1) * 128:512], SP32[:, (r + 1) * 128:512])
                else:
                    self.act(SP32[:], EX[:], AF.Ln, bias=self.cbias(1.0))
                    self.cp(SPB[:], SP32[:])
                    self.cutn += 1
                    if self.cutn == self.cut: return
                self.mm(PB[1][:, :], self.C("triincl", True), SPB[:])
                self.cutn += 1
                if self.cutn == self.cut: return
                self.mm(PB[2][:, :], self.C("ones", True), SPB[:])
                self.cutn += 1
                if self.cutn == self.cut: return
                self.tt(T1[:], PB[0][:, :], R[:], ALU.subtract)
                self.cutn += 1
                if self.cutn == self.cut: return
                self.tt(T1[:], T1[:], PB[1][:, :], ALU.subtract)
                self.cutn += 1
                if self.cutn == self.cut: return
                self.tt(R[:], R[:], PB[2][:, :], ALU.add)
                self.cutn += 1
                if self.cutn == self.cut: return
                self.act(AT[:], T1[:], AF.Exp)
                self.cutn += 1
                if self.cutn == self.cut: return
                if r >= 0:
                    if r > 0:
                        self.memset(AT[:, 0:r * 128], 0.0)
                    self.tt(AT[:, r * 128:(r + 1) * 128], AT[:, r * 128:(r + 1) * 128], utri, ALU.mult)
                self.mm(PB[3][0:64, :], self.SBV[:, jb, h * 64:(h + 1) * 64], AT[:], start=(idx == 0), stop=(jb == 0), inc=True)
                self.cutn += 1
                if self.cutn == self.cut: return
            self.cp(self.OSB[:, h, :], PB[3][0:64, :])

    def dn_prep(self, l, qt):
        PB = self.PB
        self.wload(self.WBA[:], self.wv("w_in", l, C_B, 16), "wba")
        for tt_ in range(4):
            for dc in range(8):
                self.mm(PB[0][:, tt_ * 16:(tt_ + 1) * 16], self.HT[:, dc, tt_ * 128:(tt_ + 1) * 128], self.WBA[:, dc, :],
                        start=(dc == 0), stop=(dc == 7))
        self.cp(self.BA[:], PB[0][:, 0:64])
        for tt_ in range(4):
            b = self.BA[:, tt_ * 16:tt_ * 16 + 8]
            a = self.BA[:, tt_ * 16 + 8:tt_ * 16 + 16]
            s8 = slice(tt_ * 8, (tt_ + 1) * 8)
            self.act(self.BETA[:, s8], b, AF.Sigmoid)
            self.tt(self.TMPA[:, s8], a, self.P("dtb", l * 8, 8), ALU.add)
        self.act(self.LNB[:], self.BETA[:], AF.Ln)
        self.act(self.TMPA[:], self.TMPA[:], AF.Exp)
        self.act(self.TMPA[:], self.TMPA[:], AF.Ln, bias=self.cbias(1.0))
        for tt_ in range(4):
            s8 = slice(tt_ * 8, (tt_ + 1) * 8)
            self.tt(self.GS[:, s8], self.TMPA[:, s8], self.NEGA[:], ALU.mult)
        self.mm(PB[1][:, 0:32], self.C("tri"), self.GS[:])
        self.mm(PB[1][:, 32:64], self.C("ones"), self.GS[:])
        self.cp(self.G[:], PB[1][:, 0:32])
        self.cp(self.GL[:], PB[1][:, 32:64])
        self.tt(self.GP[:], self.G[:], self.LNB[:], ALU.add)
        self.act(self.EGP[:], self.GP[:], AF.Exp)
        self.tt(self.KD[:], self.GL[:], self.G[:], ALU.subtract)
        self.act(self.KD[:], self.KD[:], AF.Exp)
        self.act(self.EGL[:], self.GL[:], AF.Exp)
        for tt_ in range(4):
            s8 = slice(tt_ * 8, (tt_ + 1) * 8)
            self.tr(PB[2][0:8, tt_ * 128:(tt_ + 1) * 128], self.G[:, s8], self.C("ident"))
            self.tr(PB[3][0:8, tt_ * 128:(tt_ + 1) * 128], self.GP[:, s8], self.C("ident"))
        self.cp(self.GTS[:], PB[2][0:8, :])
        self.cp(self.GPTS[:], PB[3][0:8, :])

    def dn_qkv(self, l, qt, h, which, Wt, wofs):
        ch = {"q": 0, "k": 8, "v": 16}[which] + h
        pb = self.PB[4]
        for dc in range(8):
            self.mm(pb[:, :], Wt[:, dc, wofs:wofs + 128], self.HT[:, dc, :], start=(dc == 0), stop=(dc == 7))
        XC, Y = self.XC, self.F[1]
        if qt == 0:
            self.memset(XC[:, 0:3], 0.0)
        else:
            self.cp(XC[:, 0:3], self.HIST[:, ch, :])
        self.cp(XC[:, 3:515], pb[:, :], eng="act")
        if qt < 3:
            self.cp(self.HIST[:, ch, :], XC[:, 512:515])
        cw = lambda k: self.P("conv", l * 96 + k * 24 + ch, 1)
        self.ts(Y[:], XC[:, 0:512], cw(0), ALU.mult)
        for k in range(1, 4):
            self.stt(Y[:], XC[:, k:k + 512], cw(k), Y[:], ALU.mult, ALU.add)
        self.act(Y[:], Y[:], AF.Silu)
        return Y

    def l2n(self, out_bf, Y, mulc, addc):
        SQ = self.F[2]
        self.tt(SQ[:], Y[:], Y[:], ALU.mult)
        self.mm(self.PB[5][:, :], self.C("ones"), SQ[:])
        self.rsqrt(SQ[:], self.PB[5][:, :], mulc, addc)
        self.tt(out_bf, Y[:], SQ[:], ALU.mult)

    def dn_head(self, l, qt, h):
        F, H, PB = self.F, self.H, self.PB
        W0 = self.W[0]
        GBC, GPBC, A, EGB = F[0], F[3], F[4], F[5]
        QN, KN, VC, ZS, L, LT, QKT, RK, KDEC, RV, M, MT, QM, NWT, QD = [H[i] for i in range(15)]
        Pm, VN = L, VC
        identb = self.C("ident", True)
        for i, c0 in enumerate((C_Q, C_K, C_V, C_Z)):
            self.wload(W0[:, :, i * 128:(i + 1) * 128], self.wv("w_in", l, c0 + h * 128, 128), "w0")
        Y = self.dn_qkv(l, qt, h, "q", W0, 0)
        self.l2n(QN[:], Y, 128.0, 128.0 * EPS)
        Y = self.dn_qkv(l, qt, h, "k", W0, 128)
        self.l2n(KN[:], Y, 1.0, EPS)
        Y = self.dn_qkv(l, qt, h, "v", W0, 256)
        self.cp(VC[:], Y[:])
        for dc in range(8):
            self.mm(PB[4][:, :], W0[:, dc, 384:512], self.HT[:, dc, :], start=(dc == 0), stop=(dc == 7))
        self.act(F[1][:], PB[4][:, :], AF.Silu)
        self.cp(ZS[:], F[1][:])
        self.mm(PB[4][:, :], self.C("sel")[0:8, h * 128:(h + 1) * 128], self.GTS[:])
        self.cp(GBC[:], PB[4][:, :])
        self.mm(PB[5][:, :], self.C("sel")[0:8, h * 128:(h + 1) * 128], self.GPTS[:])
        self.cp(GPBC[:], PB[5][:, :])
        cs = [slice(c * 128, (c + 1) * 128) for c in range(4)]
        sc = [slice(c * 8 + h, c * 8 + h + 1) for c in range(4)]
        for c in range(4):
            self.mm(PB[0][:, cs[c]], KN[:, cs[c]], KN[:, cs[c]])
            self.mm(PB[1][:, cs[c]], KN[:, cs[c]], QN[:, cs[c]])
        for c in range(4):
            self.stt(A[:, cs[c]], GBC[:, cs[c]], self.GP[:, sc[c]], self.C("neg1"), ALU.subtract, ALU.max)
        self.act(A[:], A[:], AF.Exp, scale=-1.0)
        self.tt(L[:], PB[0][:, :], A[:], ALU.mult)
        for c in range(4):
            self.stt(A[:, cs[c]], GPBC[:, cs[c]], self.G[:, sc[c]], self.C("mask3"), ALU.subtract, ALU.min)
        self.act(A[:], A[:], AF.Exp)
        self.tt(LT[:], PB[0][:, :], A[:], ALU.mult)
        for c in range(4):
            self.stt(A[:, cs[c]], GBC[:, cs[c]], self.G[:, sc[c]], self.C("mask2"), ALU.subtract, ALU.min)
        self.act(A[:], A[:], AF.Exp)
        self.tt(QKT[:], PB[1][:, :], A[:], ALU.mult)
        for c in range(4):
            self.tr(self.PT[0][:, cs[c]], KN[:, cs[c]], identb)
            self.tr(self.PT[1][:, cs[c]], VC[:, cs[c]], identb)
        for c in range(4):
            self.ts(RK[:, cs[c]], self.PT[0][:, cs[c]], self.EGP[:, sc[c]], ALU.mult)
            self.ts(KDEC[:, cs[c]], self.PT[0][:, cs[c]], self.KD[:, sc[c]], ALU.mult)
            self.ts(RV[:, cs[c]], self.PT[1][:, cs[c]], self.BETA[:, sc[c]], ALU.mult)
        for c in range(4):
            self.tt(QM[:, cs[c]], L[:, cs[c]], self.C("lm0", True), ALU.mult)
            self.tt(M[:, cs[c]], identb, QM[:, cs[c]], ALU.subtract)
        for c in range(4):
            self.tt(QM[:, cs[c]], LT[:, cs[c]], self.C("lm0T", True), ALU.mult)
            self.tt(MT[:, cs[c]], identb, QM[:, cs[c]], ALU.subtract)
        for lv in range(1, 7):
            for c in range(4):
                self.mm(PB[2][:, cs[c]], LT[:, cs[c]], M[:, cs[c]])
            self.cp(Pm[:], PB[2][:, :], eng="act")
            for c in range(4):
                self.mm(PB[3][:, cs[c]], MT[:, cs[c]], Pm[:, cs[c]])
            for c in range(4):
                self.tt(QM[:, cs[c]], PB[3][:, cs[c]], self.C(f"lm{lv}", True), ALU.mult)
            self.tt(M[:], M[:], QM[:], ALU.subtract)
            for c in range(4):
                self.tr(self.PT[0][:, cs[c]], QM[:, cs[c]], identb)
            self.tt(MT[:], MT[:], self.PT[0][:, 0:512], ALU.subtract)
        for c in range(4):
            self.mm(PB[2][:, cs[c]], RK[:, cs[c]], MT[:, cs[c]])
        self.ts(NWT[:], PB[2][:, :], -1.0, ALU.mult)
        self.act(EGB[:], GBC[:], AF.Exp)
        self.tt(QD[:], QN[:], EGB[:], ALU.mult)
        if qt == 0:
            self.memset(self.SF[:, h, :], 0.0)
            self.memset(self.SB16[:, h, :], 0.0)
        S16 = self.SB16[:, h, :]
        for c in range(4):
            pv = PB[4][:, 0:128]
            self.mm(pv, MT[:, cs[c]], RV[:, cs[c]], start=True, stop=False)
            self.mm(pv, NWT[:, cs[c]], S16, start=False, stop=True)
            self.cp(VN[:, cs[c]], pv)
            self.mm(PB[5][:, cs[c]], S16, QD[:, cs[c]], start=True, stop=False)
            self.mm(PB[5][:, cs[c]], VN[:, cs[c]], QKT[:, cs[c]], start=False, stop=True)
            ps = PB[4][:, 128:256]
            self.mm(ps, KDEC[:, cs[c]], VN[:, cs[c]])
            self.stt(self.SF[:, h, :], self.SF[:, h, :], self.EGL[:, sc[c]], ps, ALU.mult, ALU.add)
            self.cp(S16, self.SF[:, h, :])
        OT = F[1]
        self.cp(OT[:], PB[5][:, :], eng="act")
        self.dump(f"OT{qt}_{h}", OT[:], [128, 512])
        SQ = F[2]
        self.tt(SQ[:], OT[:], OT[:], ALU.mult)
        self.mm(PB[4][:, :], self.C("ones"), SQ[:])
        self.rsqrt(SQ[:], PB[4][:, :], 1.0 / 128.0, EPS)
        self.tt(OT[:], OT[:], SQ[:], ALU.mult)
        self.stt(self.OGT[:, h, :], OT[:], self.P("onw", l, 1), ZS[:], ALU.mult, ALU.mult)

    def merge_out(self, l, qt):
        F, PB = self.F, self.PB
        W0, W1 = self.W
        for oc in range(8):
            self.wload(W1[:, :, 0:128], self.wv("w_in", l, C_G + oc * 128, 128), "w1")
            self.wload(W1[:, :, 128:256], self.wv("w_in", l, C_G + D + oc * 128, 128), "w1")
            for dc in range(8):
                self.mm(PB[0][:, :], W1[:, dc, 0:128], self.HT[:, dc, :], start=(dc == 0), stop=(dc == 7))
            for dc in range(8):
                self.mm(PB[1][:, :], W1[:, dc, 128:256], self.HT[:, dc, :], start=(dc == 0), stop=(dc == 7))
            self.act(F[0][:], PB[0][:, :], AF.Sigmoid)
            self.act(F[1][:], PB[1][:, :], AF.Sigmoid)
            self.wload(W0[:, :, 0:128], self.wv("w_up_dn", l, oc * 128, 128), "w0")
            for h in range(8):
                self.mm(PB[2][:, :], W0[:, h, 0:128], self.OGT[:, h, :], start=(h == 0), stop=(h == 7))
            self.wload(self.WUS[:], self.wv("w_up_sb", l, oc * 128, 128, nk=8, p=64), "wus")
            for h in range(8):
                self.mm(PB[3][:, :], self.WUS[:, h, :], self.OSB[:, h, :], start=(h == 0), stop=(h == 7))
            self.tt(F[2][:], PB[2][:, :], F[0][:], ALU.mult)
            self.tt(F[3][:], PB[3][:, :], F[1][:], ALU.mult)
            self.tt(self.MTm[:, oc, :], F[2][:], F[3][:], ALU.add)
        self.proj_residual(("w_out", l), self.MTm, 8, [4 * qt + i for i in range(4)], 16)

    def proj_residual(self, wkey, actT, nk, tiles, modj, gate_ps=None, blk=None):
        F, PB, H = self.F, self.PB, self.H
        W0f = self.W[0][:, :, :].rearrange("p c t -> p (c t)")
        slotA = W0f[:, 0:nk * 128].rearrange("p (c t) -> p c t", c=nk)
        if 2 * nk * 128 <= 4096:
            slotB = W0f[:, 2048:2048 + nk * 128].rearrange("p (c t) -> p c t", c=nk)
            slots = [([slotA[:, k, :] for k in range(nk)], [(slotA, 0, nk)]),
                     ([slotB[:, k, :] for k in range(nk)], [(slotB, 0, nk)])]
        else:
            nh = (nk + 3) // 4
            bk = [H[k // 4][:, (k % 4) * 128:(k % 4 + 1) * 128] for k in range(nk)]
            bl = [(H[i][:, 0:min(4, nk - 4 * i) * 128].rearrange("p (c t) -> p c t", c=min(4, nk - 4 * i)), 4 * i,
                   min(4, nk - 4 * i)) for i in range(nh)]
            slots = [([slotA[:, k, :] for k in range(nk)], [(slotA, 0, nk)]), (bk, bl)]
        ng = len(tiles) // 4
        it = 0
        for oc in range(8):
            kaps, loads = slots[oc % 2]
            for (dst, k0, n) in loads:
                self.wload(dst, self.wv(wkey[0], wkey[1], oc * 128, 128, nk=n, blk=blk, k0=k0, nkb=nk), "w0")
            for g in range(ng):
                pa, pt, Fs = (PB[0], PB[1], F[0]) if it % 2 == 0 else (PB[2], PB[3], F[3])
                it += 1
                for k in range(nk):
                    self.mm(pa[:, :], kaps[k], actT[:, k, g * 512:(g + 1) * 512], start=(k == 0), stop=(k == nk - 1))
                self.ts(Fs[:], pa[:, :], self.MODT[:, modj + oc:modj + oc + 1], ALU.mult)
                if gate_ps is not None:
                    self.tt(Fs[:], Fs[:], gate_ps[g], ALU.mult)
                for i in range(4):
                    self.tr(pt[:, i * 128:(i + 1) * 128], Fs[:, i * 128:(i + 1) * 128], self.C("ident"))
                for i in range(4):
                    xs = self.X[tiles[g * 4 + i]][:, oc * 128:(oc + 1) * 128]
                    self.tt(xs, xs, pt[:, i * 128:(i + 1) * 128], ALU.add)

    def mixer(self, l):
        self.set_phase("mixer")
        for qt in range(4):
            self.norm_to_hT([4 * qt + i for i in range(4)], self.HT, self.WSC1, self.MODT[:, 0:8])
            self.sb_project(l, qt)
            self.sb_attend(qt)
            self.dn_prep(l, qt)
            for h in range(NH):
                self.dn_head(l, qt, h)
            self.merge_out(l, qt)

    def swiglu_up(self, w1key, w3key, blk=None):
        F, PB = self.F, self.PB
        SFb = self.SF[:, :, :].rearrange("p c t -> p (c t)").bitcast(BF16).rearrange("p (c t) -> p c t", c=8)
        bufs = [self.W[1], SFb]
        for fc in range(DFF // 128):
            Wb = bufs[fc % 2]
            for i, wk in enumerate((w1key, w3key)):
                self.wload(Wb[:, :, i * 128:(i + 1) * 128], self.wv(wk[0], wk[1], fc * 128, 128, blk=blk), "w1")
            for tg in range(2):
                ts_ = slice(tg * 512, (tg + 1) * 512)
                pa, pb_, Fs = (PB[2], PB[3], F[1]) if tg == 0 else (PB[4], PB[5], F[2])
                for dc in range(8):
                    self.mm(pa[:, :], Wb[:, dc, 0:128], self.H2T[:, dc, ts_], start=(dc == 0), stop=(dc == 7))
                for dc in range(8):
                    self.mm(pb_[:, :], Wb[:, dc, 128:256], self.H2T[:, dc, ts_], start=(dc == 0), stop=(dc == 7))
                self.act(Fs[:], pa[:, :], AF.Silu)
                self.tt(self.AT[:, fc, ts_], Fs[:], pb_[:, :], ALU.mult)

    def ffn_dense(self, l):
        self.set_phase("ffn")
        j = l // 2
        for hf in range(2):
            tiles = [8 * hf + i for i in range(8)]
            self.norm_to_hT(tiles, self.H2T, self.WSC2, self.MODT[:, 24:32])
            self.swiglu_up(("ffn_w1", j), ("ffn_w3", j))
            self.proj_residual(("ffn_w2", j), self.AT, DFF // 128, tiles, 40)

    def moe_alloc(self):
        sb = self.sb
        for nm in ("LG", "MX", "EE", "MK", "GATE"):
            setattr(self, nm, sb(nm, [128, 64]))
        self.DEN = sb("DEN", [128, 8])
        self.RW16 = sb("RW16", [128, 64], BF16)

    def ffn_moe(self, l):
        self.set_phase("ffn")
        j = l // 2
        F, PB = self.F, self.PB
        self.cp(self.RW16[:], self.P("rw", j * 64, 64))
        for hf in range(2):
            tiles = [8 * hf + i for i in range(8)]
            self.norm_to_hT(tiles, self.H2T, self.WSC2, self.MODT[:, 24:32])
            for i in range(8):
                for dc in range(8):
                    self.mm(PB[0][:, i * 8:(i + 1) * 8], self.H2T[:, dc, i * 128:(i + 1) * 128],
                            self.RW16[:, dc * 8:(dc + 1) * 8], start=(dc == 0), stop=(dc == 7))
            for i in range(8):
                s8 = slice(i * 8, (i + 1) * 8)
                self.tt(self.LG[:, s8], PB[0][:, s8], self.P("rb", j * 8, 8), ALU.add)
            for i in range(8):
                s8 = slice(i * 8, (i + 1) * 8)
                nc = self.nc
                self.S.op("dve", lambda o=self.MX[:, s8], a=self.LG[:, s8]: nc.vector.max(out=o, in_=a),
                          W=[self.MX[:, s8]], R=[self.LG[:, s8]])
            for i in range(8):
                s8 = slice(i * 8, (i + 1) * 8)
                self.ts(self.EE[:, s8], self.LG[:, s8], self.MX[:, i * 8:i * 8 + 1], ALU.subtract)
                self.ts(self.MK[:, s8], self.LG[:, s8], self.MX[:, i * 8 + 1:i * 8 + 2], ALU.is_ge)
                self.tt(self.DEN[:, i:i + 1], self.MX[:, i * 8 + 1:i * 8 + 2], self.MX[:, i * 8:i * 8 + 1], ALU.subtract)
            self.act(self.EE[:], self.EE[:], AF.Exp)
            self.tt(self.EE[:], self.EE[:], self.MK[:], ALU.mult)
            self.act(self.DEN[:], self.DEN[:], AF.Exp)
            self.ts(self.DEN[:], self.DEN[:], 1.0, ALU.add)
            nc = self.nc
            self.S.op("dve", lambda: nc.vector.reciprocal(out=self.DEN[:], in_=self.DEN[:]), W=[self.DEN[:]], R=[self.DEN[:]])
            for i in range(8):
                s8 = slice(i * 8, (i + 1) * 8)
                self.ts(self.GATE[:, s8], self.EE[:, s8], self.DEN[:, i:i + 1], ALU.mult)
            for i in range(8):
                pb = PB[1] if i < 4 else PB[2]
                self.tr(pb[0:8, (i % 4) * 128:(i % 4 + 1) * 128], self.GATE[:, i * 8:(i + 1) * 8], self.C("ident"))
            self.cp(self.GTS[:], PB[1][0:8, :])
            self.cp(self.GPTS[:], PB[2][0:8, :])
            self.dump(f"gate{l}_{hf}", self.GATE[:], [128, 64])
            for e in range(NE):
                self.swiglu_up(("moe_w1", j), ("moe_w3", j), blk=e)
                sel = self.C("sel")[0:8, e * 128:(e + 1) * 128]
                self.mm(PB[4][:, :], sel, self.GTS[:])
                self.mm(PB[5][:, :], sel, self.GPTS[:])
                self.proj_residual(("moe_w2", j), self.AT, DFF // 128, tiles, 40,
                                   gate_ps=[PB[4][:, :], PB[5][:, :]], blk=e)

    def final(self):
        F = self.F
        self.S.dma("sp", F[0][:], self.fn_in[0:1, 0:512].partition_broadcast(128))
        self.S.dma("sp", F[1][:], self.fn_in[0:1, 512:1024].partition_broadcast(128))
        for hf in range(2):
            tiles = [8 * hf + i for i in range(8)]
            for i, t in enumerate(tiles):
                self.act(self.XN[:], self.X[t][:], AF.Square, accum=self.SS[:, i:i + 1])
            self.rsqrt(self.RS[:, 0:8], self.SS[:, 0:8], 1.0 / D, EPS)
            for i, t in enumerate(tiles):
                for c in range(2):
                    xs = self.X[t][:, c * 512:(c + 1) * 512]
                    self.stt(xs, xs, self.RS[:, i:i + 1], F[c][:], ALU.mult, ALU.mult)
                self.S.dma("sp", self.out[t * 128:(t + 1) * 128, :], self.X[t][:])


W_SHAPES = {"ada_w": (D, 6 * D), "w_in": (D, INC), "w_up_dn": (D, D), "w_up_sb": (512, D), "w_out": (D, D),
            "ffn_w1": (D, DFF), "ffn_w3": (D, DFF), "ffn_w2": (DFF, D),
            "moe_w1": (NE * D, DFF), "moe_w3": (NE * D, DFF), "moe_w2": (NE * DFF, D)}


def weight_list(nlayers=DEPTH):
    out = []
    for l in range(nlayers):
        out += [("ada_w", l), ("w_in", l), ("w_up_dn", l), ("w_up_sb", l), ("w_out", l)]
        j = l // 2
        if l % 2 == 0:
            out += [("ffn_w1", j), ("ffn_w3", j), ("ffn_w2", j)]
        else:
            out += [("moe_w1", j), ("moe_w3", j), ("moe_w2", j)]
    return out


W_KIND = {"ada_w": "row", "w_in": "row", "w_up_dn": "row", "w_up_sb": "row", "w_out": "row",
          "ffn_w1": "row", "ffn_w3": "row", "ffn_w2": "col", "moe_w1": "exp", "moe_w3": "exp", "moe_w2": "exp"}
PACK_C = 2048


def pack_layout(wl):
    off, o = {}, 0
    for (nm, i) in wl:
        rows, cols = W_SHAPES[nm]
        off[(nm, i)] = (o, W_KIND[nm], cols)
        o += rows * cols // NCORES
    nr = (o + PACK_C * 128 - 1) // (PACK_C * 128) * (PACK_C * 128)
    return off, nr


def pack_rank(inputs, wl, r):
    off, nr = pack_layout(wl)
    buf = np.zeros((nr,), np.float32)
    for (nm, i) in wl:
        a = np.asarray(inputs[nm][i], dtype=np.float32)
        o, kind, cols = off[(nm, i)]
        if kind == "row":
            a2 = a.reshape(-1, a.shape[-1])
            rs = a2.shape[0] // NCORES
            seg = a2[r * rs:(r + 1) * rs]
        elif kind == "col":
            seg = a[:, r * 128:(r + 1) * 128]
        else:
            seg = a[r]
        buf[o:o + seg.size] = np.ascontiguousarray(seg).reshape(-1)
    return buf.reshape(-1, PACK_C)


def gather_weights(p, wl):
    nc = p.nc
    off, nr = pack_layout(wl)
    R = nr // PACK_C
    shard = p.dram_in("wpack", [R, PACK_C])
    src = nc.dram_tensor("wpack_c", [R, PACK_C], F32, kind="Internal").ap()
    dst = nc.dram_tensor("wpack_g", [NCORES * R, PACK_C], F32, kind="Internal").ap()
    p.S.dma("pool", src[:, :], shard[:, :])
    p.S.collective(lambda: nc.gpsimd.collective_compute(
        "AllGather", ALU.bypass, replica_groups=[list(range(NCORES))], ins=[src[:, :]], outs=[dst[:, :]]),
        dst[:, :], src[:, :])
    p.S.wait_all("pool", [dst[:, :]])
    p.wpack, p.wpack_off, p.wpack_nr = dst, off, nr


REPLICATE = True


def build_full(nlayers=DEPTH):
    p = Prog(direct=REPLICATE)
    p.setup()
    p.moe_alloc()
    wl = weight_list(nlayers)
    if REPLICATE:
        p.direct_weights(wl)
    else:
        gather_weights(p, wl)
    for l in range(nlayers):
        p.layer_mod(l)
        p.mixer(l)
        if l % 2 == 0:
            p.ffn_dense(l)
        else:
            p.ffn_moe(l)
    p.final()
    p.finish()
    return p


def make_in_maps(inputs, nlayers=DEPTH):
    wl = weight_list(nlayers)
    maps = []
    fn = np.ascontiguousarray(np.asarray(inputs["final_norm"], dtype=np.float32).reshape(1, D))
    shared = {}
    if REPLICATE:
        for (nm, i) in wl:
            a = np.asarray(inputs[nm][i], dtype=np.float32)
            shared[f"{nm}_{i}"] = a.reshape(-1, a.shape[-1])
    for b in range(NCORES):
        sp, _ = _small_params(inputs, b)
        m = {"x": np.ascontiguousarray(inputs["x"][b], dtype=np.float32), "cstf": CSTF_NP, "cstb": CSTB_NP,
             "sp": sp, "final_norm": fn}
        if REPLICATE:
            m.update(shared)
        else:
            m["wpack"] = pack_rank(inputs, wl, b)
        maps.append(m)
    return maps


def kernel(**inputs):
    inputs = {k: np.asarray(v) for k, v in inputs.items()}
    p = build_full()
    in_maps = make_in_maps(inputs)
    res = run_bass_kernel_spmd(p.nc, in_maps, core_ids=list(range(NCORES)))
    out = np.stack([np.asarray(r["out"], dtype=np.float32) for r in res.results], axis=0)
    return out.reshape(NCORES, S, D)
```

```python
import bisect
import contextlib
import numpy as np
import concourse.bass as bass
import concourse.mybir as mybir
from concourse.bass_utils import run_bass_kernel_spmd

F32 = mybir.dt.float32
BF16 = mybir.dt.bfloat16
AF = mybir.ActivationFunctionType
ALU = mybir.AluOpType

D = 1024
S = 2048
DEPTH = 4
NH = 8
DFF = 3584
NE = 8
INC = 7696
EPS = 1e-6
BIG = 30000.0
NCORES = 8
RELAX = False
C_Q, C_K, C_V, C_Z, C_B, C_A = 0, 1024, 2048, 3072, 4096, 4104
C_SQ, C_SK, C_SV, C_G = 4112, 4624, 5136, 5648


class Sched:
    ROT = 20000

    def __init__(self, nc, es):
        self.nc, self.es = nc, es
        self.eng = {"pe": nc.tensor, "act": nc.scalar, "dve": nc.vector, "pool": nc.gpsimd, "sp": nc.sync}
        self.sem, self.cnt, self.nsem = {}, {}, 0
        self.seen = {e: {} for e in self.eng}
        self.last_w = {}
        self.readers = {}
        self.pending = {e: [] for e in self.eng}
        self.semobj = {}
        for e in ("pe", "act", "dve", "pool"):
            self._rot(e)
        self.ninst = 0
        self.dcnt = {}
        self.regions = {}
        self.relax = RELAX
        self.dsem_by_key = {}
        self.carry = {}

    def _newsem(self, tag):
        self.nsem += 1
        s = self.es.enter_context(self.nc.semaphore(f"{tag}{self.nsem}"))
        self.semobj[id(s)] = s
        return s

    def _rot(self, e):
        self.sem[e] = self._newsem("s" + e)
        self.cnt[e] = 0

    @staticmethod
    def _dsize(dt):
        return 4 if dt == F32 else 2

    def keys(self, a):
        if isinstance(a, str):
            return [a]
        nm = a.tensor.name
        st = self.regions.get(nm)
        if st is None:
            return [nm]
        sz = self._dsize(a.dtype)
        lo = int(a.offset) * sz
        span = 0
        for (stride, count) in list(a.ap)[1:]:
            span += (int(count) - 1) * abs(int(stride))
        hi = lo + (span + 1) * sz
        i0 = bisect.bisect_right(st, lo) - 1
        out = []
        i = i0
        while i < len(st) and st[i] < hi:
            out.append(f"{nm}#{i}")
            i += 1
        return out

    def key(self, a):
        return self.keys(a)[0]

    def set_regions(self, nm, starts):
        old = [k for k in list(self.last_w.keys()) + list(self.readers.keys()) if k == nm or k.startswith(nm + "#")]
        tags = []
        for k in set(old):
            lw = self.last_w.pop(k, None)
            if lw is not None:
                tags.append(lw)
            for (sid, (val, eng_r)) in self.readers.pop(k, {}).items():
                tags.append((self.semobj[sid], val, eng_r))
        self.regions[nm] = list(starts)
        self.carry[nm] = tags

    def _wait(self, e, tag):
        sem, val, _ = tag
        d = self.seen[e]
        if d.get(id(sem), -1) >= val:
            return
        self.eng[e].wait_ge(sem, val)
        d[id(sem)] = val
        self.ninst += 1

    def op(self, e, fn, W=(), R=(), inc=True, dma_sem=None):
        wk = [k for a in W for k in self.keys(a)]
        rk = [k for a in R for k in self.keys(a)]
        for k in wk + rk:
            if "#" in k:
                for tag in self.carry.get(k.split("#")[0], ()):
                    self._wait(e, tag)
        for k in rk:
            lw = self.last_w.get(k)
            if lw is not None:
                self._wait(e, lw)
        for k in wk:
            lw = self.last_w.get(k)
            if lw is not None and not (lw[2] == e and (e == "pe" or self.relax)):
                self._wait(e, lw)
            for (sid, (val, eng_r)) in list(self.readers.get(k, {}).items()):
                if not (self.relax and eng_r == e):
                    self._wait(e, (self.semobj[sid], val, eng_r))
        ins = fn()
        self.ninst += 1
        if dma_sem is not None:
            sem, incv = dma_sem if len(dma_sem) == 2 else (dma_sem[0], 16)
            self.dcnt[id(sem)] = self.dcnt.get(id(sem), 0) + incv
            ins.then_inc(sem, incv)
            tag = (sem, self.dcnt[id(sem)], "dma")
        elif inc:
            self.cnt[e] += 1
            ins.then_inc(self.sem[e], 1)
            tag = (self.sem[e], self.cnt[e], e)
        else:
            self.pending[e].append((wk, rk))
            return ins
        items = self.pending[e] + [(wk, rk)]
        self.pending[e] = []
        for (wk_, rk_) in items:
            for k in rk_:
                self.readers.setdefault(k, {})[id(tag[0])] = (tag[1], tag[2])
            for k in wk_:
                self.last_w[k] = tag
                self.readers[k] = {}
        if dma_sem is None and self.cnt[e] >= self.ROT:
            self._rot(e)
        return ins

    def dma(self, q, out, in_, sem=None):
        eng = self.eng[q]
        k = self.key(out)
        if k not in self.dsem_by_key:
            self.dsem_by_key[k] = self._newsem("d")
        sem = self.dsem_by_key[k]
        return self.op(q, lambda: eng.dma_start(out=out, in_=in_), W=[out], R=[in_], dma_sem=(sem,))

    def collective(self, fn, out, in_):
        k = self.key(out)
        if k not in self.dsem_by_key:
            self.dsem_by_key[k] = self._newsem("g")
        return self.op("pool", fn, W=[out], R=[in_], dma_sem=(self.dsem_by_key[k], 1))

    def wait_all(self, e, keys):
        for a in keys:
            for k in self.keys(a):
                lw = self.last_w.get(k)
                if lw is not None:
                    self._wait(e, lw)


def _consts():
    i = np.arange(128)[:, None]
    j = np.arange(128)[None, :]
    f, b = {}, {}
    f["ident"] = (i == j).astype(np.float32)
    f["ones"] = np.ones((128, 128), np.float32)
    f["tri"] = (i <= j).astype(np.float32)
    f["neg1"] = np.where(i > j, 0.0, BIG).astype(np.float32)
    f["mask3"] = np.where(j > i, 0.0, -BIG).astype(np.float32)
    f["mask2"] = np.where(j >= i, 0.0, -BIG).astype(np.float32)
    sel = np.zeros((128, 1024), np.float32)
    for h in range(8):
        sel[h, h * 128:(h + 1) * 128] = 1.0
    f["sel"] = sel
    f["hm0"] = np.where(i < 64, 0.125, 0.0).astype(np.float32)[:, :1]
    f["hm1"] = np.where(i >= 64, 0.125, 0.0).astype(np.float32)[:, :1]
    b["ident"] = f["ident"]
    b["ones"] = f["ones"]
    for l in range(7):
        s = 1 << l
        m = ((i // s) % 2 == 1) & ((j // s) == (i // s) - 1)
        b[f"lm{l}"] = m.astype(np.float32)
    b["lm0T"] = b["lm0"].T.copy()
    b["triincl"] = (i >= j).astype(np.float32)
    b["utri"] = (i < j).astype(np.float32)

    def pack(c):
        off, cols, o = {}, [], 0
        for k, v in c.items():
            off[k] = (o, v.shape[1])
            o += v.shape[1]
            cols.append(v)
        return np.concatenate(cols, axis=1), off
    return pack(f), pack(b)


(CSTF_NP, CSTF_OFF), (CSTB_NP, CSTB_OFF) = _consts()


def _small_params(inp, b):
    p = {}
    L = DEPTH
    p["c"] = inp["c"][b].reshape(8, 128).T
    p["ada_b"] = inp["ada_b"].reshape(L, 48, 128).transpose(2, 0, 1).reshape(128, L * 48)
    p["nmix"] = inp["norm_mix"].reshape(L, 8, 128).transpose(2, 0, 1).reshape(128, L * 8)
    p["nffn"] = inp["norm_ffn"].reshape(L, 8, 128).transpose(2, 0, 1).reshape(128, L * 8)
    p["conv"] = inp["conv_w"].reshape(L, 4, 24, 128).transpose(3, 0, 1, 2).reshape(128, L * 96)
    p["alog"] = np.broadcast_to(inp["dn_a_log"].reshape(1, L * 8), (128, L * 8))
    p["dtb"] = np.broadcast_to(inp["dn_dt_bias"].reshape(1, L * 8), (128, L * 8))
    p["onw"] = inp["dn_out_norm"].T
    p["rb"] = np.broadcast_to(inp["router_b"].reshape(1, 16), (128, 16))
    p["rw"] = inp["router_w"].reshape(2, 8, 128, 8).transpose(2, 0, 1, 3).reshape(128, 2 * 8 * 8)
    off, cols, o = {}, [], 0
    for k, v in p.items():
        v = np.ascontiguousarray(v, dtype=np.float32)
        off[k] = (o, v.shape[1])
        o += v.shape[1]
        cols.append(v)
    return np.concatenate(cols, axis=1), off


_SP_OFF = None


def _sp_layout():
    global _SP_OFF
    if _SP_OFF is None:
        fake = {
            "c": np.zeros((8, 1024), np.float32), "ada_b": np.zeros((4, 6144), np.float32),
            "norm_mix": np.zeros((4, 1024), np.float32), "norm_ffn": np.zeros((4, 1024), np.float32),
            "conv_w": np.zeros((4, 4, 3072), np.float32), "dn_a_log": np.zeros((4, 8), np.float32),
            "dn_dt_bias": np.zeros((4, 8), np.float32), "dn_out_norm": np.zeros((4, 128), np.float32),
            "router_b": np.zeros((2, 8), np.float32), "router_w": np.zeros((2, 1024, 8), np.float32),
        }
        a, off = _small_params(fake, 0)
        _SP_OFF = (off, a.shape[1])
    return _SP_OFF


class Prog:
    def __init__(self, nlayers=DEPTH, direct=False, dumps=(), stop=None, layers=None):
        self.nl = nlayers
        self.direct = direct
        self.dumps = set(dumps)
        self.stop = stop
        self.nc = nc = bass.Bass("TRN2", target_bir_lowering=False)
        self.es = contextlib.ExitStack()
        self.S = Sched(nc, self.es)
        self.dump_names = []
        self.nsb = 0
        self.wd = {}
        self.dsems = {}
        self.heads = list(range(NH))
        self.cut = -1
        self.bg = None
        self.bg_head = None
        self.skip = ()
        self.cutn = 0
        self.cb = {}

    def sb(self, name, shape, dt=F32):
        return self.es.enter_context(self.nc.sbuf_tensor(name, list(shape), dt))

    def ps(self, name, shape, dt=F32):
        return self.es.enter_context(self.nc.psum_tensor(name, list(shape), dt))

    def dsem(self, name):
        if name not in self.dsems:
            self.dsems[name] = self.S._newsem("d" + name)
        return self.dsems[name]

    def dram_in(self, name, shape, dt=F32):
        return self.nc.dram_tensor(name, list(shape), dt, kind="ExternalInput").ap()

    def dump(self, name, ap, shape):
        if name not in self.dumps:
            return
        t = self.nc.dram_tensor("dbg_" + name, list(shape), ap.dtype, kind="ExternalOutput").ap()
        self.S.dma("sp", t, ap, self.dsem("dump"))
        self.dump_names.append("dbg_" + name)

    def mm(self, out, lhsT, rhs, start=True, stop=True, inc=None):
        nc = self.nc
        return self.S.op("pe", lambda: nc.tensor.matmul(out, lhsT, rhs, start=start, stop=stop),
                         W=[out], R=[lhsT, rhs], inc=(stop if inc is None else inc))

    def tr(self, out, in_, ident):
        nc = self.nc
        return self.S.op("pe", lambda: nc.tensor.transpose(out, in_, ident), W=[out], R=[in_, ident])

    def act(self, out, in_, func, bias=None, scale=None, accum=None, extraR=()):
        nc = self.nc
        kw = {}
        R = [in_] + list(extraR)
        W = [out]
        if bias is not None:
            kw["bias"] = bias
            if not isinstance(bias, (int, float)):
                R.append(bias)
        if scale is not None:
            kw["scale"] = scale
            if not isinstance(scale, (int, float)):
                R.append(scale)
        if accum is not None:
            kw["accum_out"] = accum
            W.append(accum)
        return self.S.op("act", lambda: nc.scalar.activation(out=out, in_=in_, func=func, **kw), W=W, R=R)

    def tt(self, out, a, b, op, eng="dve"):
        e = self.S.eng[eng]
        return self.S.op(eng, lambda: e.tensor_tensor(out=out, in0=a, in1=b, op=op), W=[out], R=[a, b])

    def ts(self, out, a, s1, op0, s2=None, op1=None, eng="dve"):
        e = self.S.eng[eng]
        R = [a] + [s for s in (s1, s2) if s is not None and not isinstance(s, (int, float))]
        kw = {}
        if op1 is not None:
            kw["op1"] = op1
        return self.S.op(eng, lambda: e.tensor_scalar(out=out, in0=a, scalar1=s1, scalar2=s2, op0=op0, **kw),
                         W=[out], R=R)

    def stt(self, out, a, s, b, op0, op1, eng="dve"):
        e = self.S.eng[eng]
        R = [a, b] + ([s] if not isinstance(s, (int, float)) else [])
        return self.S.op(eng, lambda: e.scalar_tensor_tensor(out=out, in0=a, scalar=s, in1=b, op0=op0, op1=op1),
                         W=[out], R=R)

    def cp(self, out, in_, eng="dve"):
        e = self.S.eng[eng]
        if eng == "act":
            return self.S.op("act", lambda: e.copy(out=out, in_=in_), W=[out], R=[in_])
        return self.S.op(eng, lambda: e.tensor_copy(out=out, in_=in_), W=[out], R=[in_])

    def rsqrt(self, out, in_, mulc, addc):
        self.act(out, in_, AF.Ln, bias=self.cbias(addc), scale=mulc)
        self.act(out, out, AF.Exp, scale=-0.5)

    def cbias(self, v):
        if v not in self.cb:
            i = len(self.cb)
            self.memset(self.CB[:, i:i + 1], float(v))
            self.cb[v] = i
        i = self.cb[v]
        return self.CB[:, i:i + 1]

    def colb(self, t, col0, cstride, nc_, n=128):
        W = int(list(t[:, :].ap)[0][0])
        return bass.AP(t[:, :].tensor, col0, [[W, 128], [cstride, nc_], [0, n]])

    def rowb(self, ap2d, nc_):
        a = list(ap2d.ap)
        return bass.AP(ap2d.tensor, int(ap2d.offset), [[int(a[0][0]), int(a[0][1])], [0, nc_], [int(a[1][0]), int(a[1][1])]])

    @staticmethod
    def v3(ap2d, nc_):
        return ap2d.rearrange("p (c n) -> p c n", c=nc_)

    def memset(self, ap, v, eng="dve"):
        e = self.S.eng[eng]
        return self.S.op(eng, lambda: e.memset(ap, v), W=[ap])

    def wload(self, dst, src, semname):
        q = "pool" if dst.dtype != src.dtype else "sp"
        return self.S.dma(q, dst, src, self.dsem(semname))

    def wv(self, nm, idx, c0, ncol, nk=8, p=128, blk=None, k0=0, nkb=None):
        if self.direct:
            w = self.wd[(nm, idx)]
            r0 = (0 if blk is None else blk * (nkb or nk) * p) + k0 * p
            return w[r0:r0 + nk * p, c0:c0 + ncol].rearrange("(c p) n -> p c n", p=p)
        assert k0 == 0
        off, kind, cols = self.wpack_off[(nm, idx)]
        NR = self.wpack_nr
        t = self.wpack.tensor
        if kind == "row":
            assert nk == NCORES and blk is None
            return bass.AP(t, off + c0, [[cols, p], [NR, nk], [1, ncol]])
        if kind == "col":
            assert ncol == 128 and c0 % 128 == 0
            return bass.AP(t, (c0 // 128) * NR + off, [[128, p], [p * 128, nk], [1, ncol]])
        return bass.AP(t, blk * NR + off + c0, [[cols, p], [p * cols, nk], [1, ncol]])

    def wrows(self, name, idx, r0, nr, c0, ncol):
        w = self.wd[(name, idx)]
        return w[r0:r0 + nr, c0:c0 + ncol].rearrange("(c p) n -> p c n", p=128)

    def C(self, name, bf=False):
        o, w = (CSTB_OFF if bf else CSTF_OFF)[name]
        return (self.CSTB if bf else self.CST)[:, o:o + w]

    def P(self, name, i0=0, n=None):
        off, _ = _sp_layout()
        o, w = off[name]
        n = w - i0 if n is None else n
        return self.SP[:, o + i0:o + i0 + n]

    def setup(self):
        nc = self.nc
        _, spw = _sp_layout()
        wf, wb = CSTF_NP.shape[1], CSTB_NP.shape[1]
        self.x_in = self.dram_in("x", [S, D])
        self.cstf_in = self.dram_in("cstf", [128, wf])
        self.cstb_in = self.dram_in("cstb", [128, wb])
        self.sp_in = self.dram_in("sp", [128, spw])
        self.fn_in = self.dram_in("final_norm", [1, D])
        self.out = nc.dram_tensor("out", [S, D], F32, kind="ExternalOutput").ap()
        sb = self.sb
        self.CST = sb("CST", [128, wf])
        self.CSTB = sb("CSTB", [128, wb], BF16)
        self.SP = sb("SP", [128, spw])
        self.S.dma("sp", self.CST[:], self.cstf_in[:, :], self.dsem("cst"))
        self.S.dma("sp", self.SP[:], self.sp_in[:, :], self.dsem("sp"))
        self.S.dma("pool", self.CSTB[:], self.cstb_in[:, :], self.dsem("cstb"))
        self.X = [sb(f"X{t}", [128, D]) for t in range(16)]
        for t in range(16):
            self.S.dma("sp", self.X[t][:], self.x_in[t * 128:(t + 1) * 128, :], self.dsem("xin"))
        self.PB = [self.ps(f"PB{i}", [128, 512]) for i in range(6)]
        self.PT = [self.ps(f"PT{i}", [128, 1024], BF16) for i in range(2)]
        self.CACT = sb("CACT", [128, 8])
        self.act(self.CACT[:], self.P("c"), AF.Silu)
        self.CACT16 = sb("CACT16", [128, 8], BF16)
        self.cp(self.CACT16[:], self.CACT[:])
        self.MODT = sb("MODT", [128, 48])
        self.WSC1 = sb("WSC1", [128, 8])
        self.WSC2 = sb("WSC2", [128, 8])
        self.NEGA = sb("NEGA", [128, 8])
        self.F = [sb(f"F{i}", [128, 512]) for i in range(6)]
        self.H = [sb(f"H{i}", [128, 512], BF16) for i in range(15)]
        self.W = [sb("W0", [128, 8, 512], BF16), sb("W1", [128, 8, 256], BF16)]
        self.XC = sb("XC", [128, 515])
        self.XN = self.W[1][:, 0:4, :].rearrange("p c t -> p (c t)")
        self.CB = sb("CB", [128, 8])
        self.SS = sb("SS", [128, 8])
        self.RS = sb("RS", [128, 8])
        self.BIG = sb("BIG", [128, 28672], BF16)
        self.BIG2 = sb("BIG2", [128, 8192], BF16)
        self.SF = sb("SF", [128, 8, 128])
        self.SB16 = sb("SB16", [128, 8, 128], BF16)
        self.HIST = sb("HIST", [128, 24, 3])
        self.WBA = sb("WBA", [128, 8, 16], BF16)
        self.WUS = sb("WUS", [64, 8, 128], BF16)
        self.BA = sb("BA", [128, 64])
        for nm in ("BETA", "LNB", "TMPA", "GS", "G", "GL", "GP", "EGP", "KD", "EGL"):
            setattr(self, nm, sb(nm, [128, 32]))
        self.GTS = sb("GTS", [8, 512])
        self.GPTS = sb("GPTS", [8, 512])
        self.phase = None
        for i in range(15):
            self.S.set_regions(f"H{i}", [0, 256, 512, 768])
        self.S.set_regions("F4", [0, 512, 1024, 1536])
        self.S.set_regions("W1", [0, 1024, 2048, 3072])
        print("sbuf bytes remaining after alloc:", nc.sbuf_bytes_remaining)

    def set_phase(self, ph):
        if self.phase == ph:
            return
        self.phase = ph
        B, B2 = self.BIG, self.BIG2
        if ph == "mixer":
            self.S.set_regions("BIG", [2 * v for v in (0, 8192, 16384, 20480, 24576)])
            self.S.set_regions("BIG2", [0, 8192])
            self.SBK = B[:, 0:8192].rearrange("p (c t) -> p c t", c=4)
            self.SBV = B[:, 8192:16384].rearrange("p (c t) -> p c t", c=16)
            self.OGT = B[:, 16384:20480].rearrange("p (c t) -> p c t", c=8)
            self.MTm = B[:, 20480:24576].rearrange("p (c t) -> p c t", c=8)
            self.OSB = B[0:64, 24576:28672].rearrange("p (c t) -> p c t", c=8)
            self.HT = B2[:, 0:4096].rearrange("p (c t) -> p c t", c=8)
            self.SBQP = B2[:, 4096:8192].rearrange("p (c t) -> p c t", c=8)
        else:
            self.S.set_regions("BIG", [0])
            self.S.set_regions("BIG2", [0])
            self.AT = B[:, :].rearrange("p (c t) -> p c t", c=28)
            self.H2T = B2[:, :].rearrange("p (c t) -> p c t", c=8)

    def layer_mod(self, l):
        AWB = self.W[0]
        for blk in range(12):
            self.wload(AWB[:], self.wv("ada_w", l, blk * 512, 512), "w0")
            for jj in range(4):
                j = blk * 4 + jj
                for dc in range(8):
                    self.mm(self.PB[0][:, j:j + 1], AWB[:, dc, jj * 128:(jj + 1) * 128],
                            self.CACT16[:, dc:dc + 1], start=(dc == 0), stop=(dc == 7))
        self.tt(self.MODT[:], self.PB[0][:, 0:48], self.P("ada_b", l * 48, 48), ALU.add)
        for (dst, nm, j0) in ((self.WSC1, "nmix", 8), (self.WSC2, "nffn", 32)):
            self.stt(dst[:], self.MODT[:, j0:j0 + 8], 1.0, self.P(nm, l * 8, 8), ALU.add, ALU.mult)
        self.act(self.NEGA[:], self.P("alog", l * 8, 8), AF.Exp)
        self.ts(self.NEGA[:], self.NEGA[:], -1.0, ALU.mult)
        self.dump(f"modT{l}", self.MODT[:], [128, 48])

    def norm_to_hT(self, tiles, HT, wsc, shc):
        n = len(tiles)
        for i, t in enumerate(tiles):
            self.act(self.XN[:], self.X[t][:], AF.Square, accum=self.SS[:, i:i + 1])
        self.rsqrt(self.RS[:, 0:n], self.SS[:, 0:n], 1.0 / D, EPS)
        for i, t in enumerate(tiles):
            self.ts(self.XN[:], self.X[t][:], self.RS[:, i:i + 1], ALU.mult)
            pt = self.PT[i % 2]
            for dc in range(8):
                self.tr(pt[:, dc * 128:(dc + 1) * 128], self.XN[:, dc * 128:(dc + 1) * 128], self.C("ident", True))
            for dc in range(8):
                self.act(HT[:, dc, i * 128:(i + 1) * 128], pt[:, dc * 128:(dc + 1) * 128], AF.Identity,
                         bias=shc[:, dc:dc + 1], scale=wsc[:, dc:dc + 1])

    def direct_weights(self, names_layers):
        shapes = {"ada_w": (D, 6 * D), "w_in": (D, INC), "w_up_dn": (D, D), "w_up_sb": (512, D), "w_out": (D, D),
                  "ffn_w1": (D, DFF), "ffn_w3": (D, DFF), "ffn_w2": (DFF, D),
                  "moe_w1": (NE * D, DFF), "moe_w3": (NE * D, DFF), "moe_w2": (NE * DFF, D)}
        for (nm, i) in names_layers:
            self.wd[(nm, i)] = self.dram_in(f"{nm}_{i}", list(shapes[nm]))

    def finish(self):
        S_ = self.S
        for k, sem in S_.dsem_by_key.items():
            v = S_.dcnt.get(id(sem), 0)
            if v:
                S_._wait("sp", (sem, v, "dma"))
        self.es.close()

    def proj_fm(self, l, col0, ncols, Wt, wofs, out_ps, semname):
        self.wload(Wt[:, :, wofs:wofs + ncols], self.wrows("w_in", l, 0, D, col0, ncols), semname)
        for dc in range(8):
            self.mm(out_ps, Wt[:, dc, wofs:wofs + ncols], self.HT[:, dc, :], start=(dc == 0), stop=(dc == 7))

    def sb_project(self, l, qt):
        W0, W1 = self.W
        t0 = qt * 512
        self.wload(W0[:], self.wv("w_in", l, C_SQ, 512), "w0")
        for cc in range(4):
            pb = self.PB[cc % 2]
            for dc in range(8):
                self.mm(pb[:, :], W0[:, dc, cc * 128:(cc + 1) * 128], self.HT[:, dc, :], start=(dc == 0), stop=(dc == 7))
            self.ts(self.SBQP[:, 2 * cc, :], pb[:, :], self.C("hm0"), ALU.mult)
            self.ts(self.SBQP[:, 2 * cc + 1, :], pb[:, :], self.C("hm1"), ALU.mult)
        self.wload(W0[:], self.wv("w_in", l, C_SK, 512), "w0")
        for cc in range(4):
            pb = self.PB[cc % 2]
            for dc in range(8):
                self.mm(pb[:, :], W0[:, dc, cc * 128:(cc + 1) * 128], self.HT[:, dc, :], start=(dc == 0), stop=(dc == 7))
            self.cp(self.SBK[:, cc, t0:t0 + 512], pb[:, :], eng="act")
        self.wload(W0[:], self.wv("w_in", l, C_SV, 512), "w0")
        for tt_ in range(4):
            pb = self.PB[tt_ % 2]
            for dc in range(8):
                self.mm(pb[:, :], self.HT[:, dc, tt_ * 128:(tt_ + 1) * 128], W0[:, dc, :], start=(dc == 0), stop=(dc == 7))
            self.cp(self.SBV[:, qt * 4 + tt_, :], pb[:, :])

    def sb_attend(self, qt):
        F, H, PB = self.F, self.H, self.PB
        W1f = self.W[1][:, :, :].rearrange("p c t -> p (c t)").bitcast(F32)
        sets = [
            dict(R=F[0], EX=F[1], SP32=F[2], T1=F[3], SPB=H[0], AT=H[1],
                 PZ=PB[0][:, :], PA=PB[1][:, :], PO=PB[2][:, :], POT=PB[3][0:64, :]),
            dict(R=F[4], EX=F[5], SP32=self.XC[:, 0:512], T1=W1f[:, 0:512], SPB=H[2], AT=H[3],
                 PZ=PB[4][:, :], PA=PB[5][:, :], PO=self.PT[0][:, :].bitcast(F32), POT=self.PT[1][:, :].bitcast(F32)[0:64, :]),
        ]
        utri = self.C("utri", True)
        nkt = 4 * qt + 4
        heads = list(self.heads)
        for hp in range(0, len(heads), 2):
            pair = [(heads[hp + i], sets[i]) for i in range(min(2, len(heads) - hp))]
            for h, st in pair:
                self.memset(st["R"][:, 0:512], 0.0)
            for idx, jb in enumerate(range(nkt - 1, -1, -1)):
                r = jb - 4 * qt
                steps = []
                for h, st in pair:
                    cc = h // 2
                    R, EX, SP32, T1, SPB, AT = st["R"], st["EX"], st["SP32"], st["T1"], st["SPB"], st["AT"]
                    full = lambda a: a[:] if a.ndim == 2 and a.shape[1] == 512 and hasattr(a, "tensor") else a
                    ops = []
                    ops.append(lambda st=st, cc=cc, h=h: self.mm(st["PZ"], self.SBK[:, cc, jb * 128:(jb + 1) * 128], self.SBQP[:, h, :]))
                    ops.append(lambda st=st: self.act(st["EX"][:, 0:512], st["PZ"], AF.Exp))
                    ops.append(lambda st=st: self.act(st["SP32"][:, 0:512], st["EX"][:, 0:512], AF.Ln, bias=self.cbias(1.0)))
                    if r >= 0:
                        def maskcast(st=st):
                            if r > 0:
                                self.memset(st["SPB"][:, 0:r * 128], 0.0)
                            self.tt(st["SPB"][:, r * 128:(r + 1) * 128], st["SP32"][:, r * 128:(r + 1) * 128], utri, ALU.mult)
                            if r < 3:
                                self.cp(st["SPB"][:, (r + 1) * 128:512], st["SP32"][:, (r + 1) * 128:512])
                        ops.append(maskcast)
                    else:
                        ops.append(lambda st=st: self.cp(st["SPB"][:, 0:512], st["SP32"][:, 0:512]))
                    ops.append(lambda st=st: self.mm(st["PA"], self.C("triincl", True), st["SPB"][:, 0:512]))
                    ops.append(lambda st=st: self.mm(st["PO"], self.C("ones", True), st["SPB"][:, 0:512]))
                    ops.append(lambda st=st: self.tt(st["T1"][:, 0:512], st["PZ"], st["R"][:, 0:512], ALU.subtract))
                    ops.append(lambda st=st: self.tt(st["T1"][:, 0:512], st["T1"][:, 0:512], st["PA"], ALU.subtract))
                    ops.append(lambda st=st: self.tt(st["R"][:, 0:512], st["R"][:, 0:512], st["PO"], ALU.add))
                    ops.append(lambda st=st: self.act(st["AT"][:, 0:512], st["T1"][:, 0:512], AF.Exp))
                    if r >= 0:
                        def maskat(st=st):
                            if r > 0:
                                self.memset(st["AT"][:, 0:r * 128], 0.0)
                            self.tt(st["AT"][:, r * 128:(r + 1) * 128], st["AT"][:, r * 128:(r + 1) * 128], utri, ALU.mult)
                        ops.append(maskat)
                    ops.append(lambda st=st, h=h: self.mm(st["POT"], self.SBV[:, jb, h * 64:(h + 1) * 64], st["AT"][:, 0:512],
                                                          start=(idx == 0), stop=(jb == 0), inc=True))
                    steps.append(ops)
                for k in range(max(len(o) for o in steps)):
                    for o in steps:
                        if k < len(o):
                            o[k]()
            for h, st in pair:
                self.cp(self.OSB[:, h, :], st["POT"])

    def dn_prep(self, l, qt):
        PB = self.PB
        self.wload(self.WBA[:], self.wv("w_in", l, C_B, 16), "wba")
        for tt_ in range(4):
            for dc in range(8):
                self.mm(PB[0][:, tt_ * 16:(tt_ + 1) * 16], self.HT[:, dc, tt_ * 128:(tt_ + 1) * 128], self.WBA[:, dc, :],
                        start=(dc == 0), stop=(dc == 7))
        self.cp(self.BA[:], PB[0][:, 0:64])
        for tt_ in range(4):
            b = self.BA[:, tt_ * 16:tt_ * 16 + 8]
            a = self.BA[:, tt_ * 16 + 8:tt_ * 16 + 16]
            s8 = slice(tt_ * 8, (tt_ + 1) * 8)
            self.act(self.BETA[:, s8], b, AF.Sigmoid)
            self.tt(self.TMPA[:, s8], a, self.P("dtb", l * 8, 8), ALU.add)
        self.act(self.LNB[:], self.BETA[:], AF.Ln)
        self.act(self.TMPA[:], self.TMPA[:], AF.Exp)
        self.act(self.TMPA[:], self.TMPA[:], AF.Ln, bias=self.cbias(1.0))
        for tt_ in range(4):
            s8 = slice(tt_ * 8, (tt_ + 1) * 8)
            self.tt(self.GS[:, s8], self.TMPA[:, s8], self.NEGA[:], ALU.mult)
        self.mm(PB[1][:, 0:32], self.C("tri"), self.GS[:])
        self.mm(PB[1][:, 32:64], self.C("ones"), self.GS[:])
        self.cp(self.G[:], PB[1][:, 0:32])
        self.cp(self.GL[:], PB[1][:, 32:64])
        self.tt(self.GP[:], self.G[:], self.LNB[:], ALU.add)
        self.act(self.EGP[:], self.GP[:], AF.Exp)
        self.tt(self.KD[:], self.GL[:], self.G[:], ALU.subtract)
        self.act(self.KD[:], self.KD[:], AF.Exp)
        self.act(self.EGL[:], self.GL[:], AF.Exp)
        for tt_ in range(4):
            s8 = slice(tt_ * 8, (tt_ + 1) * 8)
            self.tr(PB[2][0:8, tt_ * 128:(tt_ + 1) * 128], self.G[:, s8], self.C("ident"))
            self.tr(PB[3][0:8, tt_ * 128:(tt_ + 1) * 128], self.GP[:, s8], self.C("ident"))
        self.cp(self.GTS[:], PB[2][0:8, :])
        self.cp(self.GPTS[:], PB[3][0:8, :])

    def dn_qkv(self, l, qt, h, which, Wt, wofs):
        ch = {"q": 0, "k": 8, "v": 16}[which] + h
        pb = self.PB[4]
        for dc in range(8):
            self.mm(pb[:, :], Wt[:, dc, wofs:wofs + 128], self.HT[:, dc, :], start=(dc == 0), stop=(dc == 7))
        XC, Y = self.XC, self.F[1]
        if qt == 0:
            self.memset(XC[:, 0:3], 0.0)
        else:
            self.cp(XC[:, 0:3], self.HIST[:, ch, :])
        self.cp(XC[:, 3:515], pb[:, :], eng="act")
        if qt < 3:
            self.cp(self.HIST[:, ch, :], XC[:, 512:515])
        cw = lambda k: self.P("conv", l * 96 + k * 24 + ch, 1)
        self.ts(Y[:], XC[:, 0:512], cw(0), ALU.mult)
        for k in range(1, 4):
            self.stt(Y[:], XC[:, k:k + 512], cw(k), Y[:], ALU.mult, ALU.add)
        self.act(Y[:], Y[:], AF.Silu)
        return Y

    def l2n(self, out_bf, Y, mulc, addc):
        SQ = self.F[2]
        self.tt(SQ[:], Y[:], Y[:], ALU.mult)
        self.mm(self.PB[5][:, :], self.C("ones"), SQ[:])
        self.rsqrt(SQ[:], self.PB[5][:, :], mulc, addc)
        self.tt(out_bf, Y[:], SQ[:], ALU.mult)

    def dn_bufs(self, h):
        H = self.H
        if h % 2 == 0:
            return H[0], H[1], H[2], H[3]
        W1f = self.W[1][:, :, :].rearrange("p c t -> p (c t)")
        return tuple(W1f[:, i * 512:(i + 1) * 512] for i in range(4))

    def dn_phase1(self, l, qt, h):
        W0 = self.W[0]
        QN, KN, VC, ZS = self.dn_bufs(h)
        for i, c0 in enumerate((C_Q, C_K, C_V, C_Z)):
            self.wload(W0[:, :, i * 128:(i + 1) * 128], self.wv("w_in", l, c0 + h * 128, 128), "w0")
        yield
        for which, wofs, dst, mulc, addc in (("q", 0, QN, 128.0, 128.0 * EPS), ("k", 128, KN, 1.0, EPS), ("v", 256, VC, None, None)):
            ch = {"q": 0, "k": 8, "v": 16}[which] + h
            pb = self.PB[4]
            for dc in range(8):
                self.mm(pb[:, :], W0[:, dc, wofs:wofs + 128], self.HT[:, dc, :], start=(dc == 0), stop=(dc == 7))
            yield
            XC, Y = self.XC, self.F[1]
            if qt == 0:
                self.memset(XC[:, 0:3], 0.0)
            else:
                self.cp(XC[:, 0:3], self.HIST[:, ch, :])
            self.cp(XC[:, 3:515], pb[:, :], eng="act")
            if qt < 3:
                self.cp(self.HIST[:, ch, :], XC[:, 512:515])
            yield
            cw = lambda k: self.P("conv", l * 96 + k * 24 + ch, 1)
            self.ts(Y[:], XC[:, 0:512], cw(0), ALU.mult)
            for k in range(1, 4):
                self.stt(Y[:], XC[:, k:k + 512], cw(k), Y[:], ALU.mult, ALU.add)
            yield
            self.act(Y[:], Y[:], AF.Silu)
            yield
            if which == "v":
                self.cp(VC[:, 0:512], Y[:])
            else:
                SQ = self.F[2]
                self.tt(SQ[:], Y[:], Y[:], ALU.mult)
                self.mm(self.PB[5][:, :], self.C("ones"), SQ[:])
                yield
                self.rsqrt(SQ[:], self.PB[5][:, :], mulc, addc)
                self.tt(dst[:, 0:512], Y[:], SQ[:], ALU.mult)
            yield
        for dc in range(8):
            self.mm(self.PB[4][:, :], W0[:, dc, 384:512], self.HT[:, dc, :], start=(dc == 0), stop=(dc == 7))
        yield
        self.act(self.F[1][:], self.PB[4][:, :], AF.Silu)
        self.cp(ZS[:, 0:512], self.F[1][:])
        yield

    def bg_step(self, n=1):
        for _ in range(n):
            if self.bg is not None:
                try:
                    next(self.bg)
                except StopIteration:
                    self.bg = None

    def dn_head(self, l, qt, h, nxt=None):
        F, H, PB = self.F, self.H, self.PB
        GBC, GPBC, A, EGB = F[0], F[3], F[4], F[5]
        QN, KN, VC, ZS = self.dn_bufs(h)
        L, LT, QKT, RK, KDEC, RV, M, MT, QM, NWT, QD = [H[i] for i in range(4, 15)]
        Pm, VN = L, VC
        identb = self.C("ident", True)
        if self.bg_head == h:
            while self.bg is not None:
                self.bg_step()
        else:
            for _ in self.dn_phase1(l, qt, h):
                pass
        self.bg, self.bg_head = None, None
        self.mm(PB[4][:, :], self.C("sel")[0:8, h * 128:(h + 1) * 128], self.GTS[:])
        self.cp(GBC[:], PB[4][:, :])
        self.mm(PB[5][:, :], self.C("sel")[0:8, h * 128:(h + 1) * 128], self.GPTS[:])
        self.cp(GPBC[:], PB[5][:, :])
        cs = [slice(c * 128, (c + 1) * 128) for c in range(4)]
        sc = [slice(c * 8 + h, c * 8 + h + 1) for c in range(4)]
        for c in range(4):
            self.mm(PB[0][:, cs[c]], KN[:, cs[c]], KN[:, cs[c]])
            self.mm(PB[1][:, cs[c]], KN[:, cs[c]], QN[:, cs[c]])
        self.tt(self.v3(A[:, :], 4), self.v3(GBC[:, :], 4), self.colb(self.GP, h, 8, 4), ALU.subtract)
        self.tt(self.v3(A[:, :], 4), self.v3(A[:, :], 4), self.rowb(self.C("neg1"), 4), ALU.max)
        self.act(A[:], A[:], AF.Exp, scale=-1.0)
        self.tt(L[:], PB[0][:, :], A[:], ALU.mult)
        self.tt(self.v3(A[:, :], 4), self.v3(GPBC[:, :], 4), self.colb(self.G, h, 8, 4), ALU.subtract)
        self.tt(self.v3(A[:, :], 4), self.v3(A[:, :], 4), self.rowb(self.C("mask3"), 4), ALU.min)
        self.act(A[:], A[:], AF.Exp)
        self.tt(LT[:], PB[0][:, :], A[:], ALU.mult)
        self.tt(self.v3(A[:, :], 4), self.v3(GBC[:, :], 4), self.colb(self.G, h, 8, 4), ALU.subtract)
        self.tt(self.v3(A[:, :], 4), self.v3(A[:, :], 4), self.rowb(self.C("mask2"), 4), ALU.min)
        self.act(A[:], A[:], AF.Exp)
        self.tt(QKT[:], PB[1][:, :], A[:], ALU.mult)
        for c in range(4):
            self.tr(self.PT[0][:, cs[c]], KN[:, cs[c]], identb)
            self.tr(self.PT[1][:, cs[c]], VC[:, cs[c]], identb)
        self.tt(self.v3(RK[:, :], 4), self.v3(self.PT[0][:, 0:512], 4), self.colb(self.EGP, h, 8, 4), ALU.mult)
        self.tt(self.v3(KDEC[:, :], 4), self.v3(self.PT[0][:, 0:512], 4), self.colb(self.KD, h, 8, 4), ALU.mult)
        self.tt(self.v3(RV[:, :], 4), self.v3(self.PT[1][:, 0:512], 4), self.colb(self.BETA, h, 8, 4), ALU.mult)
        self.act(EGB[:], GBC[:], AF.Exp)
        self.tt(QD[:], QN[:, 0:512], EGB[:], ALU.mult)
        self.tt(self.v3(QM[:, :], 4), self.v3(L[:, :], 4), self.rowb(self.C("lm0", True), 4), ALU.mult)
        self.tt(self.v3(M[:, :], 4), self.rowb(identb, 4), self.v3(QM[:, :], 4), ALU.subtract)
        self.tt(self.v3(QM[:, :], 4), self.v3(LT[:, :], 4), self.rowb(self.C("lm0T", True), 4), ALU.mult)
        self.tt(self.v3(MT[:, :], 4), self.rowb(identb, 4), self.v3(QM[:, :], 4), ALU.subtract)
        if nxt is not None:
            self.bg, self.bg_head = self.dn_phase1(l, qt, nxt), nxt
        gs = [slice(0, 256), slice(256, 512)]
        PPb, PQb, PTb = [PB[2], PB[0]], [PB[3], PB[1]], [self.PT[0], self.PT[1]]
        for lv in range(1, 7):
            lm = self.C(f"lm{lv}", True)
            for g in range(2):
                for c in (2 * g, 2 * g + 1):
                    self.mm(PPb[g][:, cs[c]], LT[:, cs[c]], M[:, cs[c]])
            self.bg_step()
            for g in range(2):
                self.cp(Pm[:, gs[g]], PPb[g][:, gs[g]], eng="act")
            self.bg_step()
            for g in range(2):
                for c in (2 * g, 2 * g + 1):
                    self.mm(PQb[g][:, cs[c]], MT[:, cs[c]], Pm[:, cs[c]])
            self.bg_step()
            for g in range(2):
                self.tt(self.v3(QM[:, gs[g]], 2), self.v3(PQb[g][:, gs[g]], 2), self.rowb(lm, 2), ALU.mult)
            for g in range(2):
                self.tt(M[:, gs[g]], M[:, gs[g]], QM[:, gs[g]], ALU.subtract)
            self.bg_step()
            for g in range(2):
                for c in (2 * g, 2 * g + 1):
                    self.tr(PTb[g][:, cs[c]], QM[:, cs[c]], identb)
            self.bg_step()
            for g in range(2):
                self.tt(MT[:, gs[g]], MT[:, gs[g]], PTb[g][:, gs[g]], ALU.subtract)
        while self.bg is not None:
            self.bg_step()
        for c in range(4):
            self.mm(PB[2][:, cs[c]], RK[:, cs[c]], MT[:, cs[c]])
        self.ts(NWT[:], PB[2][:, :], -1.0, ALU.mult)
        if qt == 0:
            self.memset(self.SF[:, h, :], 0.0)
            self.memset(self.SB16[:, h, :], 0.0)
        S16 = self.SB16[:, h, :]
        for c in range(4):
            pv = PB[4][:, 0:128]
            self.mm(pv, MT[:, cs[c]], RV[:, cs[c]], start=True, stop=False)
            self.mm(pv, NWT[:, cs[c]], S16, start=False, stop=True)
            self.cp(VN[:, cs[c]], pv)
            self.mm(PB[5][:, cs[c]], S16, QD[:, cs[c]], start=True, stop=False)
            self.mm(PB[5][:, cs[c]], VN[:, cs[c]], QKT[:, cs[c]], start=False, stop=True)
            ps = PB[3][:, 0:128]
            self.mm(ps, KDEC[:, cs[c]], VN[:, cs[c]])
            self.stt(self.SF[:, h, :], self.SF[:, h, :], self.EGL[:, sc[c]], ps, ALU.mult, ALU.add)
            self.cp(S16, self.SF[:, h, :])
        OT = F[1]
        self.cp(OT[:], PB[5][:, :], eng="act")
        self.dump(f"OT{qt}_{h}", OT[:], [128, 512])
        SQ = F[2]
        self.tt(SQ[:], OT[:], OT[:], ALU.mult)
        self.mm(PB[4][:, :], self.C("ones"), SQ[:])
        self.rsqrt(SQ[:], PB[4][:, :], 1.0 / 128.0, EPS)
        self.tt(OT[:], OT[:], SQ[:], ALU.mult)
        self.stt(self.OGT[:, h, :], OT[:], self.P("onw", l, 1), ZS[:, 0:512], ALU.mult, ALU.mult)

    def merge_out(self, l, qt):
        F, PB = self.F, self.PB
        W0, W1 = self.W
        for oc in range(8):
            self.wload(W1[:, :, 0:128], self.wv("w_in", l, C_G + oc * 128, 128), "w1")
            self.wload(W1[:, :, 128:256], self.wv("w_in", l, C_G + D + oc * 128, 128), "w1")
            for dc in range(8):
                self.mm(PB[0][:, :], W1[:, dc, 0:128], self.HT[:, dc, :], start=(dc == 0), stop=(dc == 7))
            for dc in range(8):
                self.mm(PB[1][:, :], W1[:, dc, 128:256], self.HT[:, dc, :], start=(dc == 0), stop=(dc == 7))
            self.act(F[0][:], PB[0][:, :], AF.Sigmoid)
            self.act(F[1][:], PB[1][:, :], AF.Sigmoid)
            self.wload(W0[:, :, 0:128], self.wv("w_up_dn", l, oc * 128, 128), "w0")
            for h in range(8):
                self.mm(PB[2][:, :], W0[:, h, 0:128], self.OGT[:, h, :], start=(h == 0), stop=(h == 7))
            self.wload(self.WUS[:], self.wv("w_up_sb", l, oc * 128, 128, nk=8, p=64), "wus")
            for h in range(8):
                self.mm(PB[3][:, :], self.WUS[:, h, :], self.OSB[:, h, :], start=(h == 0), stop=(h == 7))
            self.tt(F[2][:], PB[2][:, :], F[0][:], ALU.mult)
            self.tt(F[3][:], PB[3][:, :], F[1][:], ALU.mult)
            self.tt(self.MTm[:, oc, :], F[2][:], F[3][:], ALU.add)
        self.proj_residual(("w_out", l), self.MTm, 8, [4 * qt + i for i in range(4)], 16)

    def proj_residual(self, wkey, actT, nk, tiles, modj, gate_ps=None, blk=None):
        F, PB, H = self.F, self.PB, self.H
        W0f = self.W[0][:, :, :].rearrange("p c t -> p (c t)")
        slotA = W0f[:, 0:nk * 128].rearrange("p (c t) -> p c t", c=nk)
        if 2 * nk * 128 <= 4096:
            slotB = W0f[:, 2048:2048 + nk * 128].rearrange("p (c t) -> p c t", c=nk)
            slots = [([slotA[:, k, :] for k in range(nk)], [(slotA, 0, nk)]),
                     ([slotB[:, k, :] for k in range(nk)], [(slotB, 0, nk)])]
        else:
            nh = (nk + 3) // 4
            bk = [H[k // 4][:, (k % 4) * 128:(k % 4 + 1) * 128] for k in range(nk)]
            bl = [(H[i][:, 0:min(4, nk - 4 * i) * 128].rearrange("p (c t) -> p c t", c=min(4, nk - 4 * i)), 4 * i,
                   min(4, nk - 4 * i)) for i in range(nh)]
            slots = [([slotA[:, k, :] for k in range(nk)], [(slotA, 0, nk)]), (bk, bl)]
        ng = len(tiles) // 4
        it = 0
        for oc in range(8):
            kaps, loads = slots[oc % 2]
            for (dst, k0, n) in loads:
                self.wload(dst, self.wv(wkey[0], wkey[1], oc * 128, 128, nk=n, blk=blk, k0=k0, nkb=nk), "w0")
            for g in range(ng):
                pa, pt, Fs = (PB[0], PB[1], F[0]) if it % 2 == 0 else (PB[2], PB[3], F[3])
                it += 1
                for k in range(nk):
                    self.mm(pa[:, :], kaps[k], actT[:, k, g * 512:(g + 1) * 512], start=(k == 0), stop=(k == nk - 1))
                self.ts(Fs[:], pa[:, :], self.MODT[:, modj + oc:modj + oc + 1], ALU.mult)
                if gate_ps is not None:
                    self.tt(Fs[:], Fs[:], gate_ps[g], ALU.mult)
                for i in range(4):
                    self.tr(pt[:, i * 128:(i + 1) * 128], Fs[:, i * 128:(i + 1) * 128], self.C("ident"))
                for i in range(4):
                    xs = self.X[tiles[g * 4 + i]][:, oc * 128:(oc + 1) * 128]
                    self.tt(xs, xs, pt[:, i * 128:(i + 1) * 128], ALU.add)

    def mixer(self, l):
        self.set_phase("mixer")
        for qt in range(4):
            self.norm_to_hT([4 * qt + i for i in range(4)], self.HT, self.WSC1, self.MODT[:, 0:8])
            self.sb_project(l, qt)
            if "sb" not in self.skip:
                self.sb_attend(qt)
            self.dn_prep(l, qt)
            for h in range(NH):
                if "dn" not in self.skip:
                    self.dn_head(l, qt, h, nxt=(h + 1 if h + 1 < NH else None))
            if "mo" not in self.skip:
                self.merge_out(l, qt)

    def swiglu_up(self, w1key, w3key, blk=None):
        F, PB = self.F, self.PB
        SFb = self.SF[:, :, :].rearrange("p c t -> p (c t)").bitcast(BF16).rearrange("p (c t) -> p c t", c=8)
        bufs = [self.W[1], SFb]
        for fc in range(DFF // 128):
            Wb = bufs[fc % 2]
            for i, wk in enumerate((w1key, w3key)):
                self.wload(Wb[:, :, i * 128:(i + 1) * 128], self.wv(wk[0], wk[1], fc * 128, 128, blk=blk), "w1")
            for tg in range(2):
                ts_ = slice(tg * 512, (tg + 1) * 512)
                pa, pb_, Fs = (PB[2], PB[3], F[1]) if tg == 0 else (PB[4], PB[5], F[2])
                for dc in range(8):
                    self.mm(pa[:, :], Wb[:, dc, 0:128], self.H2T[:, dc, ts_], start=(dc == 0), stop=(dc == 7))
                for dc in range(8):
                    self.mm(pb_[:, :], Wb[:, dc, 128:256], self.H2T[:, dc, ts_], start=(dc == 0), stop=(dc == 7))
                self.act(Fs[:], pa[:, :], AF.Silu)
                self.tt(self.AT[:, fc, ts_], Fs[:], pb_[:, :], ALU.mult)

    def ffn_dense(self, l):
        self.set_phase("ffn")
        j = l // 2
        for hf in range(2):
            tiles = [8 * hf + i for i in range(8)]
            self.norm_to_hT(tiles, self.H2T, self.WSC2, self.MODT[:, 24:32])
            self.swiglu_up(("ffn_w1", j), ("ffn_w3", j))
            self.proj_residual(("ffn_w2", j), self.AT, DFF // 128, tiles, 40)

    def moe_alloc(self):
        sb = self.sb
        for nm in ("LG", "MX", "EE", "MK", "GATE"):
            setattr(self, nm, sb(nm, [128, 64]))
        self.DEN = sb("DEN", [128, 8])
        self.RW16 = sb("RW16", [128, 64], BF16)

    def ffn_moe(self, l):
        self.set_phase("ffn")
        j = l // 2
        F, PB = self.F, self.PB
        self.cp(self.RW16[:], self.P("rw", j * 64, 64))
        for hf in range(2):
            tiles = [8 * hf + i for i in range(8)]
            self.norm_to_hT(tiles, self.H2T, self.WSC2, self.MODT[:, 24:32])
            for i in range(8):
                for dc in range(8):
                    self.mm(PB[0][:, i * 8:(i + 1) * 8], self.H2T[:, dc, i * 128:(i + 1) * 128],
                            self.RW16[:, dc * 8:(dc + 1) * 8], start=(dc == 0), stop=(dc == 7))
            for i in range(8):
                s8 = slice(i * 8, (i + 1) * 8)
                self.tt(self.LG[:, s8], PB[0][:, s8], self.P("rb", j * 8, 8), ALU.add)
            for i in range(8):
                s8 = slice(i * 8, (i + 1) * 8)
                nc = self.nc
                self.S.op("dve", lambda o=self.MX[:, s8], a=self.LG[:, s8]: nc.vector.max(out=o, in_=a),
                          W=[self.MX[:, s8]], R=[self.LG[:, s8]])
            for i in range(8):
                s8 = slice(i * 8, (i + 1) * 8)
                self.ts(self.EE[:, s8], self.LG[:, s8], self.MX[:, i * 8:i * 8 + 1], ALU.subtract)
                self.ts(self.MK[:, s8], self.LG[:, s8], self.MX[:, i * 8 + 1:i * 8 + 2], ALU.is_ge)
                self.tt(self.DEN[:, i:i + 1], self.MX[:, i * 8 + 1:i * 8 + 2], self.MX[:, i * 8:i * 8 + 1], ALU.subtract)
            self.act(self.EE[:], self.EE[:], AF.Exp)
            self.tt(self.EE[:], self.EE[:], self.MK[:], ALU.mult)
            self.act(self.DEN[:], self.DEN[:], AF.Exp)
            self.ts(self.DEN[:], self.DEN[:], 1.0, ALU.add)
            nc = self.nc
            self.S.op("dve", lambda: nc.vector.reciprocal(out=self.DEN[:], in_=self.DEN[:]), W=[self.DEN[:]], R=[self.DEN[:]])
            for i in range(8):
                s8 = slice(i * 8, (i + 1) * 8)
                self.ts(self.GATE[:, s8], self.EE[:, s8], self.DEN[:, i:i + 1], ALU.mult)
            for i in range(8):
                pb = PB[1] if i < 4 else PB[2]
                self.tr(pb[0:8, (i % 4) * 128:(i % 4 + 1) * 128], self.GATE[:, i * 8:(i + 1) * 8], self.C("ident"))
            self.cp(self.GTS[:], PB[1][0:8, :])
            self.cp(self.GPTS[:], PB[2][0:8, :])
            self.dump(f"gate{l}_{hf}", self.GATE[:], [128, 64])
            for e in range(NE):
                self.swiglu_up(("moe_w1", j), ("moe_w3", j), blk=e)
                sel = self.C("sel")[0:8, e * 128:(e + 1) * 128]
                self.mm(PB[4][:, :], sel, self.GTS[:])
                self.mm(PB[5][:, :], sel, self.GPTS[:])
                self.proj_residual(("moe_w2", j), self.AT, DFF // 128, tiles, 40,
                                   gate_ps=[PB[4][:, :], PB[5][:, :]], blk=e)

    def final(self):
        F = self.F
        self.S.dma("sp", F[0][:], self.fn_in[0:1, 0:512].partition_broadcast(128))
        self.S.dma("sp", F[1][:], self.fn_in[0:1, 512:1024].partition_broadcast(128))
        for hf in range(2):
            tiles = [8 * hf + i for i in range(8)]
            for i, t in enumerate(tiles):
                self.act(self.XN[:], self.X[t][:], AF.Square, accum=self.SS[:, i:i + 1])
            self.rsqrt(self.RS[:, 0:8], self.SS[:, 0:8], 1.0 / D, EPS)
            for i, t in enumerate(tiles):
                for c in range(2):
                    xs = self.X[t][:, c * 512:(c + 1) * 512]
                    self.stt(xs, xs, self.RS[:, i:i + 1], F[c][:], ALU.mult, ALU.mult)
                self.S.dma("sp", self.out[t * 128:(t + 1) * 128, :], self.X[t][:])


W_SHAPES = {"ada_w": (D, 6 * D), "w_in": (D, INC), "w_up_dn": (D, D), "w_up_sb": (512, D), "w_out": (D, D),
            "ffn_w1": (D, DFF), "ffn_w3": (D, DFF), "ffn_w2": (DFF, D),
            "moe_w1": (NE * D, DFF), "moe_w3": (NE * D, DFF), "moe_w2": (NE * DFF, D)}


def weight_list(nlayers=DEPTH):
    out = []
    for l in range(nlayers):
        out += [("ada_w", l), ("w_in", l), ("w_up_dn", l), ("w_up_sb", l), ("w_out", l)]
        j = l // 2
        if l % 2 == 0:
            out += [("ffn_w1", j), ("ffn_w3", j), ("ffn_w2", j)]
        else:
            out += [("moe_w1", j), ("moe_w3", j), ("moe_w2", j)]
    return out


W_KIND = {"ada_w": "row", "w_in": "row", "w_up_dn": "row", "w_up_sb": "row", "w_out": "row",
          "ffn_w1": "row", "ffn_w3": "row", "ffn_w2": "col", "moe_w1": "exp", "moe_w3": "exp", "moe_w2": "exp"}
PACK_C = 2048


def pack_layout(wl):
    off, o = {}, 0
    for (nm, i) in wl:
        rows, cols = W_SHAPES[nm]
        off[(nm, i)] = (o, W_KIND[nm], cols)
        o += rows * cols // NCORES
    nr = (o + PACK_C * 128 - 1) // (PACK_C * 128) * (PACK_C * 128)
    return off, nr


def pack_rank(inputs, wl, r):
    off, nr = pack_layout(wl)
    buf = np.zeros((nr,), np.float32)
    for (nm, i) in wl:
        a = np.asarray(inputs[nm][i], dtype=np.float32)
        o, kind, cols = off[(nm, i)]
        if kind == "row":
            a2 = a.reshape(-1, a.shape[-1])
            rs = a2.shape[0] // NCORES
            seg = a2[r * rs:(r + 1) * rs]
        elif kind == "col":
            seg = a[:, r * 128:(r + 1) * 128]
        else:
            seg = a[r]
        buf[o:o + seg.size] = np.ascontiguousarray(seg).reshape(-1)
    return buf.reshape(-1, PACK_C)


def gather_weights(p, wl):
    nc = p.nc
    off, nr = pack_layout(wl)
    R = nr // PACK_C
    shard = p.dram_in("wpack", [R, PACK_C])
    src = nc.dram_tensor("wpack_c", [R, PACK_C], F32, kind="Internal").ap()
    dst = nc.dram_tensor("wpack_g", [NCORES * R, PACK_C], F32, kind="Internal").ap()
    p.S.dma("pool", src[:, :], shard[:, :])
    p.S.collective(lambda: nc.gpsimd.collective_compute(
        "AllGather", ALU.bypass, replica_groups=[list(range(NCORES))], ins=[src[:, :]], outs=[dst[:, :]]),
        dst[:, :], src[:, :])
    p.S.wait_all("pool", [dst[:, :]])
    p.wpack, p.wpack_off, p.wpack_nr = dst, off, nr


REPLICATE = True


def build_full(nlayers=DEPTH):
    p = Prog(direct=REPLICATE)
    p.setup()
    p.moe_alloc()
    wl = weight_list(nlayers)
    if REPLICATE:
        p.direct_weights(wl)
    else:
        gather_weights(p, wl)
    for l in range(nlayers):
        p.layer_mod(l)
        p.mixer(l)
        if l % 2 == 0:
            p.ffn_dense(l)
        else:
            p.ffn_moe(l)
    p.final()
    p.finish()
    return p


def make_in_maps(inputs, nlayers=DEPTH):
    wl = weight_list(nlayers)
    maps = []
    fn = np.ascontiguousarray(np.asarray(inputs["final_norm"], dtype=np.float32).reshape(1, D))
    shared = {}
    if REPLICATE:
        for (nm, i) in wl:
            a = np.asarray(inputs[nm][i], dtype=np.float32)
            shared[f"{nm}_{i}"] = a.reshape(-1, a.shape[-1])
    for b in range(NCORES):
        sp, _ = _small_params(inputs, b)
        m = {"x": np.ascontiguousarray(inputs["x"][b], dtype=np.float32), "cstf": CSTF_NP, "cstb": CSTB_NP,
             "sp": sp, "final_norm": fn}
        if REPLICATE:
            m.update(shared)
        else:
            m["wpack"] = pack_rank(inputs, wl, b)
        maps.append(m)
    return maps


def kernel(**inputs):
    inputs = {k: np.asarray(v) for k, v in inputs.items()}
    p = build_full()
    in_maps = make_in_maps(inputs)
    res = run_bass_kernel_spmd(p.nc, in_maps, core_ids=list(range(NCORES)))
    out = np.stack([np.asarray(r["out"], dtype=np.float32) for r in res.results], axis=0)
    return out.reshape(NCORES, S, D)
```

```python
import bisect
import contextlib
import numpy as np
import concourse.bass as bass
import concourse.mybir as mybir
from concourse.bass_utils import run_bass_kernel_spmd

F32 = mybir.dt.float32
BF16 = mybir.dt.bfloat16
AF = mybir.ActivationFunctionType
ALU = mybir.AluOpType

D = 1024
S = 2048
DEPTH = 4
NH = 8
DFF = 3584
NE = 8
INC = 7696
EPS = 1e-6
BIG = 30000.0
NCORES = 8
RELAX = False
C_Q, C_K, C_V, C_Z, C_B, C_A = 0, 1024, 2048, 3072, 4096, 4104
C_SQ, C_SK, C_SV, C_G = 4112, 4624, 5136, 5648


class Sched:
    ROT = 20000

    def __init__(self, nc, es):
        self.nc, self.es = nc, es
        self.eng = {"pe": nc.tensor, "act": nc.scalar, "dve": nc.vector, "pool": nc.gpsimd, "sp": nc.sync}
        self.sem, self.cnt, self.nsem = {}, {}, 0
        self.seen = {e: {} for e in self.eng}
        self.last_w = {}
        self.readers = {}
        self.pending = {e: [] for e in self.eng}
        self.semobj = {}
        for e in ("pe", "act", "dve", "pool"):
            self._rot(e)
        self.ninst = 0
        self.dcnt = {}
        self.regions = {}
        self.relax = RELAX
        self.dsem_by_key = {}
        self.carry = {}

    def _newsem(self, tag):
        self.nsem += 1
        s = self.es.enter_context(self.nc.semaphore(f"{tag}{self.nsem}"))
        self.semobj[id(s)] = s
        return s

    def _rot(self, e):
        self.sem[e] = self._newsem("s" + e)
        self.cnt[e] = 0

    @staticmethod
    def _dsize(dt):
        return 4 if dt == F32 else 2

    def keys(self, a):
        if isinstance(a, str):
            return [a]
        nm = a.tensor.name
        st = self.regions.get(nm)
        if st is None:
            return [nm]
        sz = self._dsize(a.dtype)
        lo = int(a.offset) * sz
        span = 0
        for (stride, count) in list(a.ap)[1:]:
            span += (int(count) - 1) * abs(int(stride))
        hi = lo + (span + 1) * sz
        i0 = bisect.bisect_right(st, lo) - 1
        out = []
        i = i0
        while i < len(st) and st[i] < hi:
            out.append(f"{nm}#{i}")
            i += 1
        return out

    def key(self, a):
        return self.keys(a)[0]

    def set_regions(self, nm, starts):
        old = [k for k in list(self.last_w.keys()) + list(self.readers.keys()) if k == nm or k.startswith(nm + "#")]
        tags = []
        for k in set(old):
            lw = self.last_w.pop(k, None)
            if lw is not None:
                tags.append(lw)
            for (sid, (val, eng_r)) in self.readers.pop(k, {}).items():
                tags.append((self.semobj[sid], val, eng_r))
        self.regions[nm] = list(starts)
        self.carry[nm] = tags

    def _wait(self, e, tag):
        sem, val, _ = tag
        d = self.seen[e]
        if d.get(id(sem), -1) >= val:
            return
        self.eng[e].wait_ge(sem, val)
        d[id(sem)] = val
        self.ninst += 1

    def op(self, e, fn, W=(), R=(), inc=True, dma_sem=None):
        wk = [k for a in W for k in self.keys(a)]
        rk = [k for a in R for k in self.keys(a)]
        for k in wk + rk:
            if "#" in k:
                for tag in self.carry.get(k.split("#")[0], ()):
                    self._wait(e, tag)
        for k in rk:
            lw = self.last_w.get(k)
            if lw is not None:
                self._wait(e, lw)
        for k in wk:
            lw = self.last_w.get(k)
            if lw is not None and not (lw[2] == e and (e == "pe" or self.relax)):
                self._wait(e, lw)
            for (sid, (val, eng_r)) in list(self.readers.get(k, {}).items()):
                if not (self.relax and eng_r == e):
                    self._wait(e, (self.semobj[sid], val, eng_r))
        ins = fn()
        self.ninst += 1
        if dma_sem is not None:
            sem, incv = dma_sem if len(dma_sem) == 2 else (dma_sem[0], 16)
            self.dcnt[id(sem)] = self.dcnt.get(id(sem), 0) + incv
            ins.then_inc(sem, incv)
            tag = (sem, self.dcnt[id(sem)], "dma")
        elif inc:
            self.cnt[e] += 1
            ins.then_inc(self.sem[e], 1)
            tag = (self.sem[e], self.cnt[e], e)
        else:
            self.pending[e].append((wk, rk))
            return ins
        items = self.pending[e] + [(wk, rk)]
        self.pending[e] = []
        for (wk_, rk_) in items:
            for k in rk_:
                self.readers.setdefault(k, {})[id(tag[0])] = (tag[1], tag[2])
            for k in wk_:
                self.last_w[k] = tag
                self.readers[k] = {}
        if dma_sem is None and self.cnt[e] >= self.ROT:
            self._rot(e)
        return ins

    def dma(self, q, out, in_, sem=None):
        eng = self.eng[q]
        k = self.key(out)
        if k not in self.dsem_by_key:
            self.dsem_by_key[k] = self._newsem("d")
        sem = self.dsem_by_key[k]
        return self.op(q, lambda: eng.dma_start(out=out, in_=in_), W=[out], R=[in_], dma_sem=(sem,))

    def collective(self, fn, out, in_):
        k = self.key(out)
        if k not in self.dsem_by_key:
            self.dsem_by_key[k] = self._newsem("g")
        return self.op("pool", fn, W=[out], R=[in_], dma_sem=(self.dsem_by_key[k], 1))

    def wait_all(self, e, keys):
        for a in keys:
            for k in self.keys(a):
                lw = self.last_w.get(k)
                if lw is not None:
                    self._wait(e, lw)


def _consts():
    i = np.arange(128)[:, None]
    j = np.arange(128)[None, :]
    f, b = {}, {}
    f["ident"] = (i == j).astype(np.float32)
    f["ones"] = np.ones((128, 128), np.float32)
    f["tri"] = (i <= j).astype(np.float32)
    f["neg1"] = np.where(i > j, 0.0, BIG).astype(np.float32)
    f["mask3"] = np.where(j > i, 0.0, -BIG).astype(np.float32)
    f["mask2"] = np.where(j >= i, 0.0, -BIG).astype(np.float32)
    sel = np.zeros((128, 1024), np.float32)
    for h in range(8):
        sel[h, h * 128:(h + 1) * 128] = 1.0
    f["sel"] = sel
    f["hm0"] = np.where(i < 64, 0.125, 0.0).astype(np.float32)[:, :1]
    f["hm1"] = np.where(i >= 64, 0.125, 0.0).astype(np.float32)[:, :1]
    b["ident"] = f["ident"]
    b["ones"] = f["ones"]
    for l in range(7):
        s = 1 << l
        m = ((i // s) % 2 == 1) & ((j // s) == (i // s) - 1)
        b[f"lm{l}"] = m.astype(np.float32)
    b["lm0T"] = b["lm0"].T.copy()
    b["triincl"] = (i >= j).astype(np.float32)
    b["utri"] = (i < j).astype(np.float32)

    def pack(c):
        off, cols, o = {}, [], 0
        for k, v in c.items():
            off[k] = (o, v.shape[1])
            o += v.shape[1]
            cols.append(v)
        return np.concatenate(cols, axis=1), off
    return pack(f), pack(b)


(CSTF_NP, CSTF_OFF), (CSTB_NP, CSTB_OFF) = _consts()


def _small_params(inp, b):
    p = {}
    L = DEPTH
    p["c"] = inp["c"][b].reshape(8, 128).T
    p["ada_b"] = inp["ada_b"].reshape(L, 48, 128).transpose(2, 0, 1).reshape(128, L * 48)
    p["nmix"] = inp["norm_mix"].reshape(L, 8, 128).transpose(2, 0, 1).reshape(128, L * 8)
    p["nffn"] = inp["norm_ffn"].reshape(L, 8, 128).transpose(2, 0, 1).reshape(128, L * 8)
    p["conv"] = inp["conv_w"].reshape(L, 4, 24, 128).transpose(3, 0, 1, 2).reshape(128, L * 96)
    p["alog"] = np.broadcast_to(inp["dn_a_log"].reshape(1, L * 8), (128, L * 8))
    p["dtb"] = np.broadcast_to(inp["dn_dt_bias"].reshape(1, L * 8), (128, L * 8))
    p["onw"] = inp["dn_out_norm"].T
    p["rb"] = np.broadcast_to(inp["router_b"].reshape(1, 16), (128, 16))
    p["rw"] = inp["router_w"].reshape(2, 8, 128, 8).transpose(2, 0, 1, 3).reshape(128, 2 * 8 * 8)
    off, cols, o = {}, [], 0
    for k, v in p.items():
        v = np.ascontiguousarray(v, dtype=np.float32)
        off[k] = (o, v.shape[1])
        o += v.shape[1]
        cols.append(v)
    return np.concatenate(cols, axis=1), off


_SP_OFF = None


def _sp_layout():
    global _SP_OFF
    if _SP_OFF is None:
        fake = {
            "c": np.zeros((8, 1024), np.float32), "ada_b": np.zeros((4, 6144), np.float32),
            "norm_mix": np.zeros((4, 1024), np.float32), "norm_ffn": np.zeros((4, 1024), np.float32),
            "conv_w": np.zeros((4, 4, 3072), np.float32), "dn_a_log": np.zeros((4, 8), np.float32),
            "dn_dt_bias": np.zeros((4, 8), np.float32), "dn_out_norm": np.zeros((4, 128), np.float32),
            "router_b": np.zeros((2, 8), np.float32), "router_w": np.zeros((2, 1024, 8), np.float32),
        }
        a, off = _small_params(fake, 0)
        _SP_OFF = (off, a.shape[1])
    return _SP_OFF


class Prog:
    def __init__(self, nlayers=DEPTH, direct=False, dumps=(), stop=None, layers=None):
        self.nl = nlayers
        self.direct = direct
        self.dumps = set(dumps)
        self.stop = stop
        self.nc = nc = bass.Bass("TRN2", target_bir_lowering=False)
        self.es = contextlib.ExitStack()
        self.S = Sched(nc, self.es)
        self.dump_names = []
        self.nsb = 0
        self.wd = {}
        self.dsems = {}
        self.heads = list(range(NH))
        self.cut = -1
        self.bg = None
        self.bg_head = None
        self.skip = ()
        self.cutn = 0
        self.cb = {}

    def sb(self, name, shape, dt=F32):
        return self.es.enter_context(self.nc.sbuf_tensor(name, list(shape), dt))

    def ps(self, name, shape, dt=F32):
        return self.es.enter_context(self.nc.psum_tensor(name, list(shape), dt))

    def dsem(self, name):
        if name not in self.dsems:
            self.dsems[name] = self.S._newsem("d" + name)
        return self.dsems[name]

    def dram_in(self, name, shape, dt=F32):
        return self.nc.dram_tensor(name, list(shape), dt, kind="ExternalInput").ap()

    def dump(self, name, ap, shape):
        if name not in self.dumps:
            return
        t = self.nc.dram_tensor("dbg_" + name, list(shape), ap.dtype, kind="ExternalOutput").ap()
        self.S.dma("sp", t, ap, self.dsem("dump"))
        self.dump_names.append("dbg_" + name)

    def mm(self, out, lhsT, rhs, start=True, stop=True, inc=None):
        nc = self.nc
        return self.S.op("pe", lambda: nc.tensor.matmul(out, lhsT, rhs, start=start, stop=stop),
                         W=[out], R=[lhsT, rhs], inc=(stop if inc is None else inc))

    def tr(self, out, in_, ident):
        nc = self.nc
        return self.S.op("pe", lambda: nc.tensor.transpose(out, in_, ident), W=[out], R=[in_, ident])

    def act(self, out, in_, func, bias=None, scale=None, accum=None, extraR=()):
        nc = self.nc
        kw = {}
        R = [in_] + list(extraR)
        W = [out]
        if bias is not None:
            kw["bias"] = bias
            if not isinstance(bias, (int, float)):
                R.append(bias)
        if scale is not None:
            kw["scale"] = scale
            if not isinstance(scale, (int, float)):
                R.append(scale)
        if accum is not None:
            kw["accum_out"] = accum
            W.append(accum)
        return self.S.op("act", lambda: nc.scalar.activation(out=out, in_=in_, func=func, **kw), W=W, R=R)

    def tt(self, out, a, b, op, eng="dve"):
        e = self.S.eng[eng]
        return self.S.op(eng, lambda: e.tensor_tensor(out=out, in0=a, in1=b, op=op), W=[out], R=[a, b])

    def ts(self, out, a, s1, op0, s2=None, op1=None, eng="dve"):
        e = self.S.eng[eng]
        R = [a] + [s for s in (s1, s2) if s is not None and not isinstance(s, (int, float))]
        kw = {}
        if op1 is not None:
            kw["op1"] = op1
        return self.S.op(eng, lambda: e.tensor_scalar(out=out, in0=a, scalar1=s1, scalar2=s2, op0=op0, **kw),
                         W=[out], R=R)

    def stt(self, out, a, s, b, op0, op1, eng="dve"):
        e = self.S.eng[eng]
        R = [a, b] + ([s] if not isinstance(s, (int, float)) else [])
        return self.S.op(eng, lambda: e.scalar_tensor_tensor(out=out, in0=a, scalar=s, in1=b, op0=op0, op1=op1),
                         W=[out], R=R)

    def cp(self, out, in_, eng="dve"):
        e = self.S.eng[eng]
        if eng == "act":
            return self.S.op("act", lambda: e.copy(out=out, in_=in_), W=[out], R=[in_])
        return self.S.op(eng, lambda: e.tensor_copy(out=out, in_=in_), W=[out], R=[in_])

    def rsqrt(self, out, in_, mulc, addc):
        self.act(out, in_, AF.Ln, bias=self.cbias(addc), scale=mulc)
        self.act(out, out, AF.Exp, scale=-0.5)

    def cbias(self, v):
        if v not in self.cb:
            i = len(self.cb)
            self.memset(self.CB[:, i:i + 1], float(v))
            self.cb[v] = i
        i = self.cb[v]
        return self.CB[:, i:i + 1]

    def colb(self, t, col0, cstride, nc_, n=128):
        W = int(list(t[:, :].ap)[0][0])
        return bass.AP(t[:, :].tensor, col0, [[W, 128], [cstride, nc_], [0, n]])

    def rowb(self, ap2d, nc_):
        a = list(ap2d.ap)
        return bass.AP(ap2d.tensor, int(ap2d.offset), [[int(a[0][0]), int(a[0][1])], [0, nc_], [int(a[1][0]), int(a[1][1])]])

    @staticmethod
    def v3(ap2d, nc_):
        return ap2d.rearrange("p (c n) -> p c n", c=nc_)

    def memset(self, ap, v, eng="dve"):
        e = self.S.eng[eng]
        return self.S.op(eng, lambda: e.memset(ap, v), W=[ap])

    def wload(self, dst, src, semname):
        q = "pool" if dst.dtype != src.dtype else "sp"
        return self.S.dma(q, dst, src, self.dsem(semname))

    def wv(self, nm, idx, c0, ncol, nk=8, p=128, blk=None, k0=0, nkb=None):
        if self.direct:
            w = self.wd[(nm, idx)]
            r0 = (0 if blk is None else blk * (nkb or nk) * p) + k0 * p
            return w[r0:r0 + nk * p, c0:c0 + ncol].rearrange("(c p) n -> p c n", p=p)
        assert k0 == 0
        off, kind, cols = self.wpack_off[(nm, idx)]
        NR = self.wpack_nr
        t = self.wpack.tensor
        if kind == "row":
            assert nk == NCORES and blk is None
            return bass.AP(t, off + c0, [[cols, p], [NR, nk], [1, ncol]])
        if kind == "col":
            assert ncol == 128 and c0 % 128 == 0
            return bass.AP(t, (c0 // 128) * NR + off, [[128, p], [p * 128, nk], [1, ncol]])
        return bass.AP(t, blk * NR + off + c0, [[cols, p], [p * cols, nk], [1, ncol]])

    def wrows(self, name, idx, r0, nr, c0, ncol):
        w = self.wd[(name, idx)]
        return w[r0:r0 + nr, c0:c0 + ncol].rearrange("(c p) n -> p c n", p=128)

    def C(self, name, bf=False):
        o, w = (CSTB_OFF if bf else CSTF_OFF)[name]
        return (self.CSTB if bf else self.CST)[:, o:o + w]

    def P(self, name, i0=0, n=None):
        off, _ = _sp_layout()
        o, w = off[name]
        n = w - i0 if n is None else n
        return self.SP[:, o + i0:o + i0 + n]

    def setup(self):
        nc = self.nc
        _, spw = _sp_layout()
        wf, wb = CSTF_NP.shape[1], CSTB_NP.shape[1]
        self.x_in = self.dram_in("x", [S, D])
        self.cstf_in = self.dram_in("cstf", [128, wf])
        self.cstb_in = self.dram_in("cstb", [128, wb])
        self.sp_in = self.dram_in("sp", [128, spw])
        self.fn_in = self.dram_in("final_norm", [1, D])
        self.out = nc.dram_tensor("out", [S, D], F32, kind="ExternalOutput").ap()
        sb = self.sb
        self.CST = sb("CST", [128, wf])
        self.CSTB = sb("CSTB", [128, wb], BF16)
        self.SP = sb("SP", [128, spw])
        self.S.dma("sp", self.CST[:], self.cstf_in[:, :], self.dsem("cst"))
        self.S.dma("sp", self.SP[:], self.sp_in[:, :], self.dsem("sp"))
        self.S.dma("pool", self.CSTB[:], self.cstb_in[:, :], self.dsem("cstb"))
        self.X = [sb(f"X{t}", [128, D]) for t in range(16)]
        for t in range(16):
            self.S.dma("sp", self.X[t][:], self.x_in[t * 128:(t + 1) * 128, :], self.dsem("xin"))
        self.PB = [self.ps(f"PB{i}", [128, 512]) for i in range(6)]
        self.PT = [self.ps(f"PT{i}", [128, 1024], BF16) for i in range(2)]
        self.CACT = sb("CACT", [128, 8])
        self.act(self.CACT[:], self.P("c"), AF.Silu)
        self.CACT16 = sb("CACT16", [128, 8], BF16)
        self.cp(self.CACT16[:], self.CACT[:])
        self.MODT = sb("MODT", [128, 48])
        self.WSC1 = sb("WSC1", [128, 8])
        self.WSC2 = sb("WSC2", [128, 8])
        self.NEGA = sb("NEGA", [128, 8])
        self.F = [sb(f"F{i}", [128, 512]) for i in range(6)]
        self.H = [sb(f"H{i}", [128, 512], BF16) for i in range(15)]
        self.W = [sb("W0", [128, 8, 512], BF16), sb("W1", [128, 8, 256], BF16)]
        self.XC = sb("XC", [128, 515])
        self.XN = self.W[1][:, 0:4, :].rearrange("p c t -> p (c t)")
        self.CB = sb("CB", [128, 8])
        self.SS = sb("SS", [128, 8])
        self.RS = sb("RS", [128, 8])
        self.BIG = sb("BIG", [128, 28672], BF16)
        self.BIG2 = sb("BIG2", [128, 8192], BF16)
        self.SF = sb("SF", [128, 8, 128])
        self.SB16 = sb("SB16", [128, 8, 128], BF16)
        self.HIST = sb("HIST", [128, 24, 3])
        self.WBA = sb("WBA", [128, 8, 16], BF16)
        self.WUS = sb("WUS", [64, 8, 128], BF16)
        self.BA = sb("BA", [128, 64])
        for nm in ("BETA", "LNB", "TMPA", "GS", "G", "GL", "GP", "EGP", "KD", "EGL"):
            setattr(self, nm, sb(nm, [128, 32]))
        self.GTS = sb("GTS", [8, 512])
        self.GPTS = sb("GPTS", [8, 512])
        self.phase = None
        for i in range(15):
            self.S.set_regions(f"H{i}", [0, 256, 512, 768])
        self.S.set_regions("F4", [0, 512, 1024, 1536])
        self.S.set_regions("W1", [0, 1024, 2048, 3072])
        print("sbuf bytes remaining after alloc:", nc.sbuf_bytes_remaining)

    def set_phase(self, ph):
        if self.phase == ph:
            return
        self.phase = ph
        B, B2 = self.BIG, self.BIG2
        if ph == "mixer":
            self.S.set_regions("BIG", [2 * v for v in (0, 8192, 16384, 20480, 24576)])
            self.S.set_regions("BIG2", [0, 8192])
            self.SBK = B[:, 0:8192].rearrange("p (c t) -> p c t", c=4)
            self.SBV = B[:, 8192:16384].rearrange("p (c t) -> p c t", c=16)
            self.OGT = B[:, 16384:20480].rearrange("p (c t) -> p c t", c=8)
            self.MTm = B[:, 20480:24576].rearrange("p (c t) -> p c t", c=8)
            self.OSB = B[0:64, 24576:28672].rearrange("p (c t) -> p c t", c=8)
            self.HT = B2[:, 0:4096].rearrange("p (c t) -> p c t", c=8)
            self.SBQP = B2[:, 4096:8192].rearrange("p (c t) -> p c t", c=8)
        else:
            self.S.set_regions("BIG", [0])
            self.S.set_regions("BIG2", [0])
            self.AT = B[:, :].rearrange("p (c t) -> p c t", c=28)
            self.H2T = B2[:, :].rearrange("p (c t) -> p c t", c=8)

    def layer_mod(self, l):
        AWB = self.W[0]
        for blk in range(12):
            self.wload(AWB[:], self.wv("ada_w", l, blk * 512, 512), "w0")
            for jj in range(4):
                j = blk * 4 + jj
                for dc in range(8):
                    self.mm(self.PB[0][:, j:j + 1], AWB[:, dc, jj * 128:(jj + 1) * 128],
                            self.CACT16[:, dc:dc + 1], start=(dc == 0), stop=(dc == 7))
        self.tt(self.MODT[:], self.PB[0][:, 0:48], self.P("ada_b", l * 48, 48), ALU.add)
        for (dst, nm, j0) in ((self.WSC1, "nmix", 8), (self.WSC2, "nffn", 32)):
            self.stt(dst[:], self.MODT[:, j0:j0 + 8], 1.0, self.P(nm, l * 8, 8), ALU.add, ALU.mult)
        self.act(self.NEGA[:], self.P("alog", l * 8, 8), AF.Exp)
        self.ts(self.NEGA[:], self.NEGA[:], -1.0, ALU.mult)
        self.dump(f"modT{l}", self.MODT[:], [128, 48])

    def norm_to_hT(self, tiles, HT, wsc, shc):
        n = len(tiles)
        for i, t in enumerate(tiles):
            self.act(self.XN[:], self.X[t][:], AF.Square, accum=self.SS[:, i:i + 1])
        self.rsqrt(self.RS[:, 0:n], self.SS[:, 0:n], 1.0 / D, EPS)
        for i, t in enumerate(tiles):
            self.ts(self.XN[:], self.X[t][:], self.RS[:, i:i + 1], ALU.mult)
            pt = self.PT[i % 2]
            for dc in range(8):
                self.tr(pt[:, dc * 128:(dc + 1) * 128], self.XN[:, dc * 128:(dc + 1) * 128], self.C("ident", True))
            for dc in range(8):
                self.act(HT[:, dc, i * 128:(i + 1) * 128], pt[:, dc * 128:(dc + 1) * 128], AF.Identity,
                         bias=shc[:, dc:dc + 1], scale=wsc[:, dc:dc + 1])

    def direct_weights(self, names_layers):
        shapes = {"ada_w": (D, 6 * D), "w_in": (D, INC), "w_up_dn": (D, D), "w_up_sb": (512, D), "w_out": (D, D),
                  "ffn_w1": (D, DFF), "ffn_w3": (D, DFF), "ffn_w2": (DFF, D),
                  "moe_w1": (NE * D, DFF), "moe_w3": (NE * D, DFF), "moe_w2": (NE * DFF, D)}
        for (nm, i) in names_layers:
            self.wd[(nm, i)] = self.dram_in(f"{nm}_{i}", list(shapes[nm]))

    def finish(self):
        S_ = self.S
        for k, sem in S_.dsem_by_key.items():
            v = S_.dcnt.get(id(sem), 0)
            if v:
                S_._wait("sp", (sem, v, "dma"))
        self.es.close()

    def proj_fm(self, l, col0, ncols, Wt, wofs, out_ps, semname):
        self.wload(Wt[:, :, wofs:wofs + ncols], self.wrows("w_in", l, 0, D, col0, ncols), semname)
        for dc in range(8):
            self.mm(out_ps, Wt[:, dc, wofs:wofs + ncols], self.HT[:, dc, :], start=(dc == 0), stop=(dc == 7))

    def sb_project(self, l, qt):
        W0, W1 = self.W
        t0 = qt * 512
        self.wload(W0[:], self.wv("w_in", l, C_SQ, 512), "w0")
        for cc in range(4):
            pb = self.PB[cc % 2]
            for dc in range(8):
                self.mm(pb[:, :], W0[:, dc, cc * 128:(cc + 1) * 128], self.HT[:, dc, :], start=(dc == 0), stop=(dc == 7))
            self.ts(self.SBQP[:, 2 * cc, :], pb[:, :], self.C("hm0"), ALU.mult)
            self.ts(self.SBQP[:, 2 * cc + 1, :], pb[:, :], self.C("hm1"), ALU.mult)
        self.wload(W0[:], self.wv("w_in", l, C_SK, 512), "w0")
        for cc in range(4):
            pb = self.PB[cc % 2]
            for dc in range(8):
                self.mm(pb[:, :], W0[:, dc, cc * 128:(cc + 1) * 128], self.HT[:, dc, :], start=(dc == 0), stop=(dc == 7))
            self.cp(self.SBK[:, cc, t0:t0 + 512], pb[:, :], eng="act")
        self.wload(W0[:], self.wv("w_in", l, C_SV, 512), "w0")
        for tt_ in range(4):
            pb = self.PB[tt_ % 2]
            for dc in range(8):
                self.mm(pb[:, :], self.HT[:, dc, tt_ * 128:(tt_ + 1) * 128], W0[:, dc, :], start=(dc == 0), stop=(dc == 7))
            self.cp(self.SBV[:, qt * 4 + tt_, :], pb[:, :])

    def sb_attend(self, qt):
        F, H, PB = self.F, self.H, self.PB
        W1f = self.W[1][:, :, :].rearrange("p c t -> p (c t)").bitcast(F32)
        sets = [
            dict(R=F[0], EX=F[1], SP32=F[2], T1=F[3], SPB=H[0], AT=H[1],
                 PZ=PB[0][:, :], PA=PB[1][:, :], PO=PB[2][:, :], POT=PB[3][0:64, :]),
            dict(R=F[4], EX=F[5], SP32=self.XC[:, 0:512], T1=W1f[:, 0:512], SPB=H[2], AT=H[3],
                 PZ=PB[4][:, :], PA=PB[5][:, :], PO=self.PT[0][:, :].bitcast(F32), POT=self.PT[1][:, :].bitcast(F32)[0:64, :]),
        ]
        utri = self.C("utri", True)
        nkt = 4 * qt + 4
        heads = list(self.heads)
        for hp in range(0, len(heads), 2):
            pair = [(heads[hp + i], sets[i]) for i in range(min(2, len(heads) - hp))]
            for h, st in pair:
                self.memset(st["R"][:, 0:512], 0.0)
            for idx, jb in enumerate(range(nkt - 1, -1, -1)):
                r = jb - 4 * qt
                steps = []
                for h, st in pair:
                    cc = h // 2
                    R, EX, SP32, T1, SPB, AT = st["R"], st["EX"], st["SP32"], st["T1"], st["SPB"], st["AT"]
                    full = lambda a: a[:] if a.ndim == 2 and a.shape[1] == 512 and hasattr(a, "tensor") else a
                    ops = []
                    ops.append(lambda st=st, cc=cc, h=h: self.mm(st["PZ"], self.SBK[:, cc, jb * 128:(jb + 1) * 128], self.SBQP[:, h, :]))
                    ops.append(lambda st=st: self.act(st["EX"][:, 0:512], st["PZ"], AF.Exp))
                    ops.append(lambda st=st: self.act(st["SP32"][:, 0:512], st["EX"][:, 0:512], AF.Ln, bias=self.cbias(1.0)))
                    if r >= 0:
                        def maskcast(st=st):
                            if r > 0:
                                self.memset(st["SPB"][:, 0:r * 128], 0.0)
                            self.tt(st["SPB"][:, r * 128:(r + 1) * 128], st["SP32"][:, r * 128:(r + 1) * 128], utri, ALU.mult)
                            if r < 3:
                                self.cp(st["SPB"][:, (r + 1) * 128:512], st["SP32"][:, (r + 1) * 128:512])
                        ops.append(maskcast)
                    else:
                        ops.append(lambda st=st: self.cp(st["SPB"][:, 0:512], st["SP32"][:, 0:512]))
                    ops.append(lambda st=st: self.mm(st["PA"], self.C("triincl", True), st["SPB"][:, 0:512]))
                    ops.append(lambda st=st: self.mm(st["PO"], self.C("ones", True), st["SPB"][:, 0:512]))
                    ops.append(lambda st=st: self.tt(st["T1"][:, 0:512], st["PZ"], st["R"][:, 0:512], ALU.subtract))
                    ops.append(lambda st=st: self.tt(st["T1"][:, 0:512], st["T1"][:, 0:512], st["PA"], ALU.subtract))
                    ops.append(lambda st=st: self.tt(st["R"][:, 0:512], st["R"][:, 0:512], st["PO"], ALU.add))
                    ops.append(lambda st=st: self.act(st["AT"][:, 0:512], st["T1"][:, 0:512], AF.Exp))
                    if r >= 0:
                        def maskat(st=st):
                            if r > 0:
                                self.memset(st["AT"][:, 0:r * 128], 0.0)
                            self.tt(st["AT"][:, r * 128:(r + 1) * 128], st["AT"][:, r * 128:(r + 1) * 128], utri, ALU.mult)
                        ops.append(maskat)
                    ops.append(lambda st=st, h=h: self.mm(st["POT"], self.SBV[:, jb, h * 64:(h + 1) * 64], st["AT"][:, 0:512],
                                                          start=(idx == 0), stop=(jb == 0), inc=True))
                    steps.append(ops)
                for k in range(max(len(o) for o in steps)):
                    for o in steps:
                        if k < len(o):
                            o[k]()
            for h, st in pair:
                self.cp(self.OSB[:, h, :], st["POT"])

    def dn_prep(self, l, qt):
        PB = self.PB
        self.wload(self.WBA[:], self.wv("w_in", l, C_B, 16), "wba")
        for tt_ in range(4):
            for dc in range(8):
                self.mm(PB[0][:, tt_ * 16:(tt_ + 1) * 16], self.HT[:, dc, tt_ * 128:(tt_ + 1) * 128], self.WBA[:, dc, :],
                        start=(dc == 0), stop=(dc == 7))
        self.cp(self.BA[:], PB[0][:, 0:64])
        for tt_ in range(4):
            b = self.BA[:, tt_ * 16:tt_ * 16 + 8]
            a = self.BA[:, tt_ * 16 + 8:tt_ * 16 + 16]
            s8 = slice(tt_ * 8, (tt_ + 1) * 8)
            self.act(self.BETA[:, s8], b, AF.Sigmoid)
            self.tt(self.TMPA[:, s8], a, self.P("dtb", l * 8, 8), ALU.add)
        self.act(self.LNB[:], self.BETA[:], AF.Ln)
        self.act(self.TMPA[:], self.TMPA[:], AF.Exp)
        self.act(self.TMPA[:], self.TMPA[:], AF.Ln, bias=self.cbias(1.0))
        for tt_ in range(4):
            s8 = slice(tt_ * 8, (tt_ + 1) * 8)
            self.tt(self.GS[:, s8], self.TMPA[:, s8], self.NEGA[:], ALU.mult)
        self.mm(PB[1][:, 0:32], self.C("tri"), self.GS[:])
        self.mm(PB[1][:, 32:64], self.C("ones"), self.GS[:])
        self.cp(self.G[:], PB[1][:, 0:32])
        self.cp(self.GL[:], PB[1][:, 32:64])
        self.tt(self.GP[:], self.G[:], self.LNB[:], ALU.add)
        self.act(self.EGP[:], self.GP[:], AF.Exp)
        self.tt(self.KD[:], self.GL[:], self.G[:], ALU.subtract)
        self.act(self.KD[:], self.KD[:], AF.Exp)
        self.act(self.EGL[:], self.GL[:], AF.Exp)
        for tt_ in range(4):
            s8 = slice(tt_ * 8, (tt_ + 1) * 8)
            self.tr(PB[2][0:8, tt_ * 128:(tt_ + 1) * 128], self.G[:, s8], self.C("ident"))
            self.tr(PB[3][0:8, tt_ * 128:(tt_ + 1) * 128], self.GP[:, s8], self.C("ident"))
        self.cp(self.GTS[:], PB[2][0:8, :])
        self.cp(self.GPTS[:], PB[3][0:8, :])

    def dn_qkv(self, l, qt, h, which, Wt, wofs):
        ch = {"q": 0, "k": 8, "v": 16}[which] + h
        pb = self.PB[4]
        for dc in range(8):
            self.mm(pb[:, :], Wt[:, dc, wofs:wofs + 128], self.HT[:, dc, :], start=(dc == 0), stop=(dc == 7))
        XC, Y = self.XC, self.F[1]
        if qt == 0:
            self.memset(XC[:, 0:3], 0.0)
        else:
            self.cp(XC[:, 0:3], self.HIST[:, ch, :])
        self.cp(XC[:, 3:515], pb[:, :], eng="act")
        if qt < 3:
            self.cp(self.HIST[:, ch, :], XC[:, 512:515])
        cw = lambda k: self.P("conv", l * 96 + k * 24 + ch, 1)
        self.ts(Y[:], XC[:, 0:512], cw(0), ALU.mult)
        for k in range(1, 4):
            self.stt(Y[:], XC[:, k:k + 512], cw(k), Y[:], ALU.mult, ALU.add)
        self.act(Y[:], Y[:], AF.Silu)
        return Y

    def l2n(self, out_bf, Y, mulc, addc):
        SQ = self.F[2]
        self.tt(SQ[:], Y[:], Y[:], ALU.mult)
        self.mm(self.PB[5][:, :], self.C("ones"), SQ[:])
        self.rsqrt(SQ[:], self.PB[5][:, :], mulc, addc)
        self.tt(out_bf, Y[:], SQ[:], ALU.mult)

    def dn_bufs(self, h):
        H = self.H
        if h % 2 == 0:
            return H[0], H[1], H[2], H[3]
        W1f = self.W[1][:, :, :].rearrange("p c t -> p (c t)")
        return tuple(W1f[:, i * 512:(i + 1) * 512] for i in range(4))

    def dn_phase1(self, l, qt, h):
        W0 = self.W[0]
        QN, KN, VC, ZS = self.dn_bufs(h)
        for i, c0 in enumerate((C_Q, C_K, C_V, C_Z)):
            self.wload(W0[:, :, i * 128:(i + 1) * 128], self.wv("w_in", l, c0 + h * 128, 128), "w0")
        yield
        for which, wofs, dst, mulc, addc in (("q", 0, QN, 128.0, 128.0 * EPS), ("k", 128, KN, 1.0, EPS), ("v", 256, VC, None, None)):
            ch = {"q": 0, "k": 8, "v": 16}[which] + h
            pb = self.PB[4]
            for dc in range(8):
                self.mm(pb[:, :], W0[:, dc, wofs:wofs + 128], self.HT[:, dc, :], start=(dc == 0), stop=(dc == 7))
            yield
            XC, Y = self.XC, self.F[1]
            if qt == 0:
                self.memset(XC[:, 0:3], 0.0)
            else:
                self.cp(XC[:, 0:3], self.HIST[:, ch, :])
            self.cp(XC[:, 3:515], pb[:, :], eng="act")
            if qt < 3:
                self.cp(self.HIST[:, ch, :], XC[:, 512:515])
            yield
            cw = lambda k: self.P("conv", l * 96 + k * 24 + ch, 1)
            self.ts(Y[:], XC[:, 0:512], cw(0), ALU.mult)
            for k in range(1, 4):
                self.stt(Y[:], XC[:, k:k + 512], cw(k), Y[:], ALU.mult, ALU.add)
            yield
            self.act(Y[:], Y[:], AF.Silu)
            yield
            if which == "v":
                self.cp(VC[:, 0:512], Y[:])
            else:
                SQ = self.F[2]
                self.tt(SQ[:], Y[:], Y[:], ALU.mult)
                self.mm(self.PB[5][:, :], self.C("ones"), SQ[:])
                yield
                self.rsqrt(SQ[:], self.PB[5][:, :], mulc, addc)
                self.tt(dst[:, 0:512], Y[:], SQ[:], ALU.mult)
            yield
        for dc in range(8):
            self.mm(self.PB[4][:, :], W0[:, dc, 384:512], self.HT[:, dc, :], start=(dc == 0), stop=(dc == 7))
        yield
        self.act(self.F[1][:], self.PB[4][:, :], AF.Silu)
        self.cp(ZS[:, 0:512], self.F[1][:])
        yield

    def bg_step(self, n=1):
        for _ in range(n):
            if self.bg is not None:
                try:
                    next(self.bg)
                except StopIteration:
                    self.bg = None

    def dn_head(self, l, qt, h, nxt=None):
        F, H, PB = self.F, self.H, self.PB
        GBC, GPBC, A, EGB = F[0], F[3], F[4], F[5]
        QN, KN, VC, ZS = self.dn_bufs(h)
        L, LT, QKT, RK, KDEC, RV, M, MT, QM, NWT, QD = [H[i] for i in range(4, 15)]
        Pm, VN = L, VC
        identb = self.C("ident", True)
        if self.bg_head == h:
            while self.bg is not None:
                self.bg_step()
        else:
            for _ in self.dn_phase1(l, qt, h):
                pass
        self.bg, self.bg_head = None, None
        self.mm(PB[4][:, :], self.C("sel")[0:8, h * 128:(h + 1) * 128], self.GTS[:])
        self.cp(GBC[:], PB[4][:, :])
        self.mm(PB[5][:, :], self.C("sel")[0:8, h * 128:(h + 1) * 128], self.GPTS[:])
        self.cp(GPBC[:], PB[5][:, :])
        cs = [slice(c * 128, (c + 1) * 128) for c in range(4)]
        sc = [slice(c * 8 + h, c * 8 + h + 1) for c in range(4)]
        for c in range(4):
            self.mm(PB[0][:, cs[c]], KN[:, cs[c]], KN[:, cs[c]])
            self.mm(PB[1][:, cs[c]], KN[:, cs[c]], QN[:, cs[c]])
        self.tt(self.v3(A[:, :], 4), self.v3(GBC[:, :], 4), self.colb(self.GP, h, 8, 4), ALU.subtract)
        self.tt(self.v3(A[:, :], 4), self.v3(A[:, :], 4), self.rowb(self.C("neg1"), 4), ALU.max)
        self.act(A[:], A[:], AF.Exp, scale=-1.0)
        self.tt(L[:], PB[0][:, :], A[:], ALU.mult)
        self.tt(self.v3(A[:, :], 4), self.v3(GPBC[:, :], 4), self.colb(self.G, h, 8, 4), ALU.subtract)
        self.tt(self.v3(A[:, :], 4), self.v3(A[:, :], 4), self.rowb(self.C("mask3"), 4), ALU.min)
        self.act(A[:], A[:], AF.Exp)
        self.tt(LT[:], PB[0][:, :], A[:], ALU.mult)
        self.tt(self.v3(A[:, :], 4), self.v3(GBC[:, :], 4), self.colb(self.G, h, 8, 4), ALU.subtract)
        self.tt(self.v3(A[:, :], 4), self.v3(A[:, :], 4), self.rowb(self.C("mask2"), 4), ALU.min)
        self.act(A[:], A[:], AF.Exp)
        self.tt(QKT[:], PB[1][:, :], A[:], ALU.mult)
        for c in range(4):
            self.tr(self.PT[0][:, cs[c]], KN[:, cs[c]], identb)
            self.tr(self.PT[1][:, cs[c]], VC[:, cs[c]], identb)
        self.tt(self.v3(RK[:, :], 4), self.v3(self.PT[0][:, 0:512], 4), self.colb(self.EGP, h, 8, 4), ALU.mult)
        self.tt(self.v3(KDEC[:, :], 4), self.v3(self.PT[0][:, 0:512], 4), self.colb(self.KD, h, 8, 4), ALU.mult)
        self.tt(self.v3(RV[:, :], 4), self.v3(self.PT[1][:, 0:512], 4), self.colb(self.BETA, h, 8, 4), ALU.mult)
        self.act(EGB[:], GBC[:], AF.Exp)
        self.tt(QD[:], QN[:, 0:512], EGB[:], ALU.mult)
        self.tt(self.v3(QM[:, :], 4), self.v3(L[:, :], 4), self.rowb(self.C("lm0", True), 4), ALU.mult)
        self.tt(self.v3(M[:, :], 4), self.rowb(identb, 4), self.v3(QM[:, :], 4), ALU.subtract)
        self.tt(self.v3(QM[:, :], 4), self.v3(LT[:, :], 4), self.rowb(self.C("lm0T", True), 4), ALU.mult)
        self.tt(self.v3(MT[:, :], 4), self.rowb(identb, 4), self.v3(QM[:, :], 4), ALU.subtract)
        if nxt is not None:
            self.bg, self.bg_head = self.dn_phase1(l, qt, nxt), nxt
        gs = [slice(0, 256), slice(256, 512)]
        PPb, PQb, PTb = [PB[2], PB[0]], [PB[3], PB[1]], [self.PT[0], self.PT[1]]
        for lv in range(1, 7):
            lm = self.C(f"lm{lv}", True)
            for g in range(2):
                for c in (2 * g, 2 * g + 1):
                    self.mm(PPb[g][:, cs[c]], LT[:, cs[c]], M[:, cs[c]])
            self.bg_step()
            for g in range(2):
                self.cp(Pm[:, gs[g]], PPb[g][:, gs[g]], eng="act")
            self.bg_step()
            for g in range(2):
                for c in (2 * g, 2 * g + 1):
                    self.mm(PQb[g][:, cs[c]], MT[:, cs[c]], Pm[:, cs[c]])
            self.bg_step()
            for g in range(2):
                self.tt(self.v3(QM[:, gs[g]], 2), self.v3(PQb[g][:, gs[g]], 2), self.rowb(lm, 2), ALU.mult)
            for g in range(2):
                self.tt(M[:, gs[g]], M[:, gs[g]], QM[:, gs[g]], ALU.subtract)
            self.bg_step()
            for g in range(2):
                for c in (2 * g, 2 * g + 1):
                    self.tr(PTb[g][:, cs[c]], QM[:, cs[c]], identb)
            self.bg_step()
            for g in range(2):
                self.tt(MT[:, gs[g]], MT[:, gs[g]], PTb[g][:, gs[g]], ALU.subtract)
        while self.bg is not None:
            self.bg_step()
        for c in range(4):
            self.mm(PB[2][:, cs[c]], RK[:, cs[c]], MT[:, cs[c]])
        self.ts(NWT[:], PB[2][:, :], -1.0, ALU.mult)
        if qt == 0:
            self.memset(self.SF[:, h, :], 0.0)
            self.memset(self.SB16[:, h, :], 0.0)
        S16 = self.SB16[:, h, :]
        for c in range(4):
            pv = PB[4][:, 0:128]
            self.mm(pv, MT[:, cs[c]], RV[:, cs[c]], start=True, stop=False)
            self.mm(pv, NWT[:, cs[c]], S16, start=False, stop=True)
            self.cp(VN[:, cs[c]], pv)
            self.mm(PB[5][:, cs[c]], S16, QD[:, cs[c]], start=True, stop=False)
            self.mm(PB[5][:, cs[c]], VN[:, cs[c]], QKT[:, cs[c]], start=False, stop=True)
            ps = PB[3][:, 0:128]
            self.mm(ps, KDEC[:, cs[c]], VN[:, cs[c]])
            self.stt(self.SF[:, h, :], self.SF[:, h, :], self.EGL[:, sc[c]], ps, ALU.mult, ALU.add)
            self.cp(S16, self.SF[:, h, :])
        OT = F[1]
        self.cp(OT[:], PB[5][:, :], eng="act")
        self.dump(f"OT{qt}_{h}", OT[:], [128, 512])
        SQ = F[2]
        self.tt(SQ[:], OT[:], OT[:], ALU.mult)
        self.mm(PB[4][:, :], self.C("ones"), SQ[:])
        self.rsqrt(SQ[:], PB[4][:, :], 1.0 / 128.0, EPS)
        self.tt(OT[:], OT[:], SQ[:], ALU.mult)
        self.stt(self.OGT[:, h, :], OT[:], self.P("onw", l, 1), ZS[:, 0:512], ALU.mult, ALU.mult)

    def merge_out(self, l, qt):
        F, PB = self.F, self.PB
        W0, W1 = self.W
        for oc in range(8):
            self.wload(W1[:, :, 0:128], self.wv("w_in", l, C_G + oc * 128, 128), "w1")
            self.wload(W1[:, :, 128:256], self.wv("w_in", l, C_G + D + oc * 128, 128), "w1")
            for dc in range(8):
                self.mm(PB[0][:, :], W1[:, dc, 0:128], self.HT[:, dc, :], start=(dc == 0), stop=(dc == 7))
            for dc in range(8):
                self.mm(PB[1][:, :], W1[:, dc, 128:256], self.HT[:, dc, :], start=(dc == 0), stop=(dc == 7))
            self.act(F[0][:], PB[0][:, :], AF.Sigmoid)
            self.act(F[1][:], PB[1][:, :], AF.Sigmoid)
            self.wload(W0[:, :, 0:128], self.wv("w_up_dn", l, oc * 128, 128), "w0")
            for h in range(8):
                self.mm(PB[2][:, :], W0[:, h, 0:128], self.OGT[:, h, :], start=(h == 0), stop=(h == 7))
            self.wload(self.WUS[:], self.wv("w_up_sb", l, oc * 128, 128, nk=8, p=64), "wus")
            for h in range(8):
                self.mm(PB[3][:, :], self.WUS[:, h, :], self.OSB[:, h, :], start=(h == 0), stop=(h == 7))
            self.tt(F[2][:], PB[2][:, :], F[0][:], ALU.mult)
            self.tt(F[3][:], PB[3][:, :], F[1][:], ALU.mult)
            self.tt(self.MTm[:, oc, :], F[2][:], F[3][:], ALU.add)
        self.proj_residual(("w_out", l), self.MTm, 8, [4 * qt + i for i in range(4)], 16)

    def proj_residual(self, wkey, actT, nk, tiles, modj, gate_ps=None, blk=None):
        F, PB, H = self.F, self.PB, self.H
        W0f = self.W[0][:, :, :].rearrange("p c t -> p (c t)")
        slotA = W0f[:, 0:nk * 128].rearrange("p (c t) -> p c t", c=nk)
        if 2 * nk * 128 <= 4096:
            slotB = W0f[:, 2048:2048 + nk * 128].rearrange("p (c t) -> p c t", c=nk)
            slots = [([slotA[:, k, :] for k in range(nk)], [(slotA, 0, nk)]),
                     ([slotB[:, k, :] for k in range(nk)], [(slotB, 0, nk)])]
        else:
            nh = (nk + 3) // 4
            bk = [H[k // 4][:, (k % 4) * 128:(k % 4 + 1) * 128] for k in range(nk)]
            bl = [(H[i][:, 0:min(4, nk - 4 * i) * 128].rearrange("p (c t) -> p c t", c=min(4, nk - 4 * i)), 4 * i,
                   min(4, nk - 4 * i)) for i in range(nh)]
            slots = [([slotA[:, k, :] for k in range(nk)], [(slotA, 0, nk)]), (bk, bl)]
        ng = len(tiles) // 4
        it = 0
        for oc in range(8):
            kaps, loads = slots[oc % 2]
            for (dst, k0, n) in loads:
                self.wload(dst, self.wv(wkey[0], wkey[1], oc * 128, 128, nk=n, blk=blk, k0=k0, nkb=nk), "w0")
            for g in range(ng):
                pa, pt, Fs = (PB[0], PB[1], F[0]) if it % 2 == 0 else (PB[2], PB[3], F[3])
                it += 1
                for k in range(nk):
                    self.mm(pa[:, :], kaps[k], actT[:, k, g * 512:(g + 1) * 512], start=(k == 0), stop=(k == nk - 1))
                self.ts(Fs[:], pa[:, :], self.MODT[:, modj + oc:modj + oc + 1], ALU.mult)
                if gate_ps is not None:
                    self.tt(Fs[:], Fs[:], gate_ps[g], ALU.mult)
                for i in range(4):
                    self.tr(pt[:, i * 128:(i + 1) * 128], Fs[:, i * 128:(i + 1) * 128], self.C("ident"))
                for i in range(4):
                    xs = self.X[tiles[g * 4 + i]][:, oc * 128:(oc + 1) * 128]
                    self.tt(xs, xs, pt[:, i * 128:(i + 1) * 128], ALU.add)

    def mixer(self, l):
        self.set_phase("mixer")
        for qt in range(4):
            self.norm_to_hT([4 * qt + i for i in range(4)], self.HT, self.WSC1, self.MODT[:, 0:8])
            self.sb_project(l, qt)
            if "sb" not in self.skip:
                self.sb_attend(qt)
            self.dn_prep(l, qt)
            for h in range(NH):
                if "dn" not in self.skip:
                    self.dn_head(l, qt, h, nxt=(h + 1 if h + 1 < NH else None))
            if "mo" not in self.skip:
                self.merge_out(l, qt)

    def swiglu_up(self, w1key, w3key, blk=None):
        F, PB, H = self.F, self.PB, self.H
        SFb = self.SF[:, :, :].rearrange("p c t -> p (c t)").bitcast(BF16).rearrange("p (c t) -> p c t", c=8)
        abufs = [self.W[1], SFb]
        bbufs = [H[0:4], H[4:8]]
        for f2 in range(DFF // 256):
            A, Bt = abufs[f2 % 2], bbufs[f2 % 2]
            self.wload(A[:, :, :], self.wv(w1key[0], w1key[1], f2 * 256, 256, blk=blk), "w1")
            for t in range(4):
                self.wload(Bt[t][:, :].rearrange("p (c n) -> p c n", c=2),
                           self.wv(w3key[0], w3key[1], f2 * 256, 256, nk=2, blk=blk, k0=2 * t, nkb=8), "w1")
            for sub in range(2):
                fc = 2 * f2 + sub
                for tg in range(2):
                    ts_ = slice(tg * 512, (tg + 1) * 512)
                    pa, pb_, Fs = (PB[2], PB[3], F[1]) if tg == 0 else (PB[4], PB[5], F[2])
                    for dc in range(8):
                        self.mm(pa[:, :], A[:, dc, sub * 128:(sub + 1) * 128], self.H2T[:, dc, ts_], start=(dc == 0), stop=(dc == 7))
                    for dc in range(8):
                        c0 = (dc % 2) * 256 + sub * 128
                        self.mm(pb_[:, :], Bt[dc // 2][:, c0:c0 + 128], self.H2T[:, dc, ts_], start=(dc == 0), stop=(dc == 7))
                    self.act(Fs[:], pa[:, :], AF.Silu)
                    self.tt(self.AT[:, fc, ts_], Fs[:], pb_[:, :], ALU.mult)

    def ffn_dense(self, l):
        self.set_phase("ffn")
        j = l // 2
        for hf in range(2):
            tiles = [8 * hf + i for i in range(8)]
            self.norm_to_hT(tiles, self.H2T, self.WSC2, self.MODT[:, 24:32])
            self.swiglu_up(("ffn_w1", j), ("ffn_w3", j))
            self.proj_residual(("ffn_w2", j), self.AT, DFF // 128, tiles, 40)

    def moe_alloc(self):
        sb = self.sb
        for nm in ("LG", "MX", "EE", "MK", "GATE"):
            setattr(self, nm, sb(nm, [128, 64]))
        self.DEN = sb("DEN", [128, 8])
        self.RW16 = sb("RW16", [128, 64], BF16)

    def ffn_moe(self, l):
        self.set_phase("ffn")
        j = l // 2
        F, PB = self.F, self.PB
        self.cp(self.RW16[:], self.P("rw", j * 64, 64))
        for hf in range(2):
            tiles = [8 * hf + i for i in range(8)]
            self.norm_to_hT(tiles, self.H2T, self.WSC2, self.MODT[:, 24:32])
            for i in range(8):
                for dc in range(8):
                    self.mm(PB[0][:, i * 8:(i + 1) * 8], self.H2T[:, dc, i * 128:(i + 1) * 128],
                            self.RW16[:, dc * 8:(dc + 1) * 8], start=(dc == 0), stop=(dc == 7))
            for i in range(8):
                s8 = slice(i * 8, (i + 1) * 8)
                self.tt(self.LG[:, s8], PB[0][:, s8], self.P("rb", j * 8, 8), ALU.add)
            for i in range(8):
                s8 = slice(i * 8, (i + 1) * 8)
                nc = self.nc
                self.S.op("dve", lambda o=self.MX[:, s8], a=self.LG[:, s8]: nc.vector.max(out=o, in_=a),
                          W=[self.MX[:, s8]], R=[self.LG[:, s8]])
            for i in range(8):
                s8 = slice(i * 8, (i + 1) * 8)
                self.ts(self.EE[:, s8], self.LG[:, s8], self.MX[:, i * 8:i * 8 + 1], ALU.subtract)
                self.ts(self.MK[:, s8], self.LG[:, s8], self.MX[:, i * 8 + 1:i * 8 + 2], ALU.is_ge)
                self.tt(self.DEN[:, i:i + 1], self.MX[:, i * 8 + 1:i * 8 + 2], self.MX[:, i * 8:i * 8 + 1], ALU.subtract)
            self.act(self.EE[:], self.EE[:], AF.Exp)
            self.tt(self.EE[:], self.EE[:], self.MK[:], ALU.mult)
            self.act(self.DEN[:], self.DEN[:], AF.Exp)
            self.ts(self.DEN[:], self.DEN[:], 1.0, ALU.add)
            nc = self.nc
            self.S.op("dve", lambda: nc.vector.reciprocal(out=self.DEN[:], in_=self.DEN[:]), W=[self.DEN[:]], R=[self.DEN[:]])
            for i in range(8):
                s8 = slice(i * 8, (i + 1) * 8)
                self.ts(self.GATE[:, s8], self.EE[:, s8], self.DEN[:, i:i + 1], ALU.mult)
            for i in range(8):
                pb = PB[1] if i < 4 else PB[2]
                self.tr(pb[0:8, (i % 4) * 128:(i % 4 + 1) * 128], self.GATE[:, i * 8:(i + 1) * 8], self.C("ident"))
            self.cp(self.GTS[:], PB[1][0:8, :])
            self.cp(self.GPTS[:], PB[2][0:8, :])
            self.dump(f"gate{l}_{hf}", self.GATE[:], [128, 64])
            for e in range(NE):
                self.swiglu_up(("moe_w1", j), ("moe_w3", j), blk=e)
                sel = self.C("sel")[0:8, e * 128:(e + 1) * 128]
                self.mm(PB[4][:, :], sel, self.GTS[:])
                self.mm(PB[5][:, :], sel, self.GPTS[:])
                self.proj_residual(("moe_w2", j), self.AT, DFF // 128, tiles, 40,
                                   gate_ps=[PB[4][:, :], PB[5][:, :]], blk=e)

    def final(self):
        F = self.F
        self.S.dma("sp", F[0][:], self.fn_in[0:1, 0:512].partition_broadcast(128))
        self.S.dma("sp", F[1][:], self.fn_in[0:1, 512:1024].partition_broadcast(128))
        for hf in range(2):
            tiles = [8 * hf + i for i in range(8)]
            for i, t in enumerate(tiles):
                self.act(self.XN[:], self.X[t][:], AF.Square, accum=self.SS[:, i:i + 1])
            self.rsqrt(self.RS[:, 0:8], self.SS[:, 0:8], 1.0 / D, EPS)
            for i, t in enumerate(tiles):
                for c in range(2):
                    xs = self.X[t][:, c * 512:(c + 1) * 512]
                    self.stt(xs, xs, self.RS[:, i:i + 1], F[c][:], ALU.mult, ALU.mult)
                self.S.dma("sp", self.out[t * 128:(t + 1) * 128, :], self.X[t][:])


W_SHAPES = {"ada_w": (D, 6 * D), "w_in": (D, INC), "w_up_dn": (D, D), "w_up_sb": (512, D), "w_out": (D, D),
            "ffn_w1": (D, DFF), "ffn_w3": (D, DFF), "ffn_w2": (DFF, D),
            "moe_w1": (NE * D, DFF), "moe_w3": (NE * D, DFF), "moe_w2": (NE * DFF, D)}


def weight_list(nlayers=DEPTH):
    out = []
    for l in range(nlayers):
        out += [("ada_w", l), ("w_in", l), ("w_up_dn", l), ("w_up_sb", l), ("w_out", l)]
        j = l // 2
        if l % 2 == 0:
            out += [("ffn_w1", j), ("ffn_w3", j), ("ffn_w2", j)]
        else:
            out += [("moe_w1", j), ("moe_w3", j), ("moe_w2", j)]
    return out


W_KIND = {"ada_w": "row", "w_in": "row", "w_up_dn": "row", "w_up_sb": "row", "w_out": "row",
          "ffn_w1": "row", "ffn_w3": "row", "ffn_w2": "col", "moe_w1": "exp", "moe_w3": "exp", "moe_w2": "exp"}
PACK_C = 2048


def pack_layout(wl):
    off, o = {}, 0
    for (nm, i) in wl:
        rows, cols = W_SHAPES[nm]
        off[(nm, i)] = (o, W_KIND[nm], cols)
        o += rows * cols // NCORES
    nr = (o + PACK_C * 128 - 1) // (PACK_C * 128) * (PACK_C * 128)
    return off, nr


def pack_rank(inputs, wl, r):
    off, nr = pack_layout(wl)
    buf = np.zeros((nr,), np.float32)
    for (nm, i) in wl:
        a = np.asarray(inputs[nm][i], dtype=np.float32)
        o, kind, cols = off[(nm, i)]
        if kind == "row":
            a2 = a.reshape(-1, a.shape[-1])
            rs = a2.shape[0] // NCORES
            seg = a2[r * rs:(r + 1) * rs]
        elif kind == "col":
            seg = a[:, r * 128:(r + 1) * 128]
        else:
            seg = a[r]
        buf[o:o + seg.size] = np.ascontiguousarray(seg).reshape(-1)
    return buf.reshape(-1, PACK_C)


def gather_weights(p, wl):
    nc = p.nc
    off, nr = pack_layout(wl)
    R = nr // PACK_C
    shard = p.dram_in("wpack", [R, PACK_C])
    src = nc.dram_tensor("wpack_c", [R, PACK_C], F32, kind="Internal").ap()
    dst = nc.dram_tensor("wpack_g", [NCORES * R, PACK_C], F32, kind="Internal").ap()
    p.S.dma("pool", src[:, :], shard[:, :])
    p.S.collective(lambda: nc.gpsimd.collective_compute(
        "AllGather", ALU.bypass, replica_groups=[list(range(NCORES))], ins=[src[:, :]], outs=[dst[:, :]]),
        dst[:, :], src[:, :])
    p.S.wait_all("pool", [dst[:, :]])
    p.wpack, p.wpack_off, p.wpack_nr = dst, off, nr


REPLICATE = True


def build_full(nlayers=DEPTH):
    p = Prog(direct=REPLICATE)
    p.setup()
    p.moe_alloc()
    wl = weight_list(nlayers)
    if REPLICATE:
        p.direct_weights(wl)
    else:
        gather_weights(p, wl)
    for l in range(nlayers):
        p.layer_mod(l)
        p.mixer(l)
        if l % 2 == 0:
            p.ffn_dense(l)
        else:
            p.ffn_moe(l)
    p.final()
    p.finish()
    return p


def make_in_maps(inputs, nlayers=DEPTH):
    wl = weight_list(nlayers)
    maps = []
    fn = np.ascontiguousarray(np.asarray(inputs["final_norm"], dtype=np.float32).reshape(1, D))
    shared = {}
    if REPLICATE:
        for (nm, i) in wl:
            a = np.asarray(inputs[nm][i], dtype=np.float32)
            shared[f"{nm}_{i}"] = a.reshape(-1, a.shape[-1])
    for b in range(NCORES):
        sp, _ = _small_params(inputs, b)
        m = {"x": np.ascontiguousarray(inputs["x"][b], dtype=np.float32), "cstf": CSTF_NP, "cstb": CSTB_NP,
             "sp": sp, "final_norm": fn}
        if REPLICATE:
            m.update(shared)
        else:
            m["wpack"] = pack_rank(inputs, wl, b)
        maps.append(m)
    return maps


def kernel(**inputs):
    inputs = {k: np.asarray(v) for k, v in inputs.items()}
    p = build_full()
    in_maps = make_in_maps(inputs)
    res = run_bass_kernel_spmd(p.nc, in_maps, core_ids=list(range(NCORES)))
    out = np.stack([np.asarray(r["out"], dtype=np.float32) for r in res.results], axis=0)
    return out.reshape(NCORES, S, D)
```

```python
import bisect
import contextlib
import numpy as np
import concourse.bass as bass
import concourse.mybir as mybir
from concourse.bass_utils import run_bass_kernel_spmd

F32 = mybir.dt.float32
BF16 = mybir.dt.bfloat16
AF = mybir.ActivationFunctionType
ALU = mybir.AluOpType

D = 1024
S = 2048
DEPTH = 4
NH = 8
DFF = 3584
NE = 8
INC = 7696
EPS = 1e-6
BIG = 30000.0
NCORES = 8
RELAX = False
C_Q, C_K, C_V, C_Z, C_B, C_A = 0, 1024, 2048, 3072, 4096, 4104
C_SQ, C_SK, C_SV, C_G = 4112, 4624, 5136, 5648


class Sched:
    ROT = 20000

    def __init__(self, nc, es):
        self.nc, self.es = nc, es
        self.eng = {"pe": nc.tensor, "act": nc.scalar, "dve": nc.vector, "pool": nc.gpsimd, "sp": nc.sync}
        self.sem, self.cnt, self.nsem = {}, {}, 0
        self.seen = {e: {} for e in self.eng}
        self.last_w = {}
        self.readers = {}
        self.pending = {e: [] for e in self.eng}
        self.semobj = {}
        for e in ("pe", "act", "dve", "pool"):
            self._rot(e)
        self.ninst = 0
        self.dcnt = {}
        self.regions = {}
        self.relax = RELAX
        self.dsem_by_key = {}
        self.carry = {}

    def _newsem(self, tag):
        self.nsem += 1
        s = self.es.enter_context(self.nc.semaphore(f"{tag}{self.nsem}"))
        self.semobj[id(s)] = s
        return s

    def _rot(self, e):
        self.sem[e] = self._newsem("s" + e)
        self.cnt[e] = 0

    @staticmethod
    def _dsize(dt):
        return 4 if dt == F32 else 2

    def keys(self, a):
        if isinstance(a, str):
            return [a]
        nm = a.tensor.name
        st = self.regions.get(nm)
        if st is None:
            return [nm]
        sz = self._dsize(a.dtype)
        lo = int(a.offset) * sz
        span = 0
        for (stride, count) in list(a.ap)[1:]:
            span += (int(count) - 1) * abs(int(stride))
        hi = lo + (span + 1) * sz
        i0 = bisect.bisect_right(st, lo) - 1
        out = []
        i = i0
        while i < len(st) and st[i] < hi:
            out.append(f"{nm}#{i}")
            i += 1
        return out

    def key(self, a):
        return self.keys(a)[0]

    def set_regions(self, nm, starts):
        old = [k for k in list(self.last_w.keys()) + list(self.readers.keys()) if k == nm or k.startswith(nm + "#")]
        tags = []
        for k in set(old):
            lw = self.last_w.pop(k, None)
            if lw is not None:
                tags.append(lw)
            for (sid, (val, eng_r)) in self.readers.pop(k, {}).items():
                tags.append((self.semobj[sid], val, eng_r))
        self.regions[nm] = list(starts)
        self.carry[nm] = tags

    def _wait(self, e, tag):
        sem, val, _ = tag
        d = self.seen[e]
        if d.get(id(sem), -1) >= val:
            return
        self.eng[e].wait_ge(sem, val)
        d[id(sem)] = val
        self.ninst += 1

    def op(self, e, fn, W=(), R=(), inc=True, dma_sem=None):
        wk = [k for a in W for k in self.keys(a)]
        rk = [k for a in R for k in self.keys(a)]
        for k in wk + rk:
            if "#" in k:
                for tag in self.carry.get(k.split("#")[0], ()):
                    self._wait(e, tag)
        for k in rk:
            lw = self.last_w.get(k)
            if lw is not None:
                self._wait(e, lw)
        for k in wk:
            lw = self.last_w.get(k)
            if lw is not None and not (lw[2] == e and (e == "pe" or self.relax)):
                self._wait(e, lw)
            for (sid, (val, eng_r)) in list(self.readers.get(k, {}).items()):
                if not (self.relax and eng_r == e):
                    self._wait(e, (self.semobj[sid], val, eng_r))
        ins = fn()
        self.ninst += 1
        if dma_sem is not None:
            sem, incv = dma_sem if len(dma_sem) == 2 else (dma_sem[0], 16)
            self.dcnt[id(sem)] = self.dcnt.get(id(sem), 0) + incv
            ins.then_inc(sem, incv)
            tag = (sem, self.dcnt[id(sem)], "dma")
        elif inc:
            self.cnt[e] += 1
            ins.then_inc(self.sem[e], 1)
            tag = (self.sem[e], self.cnt[e], e)
        else:
            self.pending[e].append((wk, rk))
            return ins
        items = self.pending[e] + [(wk, rk)]
        self.pending[e] = []
        for (wk_, rk_) in items:
            for k in rk_:
                self.readers.setdefault(k, {})[id(tag[0])] = (tag[1], tag[2])
            for k in wk_:
                self.last_w[k] = tag
                self.readers[k] = {}
        if dma_sem is None and self.cnt[e] >= self.ROT:
            self._rot(e)
        return ins

    def dma(self, q, out, in_, sem=None):
        eng = self.eng[q]
        k = self.key(out)
        if k not in self.dsem_by_key:
            self.dsem_by_key[k] = self._newsem("d")
        sem = self.dsem_by_key[k]
        return self.op(q, lambda: eng.dma_start(out=out, in_=in_), W=[out], R=[in_], dma_sem=(sem,))

    def collective(self, fn, out, in_):
        k = self.key(out)
        if k not in self.dsem_by_key:
            self.dsem_by_key[k] = self._newsem("g")
        return self.op("pool", fn, W=[out], R=[in_], dma_sem=(self.dsem_by_key[k], 1))

    def wait_all(self, e, keys):
        for a in keys:
            for k in self.keys(a):
                lw = self.last_w.get(k)
                if lw is not None:
                    self._wait(e, lw)


def _consts():
    i = np.arange(128)[:, None]
    j = np.arange(128)[None, :]
    f, b = {}, {}
    f["ident"] = (i == j).astype(np.float32)
    f["ones"] = np.ones((128, 128), np.float32)
    f["tri"] = (i <= j).astype(np.float32)
    f["neg1"] = np.where(i > j, 0.0, BIG).astype(np.float32)
    f["mask3"] = np.where(j > i, 0.0, -BIG).astype(np.float32)
    f["mask2"] = np.where(j >= i, 0.0, -BIG).astype(np.float32)
    sel = np.zeros((128, 1024), np.float32)
    for h in range(8):
        sel[h, h * 128:(h + 1) * 128] = 1.0
    f["sel"] = sel
    f["hm0"] = np.where(i < 64, 0.125, 0.0).astype(np.float32)[:, :1]
    f["hm1"] = np.where(i >= 64, 0.125, 0.0).astype(np.float32)[:, :1]
    b["ident"] = f["ident"]
    b["ones"] = f["ones"]
    for l in range(7):
        s = 1 << l
        m = ((i // s) % 2 == 1) & ((j // s) == (i // s) - 1)
        b[f"lm{l}"] = m.astype(np.float32)
    b["lm0T"] = b["lm0"].T.copy()
    b["triincl"] = (i >= j).astype(np.float32)
    b["utri"] = (i < j).astype(np.float32)

    def pack(c):
        off, cols, o = {}, [], 0
        for k, v in c.items():
            off[k] = (o, v.shape[1])
            o += v.shape[1]
            cols.append(v)
        return np.concatenate(cols, axis=1), off
    return pack(f), pack(b)


(CSTF_NP, CSTF_OFF), (CSTB_NP, CSTB_OFF) = _consts()


def _small_params(inp, b):
    p = {}
    L = DEPTH
    p["c"] = inp["c"][b].reshape(8, 128).T
    p["ada_b"] = inp["ada_b"].reshape(L, 48, 128).transpose(2, 0, 1).reshape(128, L * 48)
    p["nmix"] = inp["norm_mix"].reshape(L, 8, 128).transpose(2, 0, 1).reshape(128, L * 8)
    p["nffn"] = inp["norm_ffn"].reshape(L, 8, 128).transpose(2, 0, 1).reshape(128, L * 8)
    p["conv"] = inp["conv_w"].reshape(L, 4, 24, 128).transpose(3, 0, 1, 2).reshape(128, L * 96)
    p["alog"] = np.broadcast_to(inp["dn_a_log"].reshape(1, L * 8), (128, L * 8))
    p["dtb"] = np.broadcast_to(inp["dn_dt_bias"].reshape(1, L * 8), (128, L * 8))
    p["onw"] = inp["dn_out_norm"].T
    p["rb"] = np.broadcast_to(inp["router_b"].reshape(1, 16), (128, 16))
    p["rw"] = inp["router_w"].reshape(2, 8, 128, 8).transpose(2, 0, 1, 3).reshape(128, 2 * 8 * 8)
    off, cols, o = {}, [], 0
    for k, v in p.items():
        v = np.ascontiguousarray(v, dtype=np.float32)
        off[k] = (o, v.shape[1])
        o += v.shape[1]
        cols.append(v)
    return np.concatenate(cols, axis=1), off


_SP_OFF = None


def _sp_layout():
    global _SP_OFF
    if _SP_OFF is None:
        fake = {
            "c": np.zeros((8, 1024), np.float32), "ada_b": np.zeros((4, 6144), np.float32),
            "norm_mix": np.zeros((4, 1024), np.float32), "norm_ffn": np.zeros((4, 1024), np.float32),
            "conv_w": np.zeros((4, 4, 3072), np.float32), "dn_a_log": np.zeros((4, 8), np.float32),
            "dn_dt_bias": np.zeros((4, 8), np.float32), "dn_out_norm": np.zeros((4, 128), np.float32),
            "router_b": np.zeros((2, 8), np.float32), "router_w": np.zeros((2, 1024, 8), np.float32),
        }
        a, off = _small_params(fake, 0)
        _SP_OFF = (off, a.shape[1])
    return _SP_OFF


class Prog:
    def __init__(self, nlayers=DEPTH, direct=False, dumps=(), stop=None, layers=None):
        self.nl = nlayers
        self.direct = direct
        self.dumps = set(dumps)
        self.stop = stop
        self.nc = nc = bass.Bass("TRN2", target_bir_lowering=False)
        self.es = contextlib.ExitStack()
        self.S = Sched(nc, self.es)
        self.dump_names = []
        self.nsb = 0
        self.wd = {}
        self.dsems = {}
        self.heads = list(range(NH))
        self.cut = -1
        self.bg = None
        self.bg_head = None
        self.skip = ()
        self.cutn = 0
        self.cb = {}

    def sb(self, name, shape, dt=F32):
        return self.es.enter_context(self.nc.sbuf_tensor(name, list(shape), dt))

    def ps(self, name, shape, dt=F32):
        return self.es.enter_context(self.nc.psum_tensor(name, list(shape), dt))

    def dsem(self, name):
        if name not in self.dsems:
            self.dsems[name] = self.S._newsem("d" + name)
        return self.dsems[name]

    def dram_in(self, name, shape, dt=F32):
        return self.nc.dram_tensor(name, list(shape), dt, kind="ExternalInput").ap()

    def dump(self, name, ap, shape):
        if name not in self.dumps:
            return
        t = self.nc.dram_tensor("dbg_" + name, list(shape), ap.dtype, kind="ExternalOutput").ap()
        self.S.dma("sp", t, ap, self.dsem("dump"))
        self.dump_names.append("dbg_" + name)

    def mm(self, out, lhsT, rhs, start=True, stop=True, inc=None):
        nc = self.nc
        return self.S.op("pe", lambda: nc.tensor.matmul(out, lhsT, rhs, start=start, stop=stop),
                         W=[out], R=[lhsT, rhs], inc=(stop if inc is None else inc))

    def tr(self, out, in_, ident):
        nc = self.nc
        return self.S.op("pe", lambda: nc.tensor.transpose(out, in_, ident), W=[out], R=[in_, ident])

    def act(self, out, in_, func, bias=None, scale=None, accum=None, extraR=()):
        nc = self.nc
        kw = {}
        R = [in_] + list(extraR)
        W = [out]
        if bias is not None:
            kw["bias"] = bias
            if not isinstance(bias, (int, float)):
                R.append(bias)
        if scale is not None:
            kw["scale"] = scale
            if not isinstance(scale, (int, float)):
                R.append(scale)
        if accum is not None:
            kw["accum_out"] = accum
            W.append(accum)
        return self.S.op("act", lambda: nc.scalar.activation(out=out, in_=in_, func=func, **kw), W=W, R=R)

    def tt(self, out, a, b, op, eng="dve"):
        e = self.S.eng[eng]
        return self.S.op(eng, lambda: e.tensor_tensor(out=out, in0=a, in1=b, op=op), W=[out], R=[a, b])

    def ts(self, out, a, s1, op0, s2=None, op1=None, eng="dve"):
        e = self.S.eng[eng]
        R = [a] + [s for s in (s1, s2) if s is not None and not isinstance(s, (int, float))]
        kw = {}
        if op1 is not None:
            kw["op1"] = op1
        return self.S.op(eng, lambda: e.tensor_scalar(out=out, in0=a, scalar1=s1, scalar2=s2, op0=op0, **kw),
                         W=[out], R=R)

    def stt(self, out, a, s, b, op0, op1, eng="dve"):
        e = self.S.eng[eng]
        R = [a, b] + ([s] if not isinstance(s, (int, float)) else [])
        return self.S.op(eng, lambda: e.scalar_tensor_tensor(out=out, in0=a, scalar=s, in1=b, op0=op0, op1=op1),
                         W=[out], R=R)

    def cp(self, out, in_, eng="dve"):
        e = self.S.eng[eng]
        if eng == "act":
            return self.S.op("act", lambda: e.copy(out=out, in_=in_), W=[out], R=[in_])
        return self.S.op(eng, lambda: e.tensor_copy(out=out, in_=in_), W=[out], R=[in_])

    def rsqrt(self, out, in_, mulc, addc):
        self.act(out, in_, AF.Ln, bias=self.cbias(addc), scale=mulc)
        self.act(out, out, AF.Exp, scale=-0.5)

    def cbias(self, v):
        if v not in self.cb:
            i = len(self.cb)
            self.memset(self.CB[:, i:i + 1], float(v))
            self.cb[v] = i
        i = self.cb[v]
        return self.CB[:, i:i + 1]

    def colb(self, t, col0, cstride, nc_, n=128):
        W = int(list(t[:, :].ap)[0][0])
        return bass.AP(t[:, :].tensor, col0, [[W, 128], [cstride, nc_], [0, n]])

    def rowb(self, ap2d, nc_):
        a = list(ap2d.ap)
        return bass.AP(ap2d.tensor, int(ap2d.offset), [[int(a[0][0]), int(a[0][1])], [0, nc_], [int(a[1][0]), int(a[1][1])]])

    @staticmethod
    def v3(ap2d, nc_):
        return ap2d.rearrange("p (c n) -> p c n", c=nc_)

    def memset(self, ap, v, eng="dve"):
        e = self.S.eng[eng]
        return self.S.op(eng, lambda: e.memset(ap, v), W=[ap])

    def wload(self, dst, src, semname):
        q = "pool" if dst.dtype != src.dtype else "sp"
        return self.S.dma(q, dst, src, self.dsem(semname))

    def wv(self, nm, idx, c0, ncol, nk=8, p=128, blk=None, k0=0, nkb=None):
        if self.direct:
            w = self.wd[(nm, idx)]
            r0 = (0 if blk is None else blk * (nkb or nk) * p) + k0 * p
            return w[r0:r0 + nk * p, c0:c0 + ncol].rearrange("(c p) n -> p c n", p=p)
        assert k0 == 0
        off, kind, cols = self.wpack_off[(nm, idx)]
        NR = self.wpack_nr
        t = self.wpack.tensor
        if kind == "row":
            assert nk == NCORES and blk is None
            return bass.AP(t, off + c0, [[cols, p], [NR, nk], [1, ncol]])
        if kind == "col":
            assert ncol == 128 and c0 % 128 == 0
            return bass.AP(t, (c0 // 128) * NR + off, [[128, p], [p * 128, nk], [1, ncol]])
        return bass.AP(t, blk * NR + off + c0, [[cols, p], [p * cols, nk], [1, ncol]])

    def wrows(self, name, idx, r0, nr, c0, ncol):
        w = self.wd[(name, idx)]
        return w[r0:r0 + nr, c0:c0 + ncol].rearrange("(c p) n -> p c n", p=128)

    def C(self, name, bf=False):
        o, w = (CSTB_OFF if bf else CSTF_OFF)[name]
        return (self.CSTB if bf else self.CST)[:, o:o + w]

    def P(self, name, i0=0, n=None):
        off, _ = _sp_layout()
        o, w = off[name]
        n = w - i0 if n is None else n
        return self.SP[:, o + i0:o + i0 + n]

    def setup(self):
        nc = self.nc
        _, spw = _sp_layout()
        wf, wb = CSTF_NP.shape[1], CSTB_NP.shape[1]
        self.x_in = self.dram_in("x", [S, D])
        self.cstf_in = self.dram_in("cstf", [128, wf])
        self.cstb_in = self.dram_in("cstb", [128, wb])
        self.sp_in = self.dram_in("sp", [128, spw])
        self.fn_in = self.dram_in("final_norm", [1, D])
        self.out = nc.dram_tensor("out", [S, D], F32, kind="ExternalOutput").ap()
        sb = self.sb
        self.CST = sb("CST", [128, wf])
        self.CSTB = sb("CSTB", [128, wb], BF16)
        self.SP = sb("SP", [128, spw])
        self.S.dma("sp", self.CST[:], self.cstf_in[:, :], self.dsem("cst"))
        self.S.dma("sp", self.SP[:], self.sp_in[:, :], self.dsem("sp"))
        self.S.dma("pool", self.CSTB[:], self.cstb_in[:, :], self.dsem("cstb"))
        self.X = [sb(f"X{t}", [128, D]) for t in range(16)]
        for t in range(16):
            self.S.dma("sp", self.X[t][:], self.x_in[t * 128:(t + 1) * 128, :], self.dsem("xin"))
        self.PB = [self.ps(f"PB{i}", [128, 512]) for i in range(6)]
        self.PT = [self.ps(f"PT{i}", [128, 1024], BF16) for i in range(2)]
        self.CACT = sb("CACT", [128, 8])
        self.act(self.CACT[:], self.P("c"), AF.Silu)
        self.CACT16 = sb("CACT16", [128, 8], BF16)
        self.cp(self.CACT16[:], self.CACT[:])
        self.MODT = sb("MODT", [128, 48])
        self.WSC1 = sb("WSC1", [128, 8])
        self.WSC2 = sb("WSC2", [128, 8])
        self.NEGA = sb("NEGA", [128, 8])
        self.F = [sb(f"F{i}", [128, 512]) for i in range(6)]
        self.H = [sb(f"H{i}", [128, 512], BF16) for i in range(15)]
        self.W = [sb("W0", [128, 8, 512], BF16), sb("W1", [128, 8, 256], BF16)]
        self.XC = sb("XC", [128, 515])
        self.XN = self.W[1][:, 0:4, :].rearrange("p c t -> p (c t)")
        self.CB = sb("CB", [128, 8])
        self.SS = sb("SS", [128, 8])
        self.RS = sb("RS", [128, 8])
        self.BIG = sb("BIG", [128, 28672], BF16)
        self.BIG2 = sb("BIG2", [128, 8192], BF16)
        self.SF = sb("SF", [128, 8, 128])
        self.SB16 = sb("SB16", [128, 8, 128], BF16)
        self.HIST = sb("HIST", [128, 24, 3])
        self.WBA = sb("WBA", [128, 8, 16], BF16)
        self.WUS = sb("WUS", [64, 8, 128], BF16)
        self.BA = sb("BA", [128, 64])
        for nm in ("BETA", "LNB", "TMPA", "GS", "G", "GL", "GP", "EGP", "KD", "EGL"):
            setattr(self, nm, sb(nm, [128, 32]))
        self.GTS = sb("GTS", [8, 512])
        self.GPTS = sb("GPTS", [8, 512])
        self.phase = None
        for i in range(15):
            self.S.set_regions(f"H{i}", [0, 256, 512, 768])
        self.S.set_regions("F4", [0, 512, 1024, 1536])
        self.S.set_regions("W1", [0, 1024, 2048, 3072])
        print("sbuf bytes remaining after alloc:", nc.sbuf_bytes_remaining)

    def set_phase(self, ph):
        if self.phase == ph:
            return
        self.phase = ph
        B, B2 = self.BIG, self.BIG2
        if ph == "mixer":
            self.S.set_regions("BIG", [2 * v for v in (0, 8192, 16384, 20480, 24576)])
            self.S.set_regions("BIG2", [0, 8192])
            self.SBK = B[:, 0:8192].rearrange("p (c t) -> p c t", c=4)
            self.SBV = B[:, 8192:16384].rearrange("p (c t) -> p c t", c=16)
            self.OGT = B[:, 16384:20480].rearrange("p (c t) -> p c t", c=8)
            self.MTm = B[:, 20480:24576].rearrange("p (c t) -> p c t", c=8)
            self.OSB = B[0:64, 24576:28672].rearrange("p (c t) -> p c t", c=8)
            self.HT = B2[:, 0:4096].rearrange("p (c t) -> p c t", c=8)
            self.SBQP = B2[:, 4096:8192].rearrange("p (c t) -> p c t", c=8)
        else:
            self.S.set_regions("BIG", [0])
            self.S.set_regions("BIG2", [0])
            self.AT = B[:, :].rearrange("p (c t) -> p c t", c=28)
            self.H2T = B2[:, :].rearrange("p (c t) -> p c t", c=8)

    def layer_mod(self, l):
        AWB = self.W[0]
        for blk in range(12):
            self.wload(AWB[:], self.wv("ada_w", l, blk * 512, 512), "w0")
            for jj in range(4):
                j = blk * 4 + jj
                for dc in range(8):
                    self.mm(self.PB[0][:, j:j + 1], AWB[:, dc, jj * 128:(jj + 1) * 128],
                            self.CACT16[:, dc:dc + 1], start=(dc == 0), stop=(dc == 7))
        self.tt(self.MODT[:], self.PB[0][:, 0:48], self.P("ada_b", l * 48, 48), ALU.add)
        for (dst, nm, j0) in ((self.WSC1, "nmix", 8), (self.WSC2, "nffn", 32)):
            self.stt(dst[:], self.MODT[:, j0:j0 + 8], 1.0, self.P(nm, l * 8, 8), ALU.add, ALU.mult)
        self.act(self.NEGA[:], self.P("alog", l * 8, 8), AF.Exp)
        self.ts(self.NEGA[:], self.NEGA[:], -1.0, ALU.mult)
        self.dump(f"modT{l}", self.MODT[:], [128, 48])

    def norm_to_hT(self, tiles, HT, wsc, shc):
        n = len(tiles)
        for i, t in enumerate(tiles):
            self.act(self.XN[:], self.X[t][:], AF.Square, accum=self.SS[:, i:i + 1])
        self.rsqrt(self.RS[:, 0:n], self.SS[:, 0:n], 1.0 / D, EPS)
        for i, t in enumerate(tiles):
            self.ts(self.XN[:], self.X[t][:], self.RS[:, i:i + 1], ALU.mult)
            pt = self.PT[i % 2]
            for dc in range(8):
                self.tr(pt[:, dc * 128:(dc + 1) * 128], self.XN[:, dc * 128:(dc + 1) * 128], self.C("ident", True))
            for dc in range(8):
                self.act(HT[:, dc, i * 128:(i + 1) * 128], pt[:, dc * 128:(dc + 1) * 128], AF.Identity,
                         bias=shc[:, dc:dc + 1], scale=wsc[:, dc:dc + 1])

    def direct_weights(self, names_layers):
        shapes = {"ada_w": (D, 6 * D), "w_in": (D, INC), "w_up_dn": (D, D), "w_up_sb": (512, D), "w_out": (D, D),
                  "ffn_w1": (D, DFF), "ffn_w3": (D, DFF), "ffn_w2": (DFF, D),
                  "moe_w1": (NE * D, DFF), "moe_w3": (NE * D, DFF), "moe_w2": (NE * DFF, D)}
        for (nm, i) in names_layers:
            self.wd[(nm, i)] = self.dram_in(f"{nm}_{i}", list(shapes[nm]))

    def finish(self):
        S_ = self.S
        for k, sem in S_.dsem_by_key.items():
            v = S_.dcnt.get(id(sem), 0)
            if v:
                S_._wait("sp", (sem, v, "dma"))
        self.es.close()

    def proj_fm(self, l, col0, ncols, Wt, wofs, out_ps, semname):
        self.wload(Wt[:, :, wofs:wofs + ncols], self.wrows("w_in", l, 0, D, col0, ncols), semname)
        for dc in range(8):
            self.mm(out_ps, Wt[:, dc, wofs:wofs + ncols], self.HT[:, dc, :], start=(dc == 0), stop=(dc == 7))

    def sb_project(self, l, qt):
        W0, W1 = self.W
        t0 = qt * 512
        self.wload(W0[:], self.wv("w_in", l, C_SQ, 512), "w0")
        for cc in range(4):
            pb = self.PB[cc % 2]
            for dc in range(8):
                self.mm(pb[:, :], W0[:, dc, cc * 128:(cc + 1) * 128], self.HT[:, dc, :], start=(dc == 0), stop=(dc == 7))
            self.ts(self.SBQP[:, 2 * cc, :], pb[:, :], self.C("hm0"), ALU.mult)
            self.ts(self.SBQP[:, 2 * cc + 1, :], pb[:, :], self.C("hm1"), ALU.mult)
        self.wload(W0[:], self.wv("w_in", l, C_SK, 512), "w0")
        for cc in range(4):
            pb = self.PB[cc % 2]
            for dc in range(8):
                self.mm(pb[:, :], W0[:, dc, cc * 128:(cc + 1) * 128], self.HT[:, dc, :], start=(dc == 0), stop=(dc == 7))
            self.cp(self.SBK[:, cc, t0:t0 + 512], pb[:, :], eng="act")
        self.wload(W0[:], self.wv("w_in", l, C_SV, 512), "w0")
        for tt_ in range(4):
            pb = self.PB[tt_ % 2]
            for dc in range(8):
                self.mm(pb[:, :], self.HT[:, dc, tt_ * 128:(tt_ + 1) * 128], W0[:, dc, :], start=(dc == 0), stop=(dc == 7))
            self.cp(self.SBV[:, qt * 4 + tt_, :], pb[:, :])

    def sb_attend(self, qt):
        F, H, PB = self.F, self.H, self.PB
        W1f = self.W[1][:, :, :].rearrange("p c t -> p (c t)").bitcast(F32)
        sets = [
            dict(EX=F[1], SP32=F[2], E2=F[3], SPB=H[0], AT=H[1], RS=H[4],
                 PZ=PB[0][:, :], PA=PB[1][:, :], POT=PB[3][0:64, :]),
            dict(EX=F[5], SP32=self.XC[:, 0:512], E2=W1f[:, 0:512], SPB=H[2], AT=H[3], RS=H[5],
                 PZ=PB[4][:, :], PA=PB[5][:, :], POT=self.PT[1][:, :].bitcast(F32)[0:64, :]),
        ]
        utri = self.C("utri", True)
        nkt = 4 * qt + 4
        heads = list(self.heads)
        for hp in range(0, len(heads), 2):
            pair = [(heads[hp + i], sets[i]) for i in range(min(2, len(heads) - hp))]
            for idx, jb in enumerate(range(nkt - 1, -1, -1)):
                r = jb - 4 * qt
                steps = []
                for h, st in pair:
                    cc = h // 2
                    ops = []
                    ops.append(lambda st=st, cc=cc, h=h: self.mm(st["PZ"], self.SBK[:, cc, jb * 128:(jb + 1) * 128], self.SBQP[:, h, :]))
                    ops.append(lambda st=st: self.act(st["EX"][:, 0:512], st["PZ"], AF.Exp))
                    ops.append(lambda st=st: self.act(st["SP32"][:, 0:512], st["EX"][:, 0:512], AF.Ln, bias=self.cbias(1.0)))
                    if r >= 0:
                        def maskcast(st=st):
                            if r > 0:
                                self.memset(st["SPB"][:, 0:r * 128], 0.0)
                            self.tt(st["SPB"][:, r * 128:(r + 1) * 128], st["SP32"][:, r * 128:(r + 1) * 128], utri, ALU.mult)
                            if r < 3:
                                self.cp(st["SPB"][:, (r + 1) * 128:512], st["SP32"][:, (r + 1) * 128:512])
                        ops.append(maskcast)
                    else:
                        ops.append(lambda st=st: self.cp(st["SPB"][:, 0:512], st["SP32"][:, 0:512]))

                    def suffix(st=st):
                        self.mm(st["PA"], self.C("triincl", True), st["SPB"][:, 0:512], start=True, stop=(idx == 0))
                        if idx > 0:
                            self.mm(st["PA"], self.C("ones", True), st["RS"][:, 0:512], start=False, stop=True)
                    ops.append(suffix)
                    ops.append(lambda st=st: self.act(st["E2"][:, 0:512], st["PA"], AF.Exp, scale=-1.0))

                    def attn(st=st):
                        self.tt(st["AT"][:, 0:512], st["EX"][:, 0:512], st["E2"][:, 0:512], ALU.mult)
                        if r >= 0:
                            if r > 0:
                                self.memset(st["AT"][:, 0:r * 128], 0.0)
                            self.tt(st["AT"][:, r * 128:(r + 1) * 128], st["AT"][:, r * 128:(r + 1) * 128], utri, ALU.mult)
                    ops.append(attn)
                    ops.append(lambda st=st, h=h: self.mm(st["POT"], self.SBV[:, jb, h * 64:(h + 1) * 64], st["AT"][:, 0:512],
                                                          start=(idx == 0), stop=(jb == 0), inc=True))
                    if jb > 0:
                        def runsum(st=st):
                            if idx == 0:
                                self.cp(st["RS"][:, 0:512], st["SPB"][:, 0:512])
                            else:
                                self.tt(st["RS"][:, 0:512], st["RS"][:, 0:512], st["SPB"][:, 0:512], ALU.add)
                        ops.append(runsum)
                    steps.append(ops)
                for k in range(max(len(o) for o in steps)):
                    for o in steps:
                        if k < len(o):
                            o[k]()
            for h, st in pair:
                self.cp(self.OSB[:, h, :], st["POT"])

    def dn_prep(self, l, qt):
        PB = self.PB
        self.wload(self.WBA[:], self.wv("w_in", l, C_B, 16), "wba")
        for tt_ in range(4):
            for dc in range(8):
                self.mm(PB[0][:, tt_ * 16:(tt_ + 1) * 16], self.HT[:, dc, tt_ * 128:(tt_ + 1) * 128], self.WBA[:, dc, :],
                        start=(dc == 0), stop=(dc == 7))
        self.cp(self.BA[:], PB[0][:, 0:64])
        for tt_ in range(4):
            b = self.BA[:, tt_ * 16:tt_ * 16 + 8]
            a = self.BA[:, tt_ * 16 + 8:tt_ * 16 + 16]
            s8 = slice(tt_ * 8, (tt_ + 1) * 8)
            self.act(self.BETA[:, s8], b, AF.Sigmoid)
            self.tt(self.TMPA[:, s8], a, self.P("dtb", l * 8, 8), ALU.add)
        self.act(self.LNB[:], self.BETA[:], AF.Ln)
        self.act(self.TMPA[:], self.TMPA[:], AF.Exp)
        self.act(self.TMPA[:], self.TMPA[:], AF.Ln, bias=self.cbias(1.0))
        for tt_ in range(4):
            s8 = slice(tt_ * 8, (tt_ + 1) * 8)
            self.tt(self.GS[:, s8], self.TMPA[:, s8], self.NEGA[:], ALU.mult)
        self.mm(PB[1][:, 0:32], self.C("tri"), self.GS[:])
        self.mm(PB[1][:, 32:64], self.C("ones"), self.GS[:])
        self.cp(self.G[:], PB[1][:, 0:32])
        self.cp(self.GL[:], PB[1][:, 32:64])
        self.tt(self.GP[:], self.G[:], self.LNB[:], ALU.add)
        self.act(self.EGP[:], self.GP[:], AF.Exp)
        self.tt(self.KD[:], self.GL[:], self.G[:], ALU.subtract)
        self.act(self.KD[:], self.KD[:], AF.Exp)
        self.act(self.EGL[:], self.GL[:], AF.Exp)
        for tt_ in range(4):
            s8 = slice(tt_ * 8, (tt_ + 1) * 8)
            self.tr(PB[2][0:8, tt_ * 128:(tt_ + 1) * 128], self.G[:, s8], self.C("ident"))
            self.tr(PB[3][0:8, tt_ * 128:(tt_ + 1) * 128], self.GP[:, s8], self.C("ident"))
        self.cp(self.GTS[:], PB[2][0:8, :])
        self.cp(self.GPTS[:], PB[3][0:8, :])

    def dn_qkv(self, l, qt, h, which, Wt, wofs):
        ch = {"q": 0, "k": 8, "v": 16}[which] + h
        pb = self.PB[4]
        for dc in range(8):
            self.mm(pb[:, :], Wt[:, dc, wofs:wofs + 128], self.HT[:, dc, :], start=(dc == 0), stop=(dc == 7))
        XC, Y = self.XC, self.F[1]
        if qt == 0:
            self.memset(XC[:, 0:3], 0.0)
        else:
            self.cp(XC[:, 0:3], self.HIST[:, ch, :])
        self.cp(XC[:, 3:515], pb[:, :], eng="act")
        if qt < 3:
            self.cp(self.HIST[:, ch, :], XC[:, 512:515])
        cw = lambda k: self.P("conv", l * 96 + k * 24 + ch, 1)
        self.ts(Y[:], XC[:, 0:512], cw(0), ALU.mult)
        for k in range(1, 4):
            self.stt(Y[:], XC[:, k:k + 512], cw(k), Y[:], ALU.mult, ALU.add)
        self.act(Y[:], Y[:], AF.Silu)
        return Y

    def l2n(self, out_bf, Y, mulc, addc):
        SQ = self.F[2]
        self.tt(SQ[:], Y[:], Y[:], ALU.mult)
        self.mm(self.PB[5][:, :], self.C("ones"), SQ[:])
        self.rsqrt(SQ[:], self.PB[5][:, :], mulc, addc)
        self.tt(out_bf, Y[:], SQ[:], ALU.mult)

    def dn_bufs(self, h):
        H = self.H
        if h % 2 == 0:
            return H[0], H[1], H[2], H[3]
        W1f = self.W[1][:, :, :].rearrange("p c t -> p (c t)")
        return tuple(W1f[:, i * 512:(i + 1) * 512] for i in range(4))

    def dn_phase1(self, l, qt, h):
        W0 = self.W[0]
        QN, KN, VC, ZS = self.dn_bufs(h)
        for i, c0 in enumerate((C_Q, C_K, C_V, C_Z)):
            self.wload(W0[:, :, i * 128:(i + 1) * 128], self.wv("w_in", l, c0 + h * 128, 128), "w0")
        yield
        for which, wofs, dst, mulc, addc in (("q", 0, QN, 128.0, 128.0 * EPS), ("k", 128, KN, 1.0, EPS), ("v", 256, VC, None, None)):
            ch = {"q": 0, "k": 8, "v": 16}[which] + h
            pb = self.PB[4]
            for dc in range(8):
                self.mm(pb[:, :], W0[:, dc, wofs:wofs + 128], self.HT[:, dc, :], start=(dc == 0), stop=(dc == 7))
            yield
            XC, Y = self.XC, self.F[1]
            if qt == 0:
                self.memset(XC[:, 0:3], 0.0)
            else:
                self.cp(XC[:, 0:3], self.HIST[:, ch, :])
            self.cp(XC[:, 3:515], pb[:, :], eng="act")
            if qt < 3:
                self.cp(self.HIST[:, ch, :], XC[:, 512:515])
            yield
            cw = lambda k: self.P("conv", l * 96 + k * 24 + ch, 1)
            self.ts(Y[:], XC[:, 0:512], cw(0), ALU.mult)
            for k in range(1, 4):
                self.stt(Y[:], XC[:, k:k + 512], cw(k), Y[:], ALU.mult, ALU.add)
            yield
            self.act(Y[:], Y[:], AF.Silu)
            yield
            if which == "v":
                self.cp(VC[:, 0:512], Y[:])
            else:
                SQ = self.F[2]
                self.tt(SQ[:], Y[:], Y[:], ALU.mult)
                self.mm(self.PB[5][:, :], self.C("ones"), SQ[:])
                yield
                self.rsqrt(SQ[:], self.PB[5][:, :], mulc, addc)
                self.tt(dst[:, 0:512], Y[:], SQ[:], ALU.mult)
            yield
        for dc in range(8):
            self.mm(self.PB[4][:, :], W0[:, dc, 384:512], self.HT[:, dc, :], start=(dc == 0), stop=(dc == 7))
        yield
        self.act(self.F[1][:], self.PB[4][:, :], AF.Silu)
        self.cp(ZS[:, 0:512], self.F[1][:])
        yield

    def bg_step(self, n=1):
        for _ in range(n):
            if self.bg is not None:
                try:
                    next(self.bg)
                except StopIteration:
                    self.bg = None

    def dn_head(self, l, qt, h, nxt=None):
        F, H, PB = self.F, self.H, self.PB
        GBC, GPBC, A, EGB = F[0], F[3], F[4], F[5]
        QN, KN, VC, ZS = self.dn_bufs(h)
        L, LT, QKT, RK, KDEC, RV, M, MT, QM, NWT, QD = [H[i] for i in range(4, 15)]
        Pm, VN = L, VC
        identb = self.C("ident", True)
        if self.bg_head == h:
            while self.bg is not None:
                self.bg_step()
        else:
            for _ in self.dn_phase1(l, qt, h):
                pass
        self.bg, self.bg_head = None, None
        self.mm(PB[4][:, :], self.C("sel")[0:8, h * 128:(h + 1) * 128], self.GTS[:])
        self.cp(GBC[:], PB[4][:, :])
        self.mm(PB[5][:, :], self.C("sel")[0:8, h * 128:(h + 1) * 128], self.GPTS[:])
        self.cp(GPBC[:], PB[5][:, :])
        cs = [slice(c * 128, (c + 1) * 128) for c in range(4)]
        sc = [slice(c * 8 + h, c * 8 + h + 1) for c in range(4)]
        for c in range(4):
            self.mm(PB[0][:, cs[c]], KN[:, cs[c]], KN[:, cs[c]])
            self.mm(PB[1][:, cs[c]], KN[:, cs[c]], QN[:, cs[c]])
        self.tt(self.v3(A[:, :], 4), self.v3(GBC[:, :], 4), self.colb(self.GP, h, 8, 4), ALU.subtract)
        self.tt(self.v3(A[:, :], 4), self.v3(A[:, :], 4), self.rowb(self.C("neg1"), 4), ALU.max)
        self.act(A[:], A[:], AF.Exp, scale=-1.0)
        self.tt(L[:], PB[0][:, :], A[:], ALU.mult)
        self.tt(self.v3(A[:, :], 4), self.v3(GPBC[:, :], 4), self.colb(self.G, h, 8, 4), ALU.subtract)
        self.tt(self.v3(A[:, :], 4), self.v3(A[:, :], 4), self.rowb(self.C("mask3"), 4), ALU.min)
        self.act(A[:], A[:], AF.Exp)
        self.tt(LT[:], PB[0][:, :], A[:], ALU.mult)
        self.tt(self.v3(A[:, :], 4), self.v3(GBC[:, :], 4), self.colb(self.G, h, 8, 4), ALU.subtract)
        self.tt(self.v3(A[:, :], 4), self.v3(A[:, :], 4), self.rowb(self.C("mask2"), 4), ALU.min)
        self.act(A[:], A[:], AF.Exp)
        self.tt(QKT[:], PB[1][:, :], A[:], ALU.mult)
        for c in range(4):
            self.tr(self.PT[0][:, cs[c]], KN[:, cs[c]], identb)
            self.tr(self.PT[1][:, cs[c]], VC[:, cs[c]], identb)
        self.tt(self.v3(RK[:, :], 4), self.v3(self.PT[0][:, 0:512], 4), self.colb(self.EGP, h, 8, 4), ALU.mult)
        self.tt(self.v3(KDEC[:, :], 4), self.v3(self.PT[0][:, 0:512], 4), self.colb(self.KD, h, 8, 4), ALU.mult)
        self.tt(self.v3(RV[:, :], 4), self.v3(self.PT[1][:, 0:512], 4), self.colb(self.BETA, h, 8, 4), ALU.mult)
        self.act(EGB[:], GBC[:], AF.Exp)
        self.tt(QD[:], QN[:, 0:512], EGB[:], ALU.mult)
        self.tt(self.v3(QM[:, :], 4), self.v3(L[:, :], 4), self.rowb(self.C("lm0", True), 4), ALU.mult)
        self.tt(self.v3(M[:, :], 4), self.rowb(identb, 4), self.v3(QM[:, :], 4), ALU.subtract)
        self.tt(self.v3(QM[:, :], 4), self.v3(LT[:, :], 4), self.rowb(self.C("lm0T", True), 4), ALU.mult)
        self.tt(self.v3(MT[:, :], 4), self.rowb(identb, 4), self.v3(QM[:, :], 4), ALU.subtract)
        if nxt is not None:
            self.bg, self.bg_head = self.dn_phase1(l, qt, nxt), nxt
        gs = [slice(0, 256), slice(256, 512)]
        PPb, PQb, PTb = [PB[2], PB[0]], [PB[3], PB[1]], [self.PT[0], self.PT[1]]
        for lv in range(1, 7):
            lm = self.C(f"lm{lv}", True)
            for g in range(2):
                for c in (2 * g, 2 * g + 1):
                    self.mm(PPb[g][:, cs[c]], LT[:, cs[c]], M[:, cs[c]])
            self.bg_step()
            for g in range(2):
                self.cp(Pm[:, gs[g]], PPb[g][:, gs[g]], eng="act")
            self.bg_step()
            for g in range(2):
                for c in (2 * g, 2 * g + 1):
                    self.mm(PQb[g][:, cs[c]], MT[:, cs[c]], Pm[:, cs[c]])
            self.bg_step()
            for g in range(2):
                self.tt(self.v3(QM[:, gs[g]], 2), self.v3(PQb[g][:, gs[g]], 2), self.rowb(lm, 2), ALU.mult)
            for g in range(2):
                self.tt(M[:, gs[g]], M[:, gs[g]], QM[:, gs[g]], ALU.subtract)
            self.bg_step()
            for g in range(2):
                for c in (2 * g, 2 * g + 1):
                    self.tr(PTb[g][:, cs[c]], QM[:, cs[c]], identb)
            self.bg_step()
            for g in range(2):
                self.tt(MT[:, gs[g]], MT[:, gs[g]], PTb[g][:, gs[g]], ALU.subtract)
        while self.bg is not None:
            self.bg_step()
        for c in range(4):
            self.mm(PB[2][:, cs[c]], RK[:, cs[c]], MT[:, cs[c]])
        self.ts(NWT[:], PB[2][:, :], -1.0, ALU.mult)
        if qt == 0:
            self.memset(self.SF[:, h, :], 0.0)
            self.memset(self.SB16[:, h, :], 0.0)
        S16 = self.SB16[:, h, :]
        for c in range(4):
            pv = PB[4][:, 0:128]
            self.mm(pv, MT[:, cs[c]], RV[:, cs[c]], start=True, stop=False)
            self.mm(pv, NWT[:, cs[c]], S16, start=False, stop=True)
            self.cp(VN[:, cs[c]], pv)
            self.mm(PB[5][:, cs[c]], S16, QD[:, cs[c]], start=True, stop=False)
            self.mm(PB[5][:, cs[c]], VN[:, cs[c]], QKT[:, cs[c]], start=False, stop=True)
            ps = PB[3][:, 0:128]
            self.mm(ps, KDEC[:, cs[c]], VN[:, cs[c]])
            self.stt(self.SF[:, h, :], self.SF[:, h, :], self.EGL[:, sc[c]], ps, ALU.mult, ALU.add)
            self.cp(S16, self.SF[:, h, :])
        OT = F[1]
        self.cp(OT[:], PB[5][:, :], eng="act")
        self.dump(f"OT{qt}_{h}", OT[:], [128, 512])
        SQ = F[2]
        self.tt(SQ[:], OT[:], OT[:], ALU.mult)
        self.mm(PB[4][:, :], self.C("ones"), SQ[:])
        self.rsqrt(SQ[:], PB[4][:, :], 1.0 / 128.0, EPS)
        self.tt(OT[:], OT[:], SQ[:], ALU.mult)
        self.stt(self.OGT[:, h, :], OT[:], self.P("onw", l, 1), ZS[:, 0:512], ALU.mult, ALU.mult)

    def merge_out(self, l, qt):
        F, PB = self.F, self.PB
        W0, W1 = self.W
        for oc in range(8):
            self.wload(W1[:, :, 0:128], self.wv("w_in", l, C_G + oc * 128, 128), "w1")
            self.wload(W1[:, :, 128:256], self.wv("w_in", l, C_G + D + oc * 128, 128), "w1")
            for dc in range(8):
                self.mm(PB[0][:, :], W1[:, dc, 0:128], self.HT[:, dc, :], start=(dc == 0), stop=(dc == 7))
            for dc in range(8):
                self.mm(PB[1][:, :], W1[:, dc, 128:256], self.HT[:, dc, :], start=(dc == 0), stop=(dc == 7))
            self.act(F[0][:], PB[0][:, :], AF.Sigmoid)
            self.act(F[1][:], PB[1][:, :], AF.Sigmoid)
            self.wload(W0[:, :, 0:128], self.wv("w_up_dn", l, oc * 128, 128), "w0")
            for h in range(8):
                self.mm(PB[2][:, :], W0[:, h, 0:128], self.OGT[:, h, :], start=(h == 0), stop=(h == 7))
            self.wload(self.WUS[:], self.wv("w_up_sb", l, oc * 128, 128, nk=8, p=64), "wus")
            for h in range(8):
                self.mm(PB[3][:, :], self.WUS[:, h, :], self.OSB[:, h, :], start=(h == 0), stop=(h == 7))
            self.tt(F[2][:], PB[2][:, :], F[0][:], ALU.mult)
            self.tt(F[3][:], PB[3][:, :], F[1][:], ALU.mult)
            self.tt(self.MTm[:, oc, :], F[2][:], F[3][:], ALU.add)
        self.proj_residual(("w_out", l), self.MTm, 8, [4 * qt + i for i in range(4)], 16)

    def proj_residual(self, wkey, actT, nk, tiles, modj, gate_ps=None, blk=None):
        F, PB, H = self.F, self.PB, self.H
        W0f = self.W[0][:, :, :].rearrange("p c t -> p (c t)")
        slotA = W0f[:, 0:nk * 128].rearrange("p (c t) -> p c t", c=nk)
        if 2 * nk * 128 <= 4096:
            slotB = W0f[:, 2048:2048 + nk * 128].rearrange("p (c t) -> p c t", c=nk)
            slots = [([slotA[:, k, :] for k in range(nk)], [(slotA, 0, nk)]),
                     ([slotB[:, k, :] for k in range(nk)], [(slotB, 0, nk)])]
        else:
            nh = (nk + 3) // 4
            bk = [H[k // 4][:, (k % 4) * 128:(k % 4 + 1) * 128] for k in range(nk)]
            bl = [(H[i][:, 0:min(4, nk - 4 * i) * 128].rearrange("p (c t) -> p c t", c=min(4, nk - 4 * i)), 4 * i,
                   min(4, nk - 4 * i)) for i in range(nh)]
            slots = [([slotA[:, k, :] for k in range(nk)], [(slotA, 0, nk)]), (bk, bl)]
        ng = len(tiles) // 4
        it = 0
        pending = None

        def epilogue(pa, pt, Fs, oc, g):
            self.ts(Fs[:], pa[:, :], self.MODT[:, modj + oc:modj + oc + 1], ALU.mult)
            if gate_ps is not None:
                self.tt(Fs[:], Fs[:], gate_ps[g], ALU.mult)
            for i in range(4):
                self.tr(pt[:, i * 128:(i + 1) * 128], Fs[:, i * 128:(i + 1) * 128], self.C("ident"))
            for i in range(4):
                xs = self.X[tiles[g * 4 + i]][:, oc * 128:(oc + 1) * 128]
                self.tt(xs, xs, pt[:, i * 128:(i + 1) * 128], ALU.add)

        for oc in range(8):
            kaps, loads = slots[oc % 2]
            for (dst, k0, n) in loads:
                self.wload(dst, self.wv(wkey[0], wkey[1], oc * 128, 128, nk=n, blk=blk, k0=k0, nkb=nk), "w0")
            for g in range(ng):
                pa, pt, Fs = (PB[0], PB[1], F[0]) if it % 2 == 0 else (PB[2], PB[3], F[3])
                it += 1
                for k in range(nk):
                    self.mm(pa[:, :], kaps[k], actT[:, k, g * 512:(g + 1) * 512], start=(k == 0), stop=(k == nk - 1))
                if pending is not None:
                    epilogue(*pending)
                pending = (pa, pt, Fs, oc, g)
        if pending is not None:
            epilogue(*pending)

    def mixer(self, l):
        self.set_phase("mixer")
        for qt in range(4):
            self.norm_to_hT([4 * qt + i for i in range(4)], self.HT, self.WSC1, self.MODT[:, 0:8])
            self.sb_project(l, qt)
            if "sb" not in self.skip:
                self.sb_attend(qt)
            self.dn_prep(l, qt)
            for h in range(NH):
                if "dn" not in self.skip:
                    self.dn_head(l, qt, h, nxt=(h + 1 if h + 1 < NH else None))
            if "mo" not in self.skip:
                self.merge_out(l, qt)

    def swiglu_up(self, w1key, w3key, blk=None):
        F, PB, H = self.F, self.PB, self.H
        SFb = self.SF[:, :, :].rearrange("p c t -> p (c t)").bitcast(BF16).rearrange("p (c t) -> p c t", c=8)
        abufs = [self.W[1], SFb]
        bbufs = [H[0:4], H[4:8]]
        for f2 in range(DFF // 256):
            A, Bt = abufs[f2 % 2], bbufs[f2 % 2]
            self.wload(A[:, :, :], self.wv(w1key[0], w1key[1], f2 * 256, 256, blk=blk), "w1")
            for t in range(4):
                self.wload(Bt[t][:, :].rearrange("p (c n) -> p c n", c=2),
                           self.wv(w3key[0], w3key[1], f2 * 256, 256, nk=2, blk=blk, k0=2 * t, nkb=8), "w1")
            for sub in range(2):
                fc = 2 * f2 + sub
                for tg in range(2):
                    ts_ = slice(tg * 512, (tg + 1) * 512)
                    pa, pb_, Fs = (PB[2], PB[3], F[1]) if tg == 0 else (PB[4], PB[5], F[2])
                    for dc in range(8):
                        self.mm(pa[:, :], A[:, dc, sub * 128:(sub + 1) * 128], self.H2T[:, dc, ts_], start=(dc == 0), stop=(dc == 7))
                    for dc in range(8):
                        c0 = (dc % 2) * 256 + sub * 128
                        self.mm(pb_[:, :], Bt[dc // 2][:, c0:c0 + 128], self.H2T[:, dc, ts_], start=(dc == 0), stop=(dc == 7))
                    self.act(Fs[:], pa[:, :], AF.Silu)
                    self.tt(self.AT[:, fc, ts_], Fs[:], pb_[:, :], ALU.mult)

    def ffn_dense(self, l):
        self.set_phase("ffn")
        j = l // 2
        for hf in range(2):
            tiles = [8 * hf + i for i in range(8)]
            self.norm_to_hT(tiles, self.H2T, self.WSC2, self.MODT[:, 24:32])
            self.swiglu_up(("ffn_w1", j), ("ffn_w3", j))
            self.proj_residual(("ffn_w2", j), self.AT, DFF // 128, tiles, 40)

    def moe_alloc(self):
        sb = self.sb
        for nm in ("LG", "MX", "EE", "MK", "GATE"):
            setattr(self, nm, sb(nm, [128, 64]))
        self.DEN = sb("DEN", [128, 8])
        self.RW16 = sb("RW16", [128, 64], BF16)

    def ffn_moe(self, l):
        self.set_phase("ffn")
        j = l // 2
        F, PB = self.F, self.PB
        self.cp(self.RW16[:], self.P("rw", j * 64, 64))
        for hf in range(2):
            tiles = [8 * hf + i for i in range(8)]
            self.norm_to_hT(tiles, self.H2T, self.WSC2, self.MODT[:, 24:32])
            for i in range(8):
                for dc in range(8):
                    self.mm(PB[0][:, i * 8:(i + 1) * 8], self.H2T[:, dc, i * 128:(i + 1) * 128],
                            self.RW16[:, dc * 8:(dc + 1) * 8], start=(dc == 0), stop=(dc == 7))
            for i in range(8):
                s8 = slice(i * 8, (i + 1) * 8)
                self.tt(self.LG[:, s8], PB[0][:, s8], self.P("rb", j * 8, 8), ALU.add)
            for i in range(8):
                s8 = slice(i * 8, (i + 1) * 8)
                nc = self.nc
                self.S.op("dve", lambda o=self.MX[:, s8], a=self.LG[:, s8]: nc.vector.max(out=o, in_=a),
                          W=[self.MX[:, s8]], R=[self.LG[:, s8]])
            for i in range(8):
                s8 = slice(i * 8, (i + 1) * 8)
                self.ts(self.EE[:, s8], self.LG[:, s8], self.MX[:, i * 8:i * 8 + 1], ALU.subtract)
                self.ts(self.MK[:, s8], self.LG[:, s8], self.MX[:, i * 8 + 1:i * 8 + 2], ALU.is_ge)
                self.tt(self.DEN[:, i:i + 1], self.MX[:, i * 8 + 1:i * 8 + 2], self.MX[:, i * 8:i * 8 + 1], ALU.subtract)
            self.act(self.EE[:], self.EE[:], AF.Exp)
            self.tt(self.EE[:], self.EE[:], self.MK[:], ALU.mult)
            self.act(self.DEN[:], self.DEN[:], AF.Exp)
            self.ts(self.DEN[:], self.DEN[:], 1.0, ALU.add)
            nc = self.nc
            self.S.op("dve", lambda: nc.vector.reciprocal(out=self.DEN[:], in_=self.DEN[:]), W=[self.DEN[:]], R=[self.DEN[:]])
            for i in range(8):
                s8 = slice(i * 8, (i + 1) * 8)
                self.ts(self.GATE[:, s8], self.EE[:, s8], self.DEN[:, i:i + 1], ALU.mult)
            for i in range(8):
                pb = PB[1] if i < 4 else PB[2]
                self.tr(pb[0:8, (i % 4) * 128:(i % 4 + 1) * 128], self.GATE[:, i * 8:(i + 1) * 8], self.C("ident"))
            self.cp(self.GTS[:], PB[1][0:8, :])
            self.cp(self.GPTS[:], PB[2][0:8, :])
            self.dump(f"gate{l}_{hf}", self.GATE[:], [128, 64])
            for e in range(NE):
                self.swiglu_up(("moe_w1", j), ("moe_w3", j), blk=e)
                sel = self.C("sel")[0:8, e * 128:(e + 1) * 128]
                self.mm(PB[4][:, :], sel, self.GTS[:])
                self.mm(PB[5][:, :], sel, self.GPTS[:])
                self.proj_residual(("moe_w2", j), self.AT, DFF // 128, tiles, 40,
                                   gate_ps=[PB[4][:, :], PB[5][:, :]], blk=e)

    def final(self):
        F = self.F
        self.S.dma("sp", F[0][:], self.fn_in[0:1, 0:512].partition_broadcast(128))
        self.S.dma("sp", F[1][:], self.fn_in[0:1, 512:1024].partition_broadcast(128))
        for hf in range(2):
            tiles = [8 * hf + i for i in range(8)]
            for i, t in enumerate(tiles):
                self.act(self.XN[:], self.X[t][:], AF.Square, accum=self.SS[:, i:i + 1])
            self.rsqrt(self.RS[:, 0:8], self.SS[:, 0:8], 1.0 / D, EPS)
            for i, t in enumerate(tiles):
                for c in range(2):
                    xs = self.X[t][:, c * 512:(c + 1) * 512]
                    self.stt(xs, xs, self.RS[:, i:i + 1], F[c][:], ALU.mult, ALU.mult)
                self.S.dma("sp", self.out[t * 128:(t + 1) * 128, :], self.X[t][:])


W_SHAPES = {"ada_w": (D, 6 * D), "w_in": (D, INC), "w_up_dn": (D, D), "w_up_sb": (512, D), "w_out": (D, D),
            "ffn_w1": (D, DFF), "ffn_w3": (D, DFF), "ffn_w2": (DFF, D),
            "moe_w1": (NE * D, DFF), "moe_w3": (NE * D, DFF), "moe_w2": (NE * DFF, D)}


def weight_list(nlayers=DEPTH):
    out = []
    for l in range(nlayers):
        out += [("ada_w", l), ("w_in", l), ("w_up_dn", l), ("w_up_sb", l), ("w_out", l)]
        j = l // 2
        if l % 2 == 0:
            out += [("ffn_w1", j), ("ffn_w3", j), ("ffn_w2", j)]
        else:
            out += [("moe_w1", j), ("moe_w3", j), ("moe_w2", j)]
    return out


W_KIND = {"ada_w": "row", "w_in": "row", "w_up_dn": "row", "w_up_sb": "row", "w_out": "row",
          "ffn_w1": "row", "ffn_w3": "row", "ffn_w2": "col", "moe_w1": "exp", "moe_w3": "exp", "moe_w2": "exp"}
PACK_C = 2048


def pack_layout(wl):
    off, o = {}, 0
    for (nm, i) in wl:
        rows, cols = W_SHAPES[nm]
        off[(nm, i)] = (o, W_KIND[nm], cols)
        o += rows * cols // NCORES
    nr = (o + PACK_C * 128 - 1) // (PACK_C * 128) * (PACK_C * 128)
    return off, nr


def pack_rank(inputs, wl, r):
    off, nr = pack_layout(wl)
    buf = np.zeros((nr,), np.float32)
    for (nm, i) in wl:
        a = np.asarray(inputs[nm][i], dtype=np.float32)
        o, kind, cols = off[(nm, i)]
        if kind == "row":
            a2 = a.reshape(-1, a.shape[-1])
            rs = a2.shape[0] // NCORES
            seg = a2[r * rs:(r + 1) * rs]
        elif kind == "col":
            seg = a[:, r * 128:(r + 1) * 128]
        else:
            seg = a[r]
        buf[o:o + seg.size] = np.ascontiguousarray(seg).reshape(-1)
    return buf.reshape(-1, PACK_C)


def gather_weights(p, wl):
    nc = p.nc
    off, nr = pack_layout(wl)
    R = nr // PACK_C
    shard = p.dram_in("wpack", [R, PACK_C])
    src = nc.dram_tensor("wpack_c", [R, PACK_C], F32, kind="Internal").ap()
    dst = nc.dram_tensor("wpack_g", [NCORES * R, PACK_C], F32, kind="Internal").ap()
    p.S.dma("pool", src[:, :], shard[:, :])
    p.S.collective(lambda: nc.gpsimd.collective_compute(
        "AllGather", ALU.bypass, replica_groups=[list(range(NCORES))], ins=[src[:, :]], outs=[dst[:, :]]),
        dst[:, :], src[:, :])
    p.S.wait_all("pool", [dst[:, :]])
    p.wpack, p.wpack_off, p.wpack_nr = dst, off, nr


REPLICATE = True


def build_full(nlayers=DEPTH):
    p = Prog(direct=REPLICATE)
    p.setup()
    p.moe_alloc()
    wl = weight_list(nlayers)
    if REPLICATE:
        p.direct_weights(wl)
    else:
        gather_weights(p, wl)
    for l in range(nlayers):
        p.layer_mod(l)
        p.mixer(l)
        if l % 2 == 0:
            p.ffn_dense(l)
        else:
            p.ffn_moe(l)
    p.final()
    p.finish()
    return p


def make_in_maps(inputs, nlayers=DEPTH):
    wl = weight_list(nlayers)
    maps = []
    fn = np.ascontiguousarray(np.asarray(inputs["final_norm"], dtype=np.float32).reshape(1, D))
    shared = {}
    if REPLICATE:
        for (nm, i) in wl:
            a = np.asarray(inputs[nm][i], dtype=np.float32)
            shared[f"{nm}_{i}"] = a.reshape(-1, a.shape[-1])
    for b in range(NCORES):
        sp, _ = _small_params(inputs, b)
        m = {"x": np.ascontiguousarray(inputs["x"][b], dtype=np.float32), "cstf": CSTF_NP, "cstb": CSTB_NP,
             "sp": sp, "final_norm": fn}
        if REPLICATE:
            m.update(shared)
        else:
            m["wpack"] = pack_rank(inputs, wl, b)
        maps.append(m)
    return maps


def kernel(**inputs):
    inputs = {k: np.asarray(v) for k, v in inputs.items()}
    p = build_full()
    in_maps = make_in_maps(inputs)
    res = run_bass_kernel_spmd(p.nc, in_maps, core_ids=list(range(NCORES)))
    out = np.stack([np.asarray(r["out"], dtype=np.float32) for r in res.results], axis=0)
    return out.reshape(NCORES, S, D)
```
